# Optimizing a Trainium2 kernel written in Bass

```python
import jax, jax.numpy as jnp
from jax import lax
import numpy as np

D_MODEL = 1024
BATCH = 16
SEQ = 4096
DEPTH = 4

GRID_W = 64
CTX_LEN = 256
HEAD_DIM = 64
RET_HEADS = 4
RET_DK = 64
RET_DV = 128
RET_CHUNK = 128
WIN_Q_HEADS = 4
WIN_KV_HEADS = 2
WINDOW = 128
GLB_Q_HEADS = 4
GLB_KV_HEADS = 2
BLOCK_Q = 128
ROPE_BASE = 10000.0
N_EXPERTS = 16
N_GROUPS = 4
EXPERTS_PER_GROUP = N_EXPERTS // N_GROUPS
TOP_K = 2
D_EXPERT = 1024
MOE_BLOCK = 256
EPS = 1e-6
ADA_INIT = 0.3
NEG_INF = -1e30
SPLIT_SIZES = (RET_HEADS * RET_DK, RET_HEADS * RET_DK, RET_HEADS * RET_DV, RET_HEADS * RET_DV,
               WIN_Q_HEADS * HEAD_DIM, WIN_KV_HEADS * HEAD_DIM, WIN_KV_HEADS * HEAD_DIM,
               GLB_Q_HEADS * HEAD_DIM, GLB_KV_HEADS * HEAD_DIM, GLB_KV_HEADS * HEAD_DIM)
D_IN = sum(SPLIT_SIZES)
D_MIX = RET_HEADS * RET_DV + WIN_Q_HEADS * HEAD_DIM + GLB_Q_HEADS * HEAD_DIM

kernel_name = 'hybrid_parallel_heads_retention_swa_axial_moe_dit'


def _rms(x, gain):
    x32 = x.astype(jnp.float32)
    y = x32 * lax.rsqrt(jnp.mean(x32 * x32, axis=-1, keepdims=True) + EPS)
    return (y * gain.astype(jnp.float32)).astype(x.dtype)


def _modulate(h, shift, scale):
    return h * (1 + scale) + shift


def _split_cols(p):
    outs, start = [], 0
    for s in SPLIT_SIZES:
        outs.append(p[..., start:start + s])
        start += s
    return outs


def _heads(t, n_heads):
    b, l, _ = t.shape
    return t.reshape(b, l, n_heads, -1).transpose(0, 2, 1, 3)


def _rope_tables(n_tok, dtype):
    rows = n_tok // GRID_W
    row = jnp.repeat(jnp.arange(rows), GRID_W).astype(jnp.float32)
    col = (jnp.arange(rows * GRID_W) % GRID_W).astype(jnp.float32)
    half = HEAD_DIM // 2
    inv = jnp.power(ROPE_BASE, -jnp.arange(0, half, 2, dtype=jnp.float32) / half)
    ang = jnp.concatenate([row[:, None] * inv, col[:, None] * inv], -1)
    return jnp.cos(ang).astype(dtype), jnp.sin(ang).astype(dtype)


def _rope_2d(x, cos, sin):
    q = HEAD_DIM // 4
    parts = []
    for a in range(2):
        xa = x[..., 2 * a * q:(2 * a + 2) * q]
        x1, x2 = xa[..., :q], xa[..., q:]
        c, s = cos[:, a * q:(a + 1) * q], sin[:, a * q:(a + 1) * q]
        parts += [x1 * c - x2 * s, x2 * c + x1 * s]
    return jnp.concatenate(parts, -1)


def _attn_prep(q, k, v, n_q, n_kv, gain, rope):
    q = _rms(_heads(q, n_q), gain[0])
    k = _rms(_heads(k, n_kv), gain[1])
    v = _heads(v, n_kv)
    if rope is not None:
        q = _rope_2d(q, *rope)
        k = _rope_2d(k, *rope)
    b, _, l, _ = q.shape
    q = (q * HEAD_DIM ** -0.5).reshape(b, n_kv, n_q // n_kv, l, HEAD_DIM)
    return q, k, v


def _attend(q, k, v, mask, sink):
    s = jnp.einsum('bhgqd,bhkd->bhgqk', q, k).astype(jnp.float32)
    if mask is not None:
        s = jnp.where(mask, s, NEG_INF)
    m = jnp.max(s, axis=-1, keepdims=True)
    if sink is not None:
        sk = sink.astype(jnp.float32)[None, :, :, None, None]
        m = jnp.maximum(m, sk)
    p = jnp.exp(s - m)
    den = jnp.sum(p, axis=-1, keepdims=True)
    if sink is not None:
        den = den + jnp.exp(sk - m)
    return jnp.einsum('bhgqk,bhkd->bhgqd', (p / den).astype(v.dtype), v)


def _to_blocks(q):
    b, hk, g, l, hd = q.shape
    return jnp.moveaxis(q.reshape(b, hk, g, l // BLOCK_Q, BLOCK_Q, hd), 3, 0)


def _from_blocks(o):
    nb, b, hk, g, bq, hd = o.shape
    return jnp.moveaxis(o, 0, 3).reshape(b, hk, g, nb * bq, hd)


def _merge_heads(o):
    b, hk, g, l, hd = o.shape
    return o.reshape(b, hk * g, l, hd).transpose(0, 2, 1, 3).reshape(b, l, hk * g * hd)


def _ret_scan(q, k, v, log_g, s0):
    b, h, l, _ = q.shape
    n = l // RET_CHUNK
    idx = jnp.arange(RET_CHUNK, dtype=jnp.float32)
    diff = idx[:, None] - idx[None, :]
    d_in = jnp.where(diff >= 0, jnp.exp(jnp.maximum(diff, 0.0) * log_g[:, None, None]), 0.0).astype(q.dtype)
    xi = jnp.exp((idx + 1.0) * log_g[:, None]).astype(q.dtype)[..., None]
    zeta = jnp.exp((RET_CHUNK - 1.0 - idx) * log_g[:, None]).astype(q.dtype)[..., None]
    g_c = jnp.exp(RET_CHUNK * log_g).astype(q.dtype)[:, None, None]

    def chunks(t):
        return jnp.moveaxis(t.reshape(b, h, n, RET_CHUNK, t.shape[-1]), 2, 0)

    def step(s, inp):
        qi, ki, vi = inp
        att = jnp.einsum('bhqd,bhkd->bhqk', qi, ki) * d_in
        o = jnp.einsum('bhqk,bhkv->bhqv', att, vi) + jnp.einsum('bhqd,bhdv->bhqv', qi, s) * xi
        s = g_c * s + jnp.einsum('bhkd,bhkv->bhdv', ki * zeta, vi)
        return s, o

    s_fin, o = lax.scan(step, s0, (chunks(q), chunks(k), chunks(v)))
    return jnp.moveaxis(o, 0, 2).reshape(b, h, l, -1), s_fin


def _ret_out(o, g):
    o32 = o.astype(jnp.float32)
    mu = jnp.mean(o32, axis=-1, keepdims=True)
    var = jnp.mean(jnp.square(o32 - mu), axis=-1, keepdims=True)
    o = ((o32 - mu) * lax.rsqrt(var + EPS)).astype(g.dtype)
    b, h, l, dv = o.shape
    return jax.nn.silu(g) * o.transpose(0, 2, 1, 3).reshape(b, l, h * dv)


def _retention(q, k, v, g, qc, kc, vc, gc, decay_logit, need_ctx):
    log_g = jax.nn.log_sigmoid(decay_logit.astype(jnp.float32))

    def prep(q, k, v):
        return _heads(q, RET_HEADS), _heads(k, RET_HEADS) * RET_DK ** -0.5, _heads(v, RET_HEADS)

    q, k, v = prep(q, k, v)
    qc, kc, vc = prep(qc, kc, vc)
    flip = lambda t: jnp.flip(t, axis=2)
    s0 = jnp.zeros((q.shape[0], RET_HEADS, RET_DK, RET_DV), q.dtype)
    oc_f, s_f = _ret_scan(qc, kc, vc, log_g[0], s0)
    oc_b, s_b = _ret_scan(flip(qc), flip(kc), flip(vc), log_g[1], s0)
    o_f, _ = _ret_scan(q, k, v, log_g[0], s_f)
    o_b, _ = _ret_scan(flip(q), flip(k), flip(v), log_g[1], s_b)
    y = _ret_out(o_f + flip(o_b), g)
    yc = _ret_out(oc_f + flip(oc_b), gc) if need_ctx else None
    return y, yc


def _window_mixer(q, k, v, qc, kc, vc, gain, sink, rope, need_ctx):
    q, k, v = _attn_prep(q, k, v, WIN_Q_HEADS, WIN_KV_HEADS, gain, rope)
    qc, kc, vc = _attn_prep(qc, kc, vc, WIN_Q_HEADS, WIN_KV_HEADS, gain, None)
    sink = sink.reshape(WIN_KV_HEADS, WIN_Q_HEADS // WIN_KV_HEADS)
    l = q.shape[3]
    nb = l // BLOCK_Q
    span = BLOCK_Q + 2 * WINDOW
    pad = ((0, 0), (0, 0), (WINDOW, WINDOW), (0, 0))
    kp, vp = jnp.pad(k, pad), jnp.pad(v, pad)
    off = jnp.arange(span) - WINDOW
    band = jnp.abs(off[None, :] - jnp.arange(BLOCK_Q)[:, None]) <= WINDOW
    ctx_ok = jnp.ones((BLOCK_Q, kc.shape[2]), bool)

    def block(args):
        qb, i = args
        start = i * BLOCK_Q
        kw = lax.dynamic_slice_in_dim(kp, start, span, axis=2)
        vw = lax.dynamic_slice_in_dim(vp, start, span, axis=2)
        kpos = start + off
        mask = band & ((kpos >= 0) & (kpos < l))[None, :]
        return _attend(qb, jnp.concatenate([kw, kc], 2), jnp.concatenate([vw, vc], 2),
                       jnp.concatenate([mask, ctx_ok], 1), sink)

    o = lax.map(block, (_to_blocks(q), jnp.arange(nb)))
    y = _merge_heads(_from_blocks(o))
    yc = _merge_heads(_attend(qc, kc, vc, None, sink)) if need_ctx else None
    return y, yc


def _global_mixer(q, k, v, qc, kc, vc, gain, rope, need_ctx):
    q, k, v = _attn_prep(q, k, v, GLB_Q_HEADS, GLB_KV_HEADS, gain, rope)
    qc, kc, vc = _attn_prep(qc, kc, vc, GLB_Q_HEADS, GLB_KV_HEADS, gain, None)
    k_all = jnp.concatenate([k, kc], 2)
    v_all = jnp.concatenate([v, vc], 2)
    o = lax.map(lambda qb: _attend(qb, k_all, v_all, None, None), _to_blocks(q))
    y = _merge_heads(_from_blocks(o))
    yc = _merge_heads(_attend(qc, kc, vc, None, None)) if need_ctx else None
    return y, yc


def _mixers(px, pc, ret_decay, win_gain, win_sink, glb_gain, rope, need_ctx):
    rq, rk, rv, rg, wq, wk, wv, gq, gk, gv = _split_cols(px)
    rqc, rkc, rvc, rgc, wqc, wkc, wvc, gqc, gkc, gvc = _split_cols(pc)
    yr, yrc = _retention(rq, rk, rv, rg, rqc, rkc, rvc, rgc, ret_decay, need_ctx)
    yw, ywc = _window_mixer(wq, wk, wv, wqc, wkc, wvc, win_gain, win_sink, rope, need_ctx)
    yg, ygc = _global_mixer(gq, gk, gv, gqc, gkc, gvc, glb_gain, rope, need_ctx)
    y = jnp.concatenate([yr, yw, yg], -1)
    yc = jnp.concatenate([yrc, ywc, ygc], -1) if need_ctx else None
    return y, yc


def _route(h, w_router, b_router):
    s = jax.nn.sigmoid(jnp.dot(h, w_router).astype(jnp.float32))
    grouped = (s + b_router.astype(jnp.float32)).reshape(-1, N_GROUPS, EXPERTS_PER_GROUP)
    gscore = lax.top_k(grouped, TOP_K)[0].sum(-1)
    grp = jnp.argmax(gscore, axis=-1)
    cand = jnp.take_along_axis(grouped, grp[:, None, None], axis=1)[:, 0]
    _, loc = lax.top_k(cand, TOP_K)
    e_idx = grp[:, None] * EXPERTS_PER_GROUP + loc
    w = jnp.take_along_axis(s, e_idx, axis=1)
    return e_idx, w / jnp.sum(w, axis=-1, keepdims=True)


def _moe(h, w_router, b_router, w_gu, w_down):
    t, d = h.shape
    e_idx, gate = _route(h, w_router, b_router)
    flat_e = e_idx.reshape(-1)
    order = jnp.argsort(flat_e)
    e_sorted = flat_e[order]
    tok = order // TOP_K
    counts = jnp.bincount(flat_e, length=N_EXPERTS)
    padded = (counts + MOE_BLOCK - 1) // MOE_BLOCK * MOE_BLOCK
    pad_end = jnp.cumsum(padded)
    dest = (pad_end - padded)[e_sorted] + jnp.arange(t * TOP_K) - (jnp.cumsum(counts) - counts)[e_sorted]
    n_blocks = (t * TOP_K + N_EXPERTS * (MOE_BLOCK - 1) + MOE_BLOCK - 1) // MOE_BLOCK
    xp = jnp.zeros((n_blocks * MOE_BLOCK, d), h.dtype).at[dest].set(h[tok])
    blk_e = jnp.minimum(jnp.searchsorted(pad_end, jnp.arange(n_blocks) * MOE_BLOCK, side='right'), N_EXPERTS - 1)

    def expert_block(args):
        xb, e = args
        a, u = jnp.split(xb @ w_gu[e], 2, axis=-1)
        return (jax.nn.silu(a) * u) @ w_down[e]

    yp = lax.map(expert_block, (xp.reshape(n_blocks, MOE_BLOCK, d), blk_e)).reshape(-1, d)
    y = yp[dest] * gate.reshape(-1)[order][:, None].astype(h.dtype)
    return jnp.zeros_like(h).at[tok].add(y)


def setup_inputs(seed: int = 0) -> dict:
    key = jax.random.key(seed)
    ks = jax.random.split(key, 20)
    f32 = jnp.float32
    nrm = lambda k, shape, scale: jax.random.normal(k, shape, f32) * scale
    base_gamma = 1.0 - jnp.power(2.0, -5.0 - jnp.arange(RET_HEADS, dtype=f32))
    base_logit = jnp.log(base_gamma) - jnp.log1p(-base_gamma)
    return {
        'x': nrm(ks[0], (BATCH, SEQ, D_MODEL), 1.0),
        'c': nrm(ks[1], (BATCH, D_MODEL), 1.0),
        'ctx': nrm(ks[2], (BATCH, CTX_LEN, D_MODEL), 1.0),
        'c_ctx': nrm(ks[3], (D_MODEL,), 1.0),
        'ada_w': nrm(ks[4], (DEPTH, D_MODEL, 6 * D_MODEL), ADA_INIT * D_MODEL ** -0.5),
        'ada_b': nrm(ks[5], (DEPTH, 6 * D_MODEL), 0.02),
        'norm1': 1.0 + nrm(ks[6], (DEPTH, D_MODEL), 0.02),
        'norm2': 1.0 + nrm(ks[7], (DEPTH, D_MODEL), 0.02),
        'w_in': nrm(ks[8], (DEPTH, D_MODEL, D_IN), D_MODEL ** -0.5),
        'w_out': nrm(ks[9], (DEPTH, D_MIX, D_MODEL), D_MIX ** -0.5),
        'ret_decay': base_logit[None, None, :] + nrm(ks[10], (DEPTH, 2, RET_HEADS), 0.1),
        'win_qk_gain': 1.0 + nrm(ks[11], (DEPTH, 2, HEAD_DIM), 0.02),
        'win_sink': nrm(ks[12], (DEPTH, WIN_Q_HEADS), 0.5),
        'glb_qk_gain': 1.0 + nrm(ks[13], (DEPTH, 2, HEAD_DIM), 0.02),
        'w_router': nrm(ks[14], (D_MODEL, N_EXPERTS), D_MODEL ** -0.5),
        'b_router': nrm(ks[15], (N_EXPERTS,), 0.01),
        'w_gate_up': nrm(ks[16], (DEPTH, N_EXPERTS, D_MODEL, 2 * D_EXPERT), D_MODEL ** -0.5),
        'w_down': nrm(ks[17], (DEPTH, N_EXPERTS, D_EXPERT, D_MODEL), D_EXPERT ** -0.5),
    }


def reference(x, c, ctx, c_ctx, ada_w, ada_b, norm1, norm2, w_in, w_out, ret_decay, win_qk_gain, win_sink,
              glb_qk_gain, w_router, b_router, w_gate_up, w_down):
    b, l, d = x.shape
    rope = _rope_tables(l, x.dtype)
    sc, sc_ctx = jax.nn.silu(c), jax.nn.silu(c_ctx)
    for i in range(DEPTH):
        last = i == DEPTH - 1
        mod = (sc @ ada_w[i] + ada_b[i])[:, None, :]
        mod_c = sc_ctx @ ada_w[i] + ada_b[i]
        sh1, s1, g1, sh2, s2, g2 = jnp.split(mod, 6, axis=-1)
        sh1c, s1c, g1c, sh2c, s2c, g2c = jnp.split(mod_c, 6, axis=-1)
        px = _modulate(_rms(x, norm1[i]), sh1, s1) @ w_in[i]
        pc = _modulate(_rms(ctx, norm1[i]), sh1c, s1c) @ w_in[i]
        yx, yc = _mixers(px, pc, ret_decay[i], win_qk_gain[i], win_sink[i], glb_qk_gain[i], rope, not last)
        x = x + g1 * (yx @ w_out[i])
        hx = _modulate(_rms(x, norm2[i]), sh2, s2).reshape(b * l, d)
        if last:
            x = x + g2 * _moe(hx, w_router, b_router, w_gate_up[i], w_down[i]).reshape(b, l, d)
        else:
            ctx = ctx + g1c * (yc @ w_out[i])
            hc = _modulate(_rms(ctx, norm2[i]), sh2c, s2c).reshape(-1, d)
            y = _moe(jnp.concatenate([hx, hc], 0), w_router, b_router, w_gate_up[i], w_down[i])
            x = x + g2 * y[:b * l].reshape(b, l, d)
            ctx = ctx + g2c * y[b * l:].reshape(ctx.shape)
    return x
```

```python
import contextlib
import types
import numpy as np
import ml_dtypes
import concourse.bass as bass
import concourse.mybir as mybir
from concourse.bass_utils import run_bass_kernel_spmd

F32 = mybir.dt.float32
BF16 = mybir.dt.bfloat16
AF = mybir.ActivationFunctionType
ALU = mybir.AluOpType
AX = mybir.AxisListType

PHYS = {'pe': 'tensor', 'act': 'scalar', 'dve': 'vector', 'pool': 'gpsimd',
        'sp': 'sync', 'pq': 'gpsimd'}
IS_DMA = {'sp', 'pq'}


NS = 8


def freeze(fn):
    if fn.__closure__ is None:
        return fn
    cells = []
    for c in fn.__closure__:
        try:
            cells.append(types.CellType(c.cell_contents))
        except ValueError:
            cells.append(c)
    return types.FunctionType(fn.__code__, fn.__globals__, fn.__name__, fn.__defaults__, tuple(cells))


class Sync:
    def __init__(self, nc, stack):
        self.nc = nc
        self.sem = {}
        self.base = {}
        for e in PHYS:
            if e in IS_DMA:
                self.sem[e] = [stack.enter_context(nc.semaphore(f"s_{e}{k}")) for k in range(NS)]
                self.base[e] = [0] * NS
            else:
                self.sem[e] = stack.enter_context(nc.semaphore(f"s_{e}"))
                self.base[e] = 0


class Phase:
    def __init__(self, sync, name):
        self.sync = sync
        self.nc = sync.nc
        self.name = name
        self.ops = {p: [] for p in ('tensor', 'scalar', 'vector', 'gpsimd', 'sync')}
        self.seq = {e: 0 for e in PHYS}
        self.res = {}
        self.flag = {e: set() for e in PHYS}

    def op(self, eng, fn, reads=(), writes=()):
        deps = set()

        def need(d):
            if d is None:
                return
            if d[0] == 'pe' and eng == 'pe':
                return
            deps.add(d)

        for r in reads:
            st = self.res.get(r)
            if st is not None:
                need(st[0])
        for w in writes:
            st = self.res.get(w)
            if st is not None:
                need(st[0])
                for d in st[1]:
                    need(d)
        self.seq[eng] += 1
        me = (eng, self.seq[eng])
        for r in reads:
            st = self.res.get(r)
            if st is None:
                self.res[r] = [None, [me]]
            else:
                st[1].append(me)
        for w in writes:
            self.res[w] = [me, []]
        if eng in IS_DMA and self.seq[eng] > NS:
            deps.add((eng, self.seq[eng] - NS))
        dd = {}
        for e, sq in deps:
            key = (e, (sq - 1) % NS) if e in IS_DMA else (e, 0)
            dd[key] = max(dd.get(key, 0), sq)
        self.ops[PHYS[eng]].append((eng, self.seq[eng], dd, freeze(fn)))
        return me

    def emit(self):
        nc = self.nc
        sy = self.sync
        final_wait = {}
        for e in IS_DMA:
            for sq in range(max(1, self.seq[e] - NS + 1), self.seq[e] + 1):
                final_wait[(e, (sq - 1) % NS)] = sq
        for p, lst in self.ops.items():
            seen = {}
            for i, (eng, seq, dd, fn) in enumerate(lst):
                nd = {}
                for key, sq in dd.items():
                    if seen.get(key, 0) >= sq:
                        continue
                    seen[key] = sq
                    nd[key] = sq
                    if key[0] not in IS_DMA:
                        self.flag[key[0]].add(sq)
                lst[i] = (eng, seq, nd, fn)
        rank = {}
        for e in PHYS:
            if e in IS_DMA:
                continue
            fl = sorted(self.flag[e])
            rank[e] = {sq: i + 1 for i, sq in enumerate(fl)}

        def wait(engine, key, sq):
            e = key[0]
            if e in IS_DMA:
                slot = (sq - 1) % NS
                engine.wait_ge(sy.sem[e][slot], (sy.base[e][slot] + (sq - 1) // NS + 1) * 16)
            else:
                engine.wait_ge(sy.sem[e], sy.base[e] + rank[e][sq])

        with nc.Block() as block:
            for p, lst in self.ops.items():
                fw = final_wait if p == 'sync' else {}
                if not lst and not fw:
                    continue

                def body(engine, lst=lst, fw=fw):
                    for eng, seq, nd, fn in lst:
                        for key, sq in nd.items():
                            wait(engine, key, sq)
                        inst = fn(engine)
                        if eng in IS_DMA:
                            inst.then_inc(sy.sem[eng][(seq - 1) % NS], 16)
                        elif seq in self.flag[eng]:
                            inst.then_inc(sy.sem[eng], 1)
                    for key, sq in fw.items():
                        wait(engine, key, sq)

                getattr(block, p)(body)
        for e in PHYS:
            if e in IS_DMA:
                for q in range(1, self.seq[e] + 1):
                    sy.base[e][(q - 1) % NS] += 1
            else:
                sy.base[e] += len(self.flag[e])
        return sum(len(l) for l in self.ops.values())


class Rot:
    def __init__(self, tiles, name):
        self.t = tiles
        self.name = name
        self.i = 0

    def next(self):
        k = self.i % len(self.t)
        self.i += 1
        return self.t[k], (self.name, k)


D = 1024
KC = 8
CTX = 256
EPS = 1e-6
NE = 16
MB = 512
I32 = mybir.dt.int32


def host_constants(L):
    c = {}
    c['ident'] = np.eye(128, dtype=np.float32)
    blk = np.zeros((128, 128), np.float32)
    blk[:64, :64] = 1.0
    blk[64:, 64:] = 1.0
    c['blk64'] = blk
    prot = np.zeros((128, 128), np.float32)
    for m in range(128):
        if (m % 32) < 16:
            prot[m + 16, m] = -1.0
        else:
            prot[m - 16, m] = 1.0
    c['prot'] = prot
    t = np.arange(L)
    row = (t // 64).astype(np.float32)
    col = (t % 64).astype(np.float32)
    inv = np.power(np.float32(10000.0), -np.arange(0, 32, 2, dtype=np.float32) / np.float32(32)).astype(np.float32)
    cosT = np.zeros((128, L), np.float32)
    sinT = np.zeros((128, L), np.float32)
    for p in range(128):
        d = p % 64
        a = d // 32
        i = d % 16
        ang = (row if a == 0 else col) * inv[i]
        cosT[p] = np.cos(ang.astype(np.float32))
        sinT[p] = np.sin(ang.astype(np.float32))
    c['cosT'] = cosT
    c['sinT'] = sinT
    k = np.arange(128)[:, None].astype(np.float32)
    q = np.arange(128)[None, :].astype(np.float32)
    c['dif_f'] = np.maximum(q - k, 0.0).astype(np.float32)
    c['msk_f'] = (q >= k).astype(np.float32)
    c['dif_b'] = np.maximum(k - q, 0.0).astype(np.float32)
    c['msk_b'] = (k >= q).astype(np.float32)
    iot = np.zeros((128, 4 * 128), np.float32)
    iot[:, 0:128] = q + 1.0
    iot[:, 128:256] = 128.0 - q
    iot[:, 256:384] = 127.0 - k
    iot[:, 384:512] = k + 0.0 * q
    c['iot'] = iot
    wm = np.zeros((6, 128, 512), np.float32)
    for oi, o in enumerate(range(-1, 5)):
        kp = o * 128 + np.arange(128)[:, None]
        qp = np.arange(512)[None, :]
        wm[oi] = np.where(np.abs(kp - qp) <= 128, 0.0, -30000.0)
    c['wmask'] = wm
    sel = np.zeros((16, 16 * 128), np.float32)
    for e in range(16):
        sel[e, e * 128:(e + 1) * 128] = 1.0
    c['sel'] = sel
    c['ustrict'] = (np.arange(128)[:, None] < np.arange(128)[None, :]).astype(np.float32)
    c['pidx'] = (np.arange(8)[None, :] * 128 + np.arange(128)[:, None]).astype(np.float32)
    c['blkiota'] = np.broadcast_to((np.arange(64) * float(MB))[None, :], (128, 64)).astype(np.float32).copy()
    return c


CONST_SHAPES = lambda L: {'ident': [128, 128], 'blk64': [128, 128], 'prot': [128, 128], 'cosT': [128, L],
                          'sinT': [128, L], 'dif_f': [128, 128], 'msk_f': [128, 128], 'dif_b': [128, 128],
                          'msk_b': [128, 128], 'iot': [128, 512], 'wmask': [6, 128, 512], 'sel': [16, 2048],
                          'ustrict': [128, 128], 'blkiota': [128, 64], 'pidx': [128, 8]}


class K:
    pass


def build(L, NB, DEPTH):
    s = K()
    s.L, s.NB, s.DEPTH = L, NB, DEPTH
    T = s.T = CTX + L
    NT = s.NT = T // 128
    NBC = s.NBC = NB + 1
    TALL = s.TALL = NB * T
    nc = s.nc = bass.Bass("TRN2", target_bir_lowering=False)

    def din(name, shape):
        return nc.dram_tensor(name, list(shape), F32, kind="ExternalInput").ap()

    s.x_in = din("x", [NB, L, D])
    s.c_in = din("c", [NB, D])
    s.ctx_in = din("ctx", [NB, CTX, D])
    s.cctx_in = din("c_ctx", [D])
    s.ada_w = din("ada_w", [DEPTH, D, 6 * D])
    s.ada_b = din("ada_b", [DEPTH, 6 * D])
    s.norm1 = din("norm1", [DEPTH, D])
    s.norm2 = din("norm2", [DEPTH, D])
    s.w_in = din("w_in", [DEPTH, D, 2560])
    s.w_out = din("w_out", [DEPTH, D, D])
    s.ret_decay = din("ret_decay", [DEPTH, 8])
    s.win_gain = din("win_qk_gain", [DEPTH, 2, 64])
    s.win_sink = din("win_sink", [DEPTH, 4])
    s.glb_gain = din("glb_qk_gain", [DEPTH, 2, 64])
    s.w_router = din("w_router", [D, NE])
    s.b_router = din("b_router", [NE])
    s.w_gu = din("w_gate_up", [DEPTH, NE, D, 2 * D])
    s.w_dn = din("w_down", [DEPTH, NE, D, D])
    s.cst = {k: din("k_" + k, sh) for k, sh in CONST_SHAPES(L).items()}
    s.out_x = nc.dram_tensor("out", [NB, L, D], F32, kind="ExternalOutput").ap()
    s.out_c = nc.dram_tensor("ctx_out", [NB, CTX, D], F32, kind="ExternalOutput").ap()
    import os
    ext = os.environ.get("EXT", "").split(",")
    dkf = lambda nm: dict(kind="ExternalOutput") if (DEBUG or nm in ext) else {}
    dk = {}
    s.XT = nc.dram_tensor("XT", [NB, KC, 128, T], F32, **dkf("XT")).ap()
    s.FM = nc.dram_tensor("FMs", [12, 128, T], BF16, **dkf("FM")).ap()
    s.TOK = nc.dram_tensor("TOKs", [T, 1536], BF16, **dkf("TOK")).ap()
    s.YT = nc.dram_tensor("YTs", [KC, 128, T], BF16, **dkf("YT")).ap()
    s.H2T = nc.dram_tensor("H2Ts", [KC, 128, TALL], BF16, **dkf("H2T")).ap()
    s.WGT = nc.dram_tensor("WGTs", [NE, TALL], F32, **dkf("WGT")).ap()
    s.NTA = NB * NT
    s.NBLK = (2 * TALL + NE * (MB - 1) + MB - 1) // MB
    assert s.NBLK <= 64
    s.WBgu = nc.dram_tensor("WBgu", [NE, 128, KC, 2 * D], BF16).ap()
    s.WBdn = nc.dram_tensor("WBdn", [NE, 128, KC, D], BF16).ap()
    s.H2TOK = nc.dram_tensor("H2TOK", [TALL, D], BF16).ap()
    s.XP = nc.dram_tensor("XPs", [s.NBLK * MB, D], BF16).ap()
    s.YP = nc.dram_tensor("YPs", [s.NBLK * MB, D], F32).ap()
    s.chunks = [(0, CTX)] + [(CTX + i, min(512, L - i)) for i in range(0, L, 512)]
    s.nphase = 0
    s.ninst = 0
    with contextlib.ExitStack() as gst:
        s.gst = gst
        s.sync = Sync(nc, gst)
        s.ident = sb(s, "ident", [128, 128], F32)
        s.ident_bf = sb(s, "ident_bf", [128, 128], BF16)
        s.ones_bf = sb(s, "ones_bf", [128, 128], BF16)
        s.ones_f = sb(s, "ones_f", [128, 128], F32)
        s.blk64 = sb(s, "blk64", [128, 128], BF16)
        s.prot = sb(s, "prot", [128, 128], BF16)
        s.modT = sb(s, "modT", [128, DEPTH, 48, NBC], F32)
        s.gs1 = sb(s, "gs1", [128, DEPTH, KC, NBC], F32)
        s.gs2 = sb(s, "gs2", [128, DEPTH, KC, NBC], F32)
        s.wr_bf = sb(s, "wr_bf", [128, KC, NE], BF16)
        s.br_bc = sb(s, "br_bc", [128, NE], F32)
        s.ustrict = sb(s, "ustrict", [128, 128], F32)
        s.blkiota = sb(s, "blkiota", [128, 64], F32)
        s.SELt = sb(s, "SELt", [128, s.NTA, NE], F32)
        s.WGt = sb(s, "WGt", [128, s.NTA, NE], F32)
        s.RKt = sb(s, "RKt", [128, s.NTA, NE], F32)
        s.cbase = sb(s, "cbase", [128, NE], F32)
        s.DAf = sb(s, "DAf", [128, s.NTA], F32)
        s.DBf = sb(s, "DBf", [128, s.NTA], F32)
        s.DAi = sb(s, "DAi", [128, s.NTA], I32)
        s.DBi = sb(s, "DBi", [128, s.NTA], I32)
        s.WA = sb(s, "WA", [128, s.NTA], F32)
        s.WB = sb(s, "WB", [128, s.NTA], F32)
        s.IDXi = sb(s, "IDXi", [128, 64], I32)
        s.pidx = sb(s, "pidx", [128, KC], F32)
        s.PS = [gst.enter_context(nc.psum_tensor(f"ps{i}", [128, 512], F32)) for i in range(7)]
        s.PSB = gst.enter_context(nc.psum_tensor("psb", [128, 1024], BF16))
        phase_consts(s)
        phase_input(s)
        for l in range(DEPTH):
            with contextlib.ExitStack() as stL:
                phase_tables(s, l, stL)
                for b in range(NB):
                    phase_proj(s, l, b)
                    phase_ret(s, l, b)
                    phase_attn(s, l, b)
                    phase_out(s, l, b)
            phase_dest(s, l)
            phase_moe_sparse(s, l)
            phase_comb(s, l)
    return nc, s.ninst


def sb(s, name, shape, dt, st=None):
    s.nsb = getattr(s, 'nsb', 0) + 1
    return (st or s.gst).enter_context(s.nc.sbuf_tensor(f"{name}_{s.nsb}", list(shape), dt))


def new_phase(s, tag):
    s.nphase += 1
    return Phase(s.sync, f"{tag}{s.nphase}")


def done(s, ph):
    s.ninst += ph.emit()


def phase_consts(s):
    nc, NB, NBC, DEPTH, cst = s.nc, s.NB, s.NBC, s.DEPTH, s.cst
    with contextlib.ExitStack() as st0:
        ph = new_phase(s, "c")
        ph.op('sp', lambda e: e.dma_start(out=s.ident[:], in_=cst['ident']), writes=['ident'])
        ph.op('pq', lambda e: e.dma_start(out=s.ident_bf[:], in_=cst['ident']), writes=['ident_bf'])
        ph.op('pq', lambda e: e.dma_start(out=s.blk64[:], in_=cst['blk64']), writes=['blk64'])
        ph.op('pq', lambda e: e.dma_start(out=s.prot[:], in_=cst['prot']), writes=['prot'])
        ph.op('sp', lambda e: e.dma_start(out=s.ustrict[:], in_=cst['ustrict']), writes=['ustrict'])
        ph.op('sp', lambda e: e.dma_start(out=s.blkiota[:], in_=cst['blkiota']), writes=['blkiota'])
        ph.op('sp', lambda e: e.dma_start(out=s.pidx[:], in_=cst['pidx']), writes=['pidx'])
        zrow = sb(s, "zrow", [128, D], BF16, st0)
        ph.op('dve', lambda e: e.memset(zrow[:], 0.0), writes=['zrow'])
        for r0 in range(0, s.NBLK * MB, 128):
            ph.op('sp', lambda e, r0=r0: e.dma_start(out=s.XP[r0:r0 + 128, :], in_=zrow[:]), reads=['zrow'], writes=[('XP', r0)])
        ph.op('dve', lambda e: e.memset(s.ones_bf[:], 1.0), writes=['ones_bf'])
        ph.op('dve', lambda e: e.memset(s.ones_f[:], 1.0), writes=['ones_f'])
        ph.op('pq', lambda e: e.dma_start(out=s.wr_bf[:], in_=s.w_router.rearrange("(k p) n -> p k n", p=128)), writes=['wr'])
        ph.op('sp', lambda e: e.dma_start(out=s.br_bc[:], in_=s.b_router.partition_broadcast(128)), writes=['br'])
        cT = sb(s, "cT", [128, KC, NBC], F32, st0)
        scT = sb(s, "scT", [128, KC, NBC], BF16, st0)
        for b in range(NB):
            ph.op('sp', lambda e, b=b: e.dma_start(out=cT[:, :, b], in_=s.c_in[b].rearrange("(k p) -> p k", p=128),
                                                    allow_slow_non_contiguous=True), writes=['cT'])
        ph.op('sp', lambda e: e.dma_start(out=cT[:, :, NB], in_=s.cctx_in.rearrange("(k p) -> p k", p=128),
                                          allow_slow_non_contiguous=True), writes=['cT'])
        ph.op('act', lambda e: e.activation(out=scT[:], in_=cT[:], func=AF.Silu), reads=['cT'], writes=['scT'])
        adab = sb(s, "adab", [128, DEPTH, 48], F32, st0)
        n1T = sb(s, "n1T", [128, DEPTH, KC], F32, st0)
        n2T = sb(s, "n2T", [128, DEPTH, KC], F32, st0)
        for l in range(DEPTH):
            for (dst, src) in ((adab, s.ada_b), (n1T, s.norm1), (n2T, s.norm2)):
                ph.op('sp', lambda e, l=l, dst=dst, src=src: e.dma_start(
                    out=dst[:, l, :], in_=src[l].rearrange("(c p) -> p c", p=128), allow_slow_non_contiguous=True),
                    writes=['smallT'])
        awr = Rot([sb(s, f"aw{i}", [128, KC, 1536], BF16, st0) for i in range(2)], "aw")
        for l in range(DEPTH):
            for qt in range(4):
                aw, awk = awr.next()
                ph.op('pq', lambda e, aw=aw, l=l, qt=qt: e.dma_start(
                    out=aw[:], in_=s.ada_w[l].rearrange("(k p) n -> p k n", p=128)[:, :, qt * 1536:(qt + 1) * 1536]),
                    writes=[awk])
                for f in range(12):
                    fc = qt * 12 + f
                    ps = s.PS[fc % 4]
                    for k in range(KC):
                        ph.op('pe', lambda e, ps=ps, aw=aw, f=f, k=k: e.matmul(
                            ps[:, 0:NBC], aw[:, k, f * 128:(f + 1) * 128], scT[:, k, :], start=(k == 0), stop=(k == KC - 1)),
                            reads=[awk, 'scT'], writes=[('ps', fc % 4)])
                    ph.op('dve', lambda e, ps=ps, l=l, fc=fc: e.tensor_scalar(
                        out=s.modT[:, l, fc, :], in0=ps[:, 0:NBC], scalar1=adab[:, l, fc:fc + 1], scalar2=None, op0=ALU.add),
                        reads=[('ps', fc % 4), 'smallT'], writes=['modT'])
            for (gs, nT, base) in ((s.gs1, n1T, 8), (s.gs2, n2T, 32)):
                for b in range(NBC):
                    ph.op('dve', lambda e, gs=gs, nT=nT, base=base, b=b, l=l: e.scalar_tensor_tensor(
                        out=gs[:, l, :, b], in0=s.modT[:, l, base:base + 8, b], scalar=1.0, in1=nT[:, l, :],
                        op0=ALU.add, op1=ALU.mult), reads=['modT', 'smallT'], writes=['gs'])
        done(s, ph)


def phase_input(s):
    NB, NT = s.NB, s.NT
    with contextlib.ExitStack() as st1:
        ph = new_phase(s, "i")
        xtr = Rot([sb(s, f"xin{i}", [128, D], F32, st1) for i in range(3)], "xin")
        str_ = Rot([sb(s, f"xstg{i}", [128, KC, 128], F32, st1) for i in range(3)], "xstg")
        for b in range(NB):
            for i in range(NT):
                t0 = i * 128
                src = s.ctx_in[b, t0:t0 + 128, :] if t0 < CTX else s.x_in[b, t0 - CTX:t0 - CTX + 128, :]
                xt, xk = xtr.next()
                ph.op('sp', lambda e, xt=xt, src=src: e.dma_start(out=xt[:], in_=src), writes=[xk])
                stg, sk = str_.next()
                for half in range(2):
                    ps = s.PS[half]
                    for j in range(4):
                        c = half * 4 + j
                        ph.op('pe', lambda e, ps=ps, j=j, c=c, xt=xt: e.transpose(
                            ps[:, j * 128:(j + 1) * 128], xt[:, c * 128:(c + 1) * 128], s.ident[:]),
                            reads=[xk, 'ident'], writes=[('ps', half)])
                    if half == 0:
                        ph.op('act', lambda e, ps=ps, stg=stg: e.activation(
                            out=stg[:, 0:4, :], in_=ps[:].rearrange("p (j t) -> p j t", j=4), func=AF.Copy),
                            reads=[('ps', half)], writes=[(sk, half)])
                    else:
                        ph.op('dve', lambda e, ps=ps, stg=stg: e.tensor_copy(
                            out=stg[:, 4:8, :], in_=ps[:].rearrange("p (j t) -> p j t", j=4)),
                            reads=[('ps', half)], writes=[(sk, half)])
                ph.op('sp', lambda e, stg=stg, b=b, t0=t0: e.dma_start(
                    out=s.XT[b, :, :, t0:t0 + 128].rearrange("k p t -> p k t"), in_=stg[:]),
                    reads=[(sk, 0), (sk, 1)], writes=[('XT', b, t0)])
        done(s, ph)


def norm_mod(s, ph, xs, xk, n, gs, sh, l, bcol, hT, hk, sqr, rstd, tmpr):
    psS = s.PS[6]
    for c in range(KC):
        sq, sqk = sqr.next()
        ph.op('act', lambda e, c=c, sq=sq: e.activation(out=sq[:, :n], in_=xs[:, c, :n], func=AF.Square),
              reads=[xk], writes=[sqk])
        ph.op('pe', lambda e, c=c, sq=sq: e.matmul(psS[:, :n], s.ones_bf[:], sq[:, :n], start=(c == 0), stop=(c == KC - 1)),
              reads=[sqk, 'ones_bf'], writes=[('ps', 6)])
    ph.op('act', lambda e: e.activation(out=rstd[:, :n], in_=psS[:, :n], func=AF.Sqrt, bias=EPS, scale=1.0 / D),
          reads=[('ps', 6)], writes=['rstd'])
    ph.op('dve', lambda e: e.reciprocal(out=rstd[:, :n], in_=rstd[:, :n]), reads=['rstd'], writes=['rstd'])
    for c in range(KC):
        tmp, tk = tmpr.next()
        ph.op('dve', lambda e, c=c, tmp=tmp: e.scalar_tensor_tensor(
            out=tmp[:, :n], in0=xs[:, c, :n], scalar=gs[:, l, c, bcol:bcol + 1], in1=rstd[:, :n],
            op0=ALU.mult, op1=ALU.mult), reads=[xk, 'rstd'], writes=[tk])
        ph.op('act', lambda e, c=c, tmp=tmp: e.activation(
            out=hT[:, c, :n], in_=tmp[:, :n], func=AF.Identity, bias=s.modT[:, l, sh + c, bcol:bcol + 1], scale=1.0),
            reads=[tk], writes=[hk])


def phase_tables(s, l, stL):
    cst = s.cst
    s.gcol = gcol = sb(s, "gcol", [128, 4], F32, stL)
    s.esink = esink = sb(s, "esink", [128, 4], F32, stL)
    lg = sb(s, "lg", [128, 8], F32, stL)
    lgp = sb(s, "lgp", [128, 4], F32, stL)
    s.dsum = dsum = sb(s, "dsum", [128, 4, 128], F32, stL)
    s.xit = xit = sb(s, "xit", [128, 4, 128], F32, stL)
    s.zt = zt = sb(s, "zt", [128, 2, 256], F32, stL)
    s.gcp = gcp = sb(s, "gcp", [128, 4], F32, stL)
    iot = sb(s, "iot", [128, 512], F32, stL)
    dcf = sb(s, "dcf", [128, 4, 128], F32, stL)
    z4 = sb(s, "z4", [128, 8], F32, stL)
    tmpd = sb(s, "tmpd", [128, 128], F32, stL)
    ph = new_phase(s, "t")
    ph.op('dve', lambda e: e.memset(s.cbase[:], 0.0), writes=['cbase'])
    for (i, (src, sc)) in enumerate(((s.win_gain[l, 0], 0.125), (s.win_gain[l, 1], 1.0),
                                     (s.glb_gain[l, 0], 0.125), (s.glb_gain[l, 1], 1.0))):
        for hf in range(2):
            ph.op('sp', lambda e, i=i, src=src, hf=hf: e.dma_start(
                out=gcol[hf * 64:(hf + 1) * 64, i:i + 1], in_=src.rearrange("(d o) -> d o", o=1)), writes=['gcol'])
        if sc != 1.0:
            ph.op('dve', lambda e, i=i, sc=sc: e.tensor_scalar(
                out=gcol[:, i:i + 1], in0=gcol[:, i:i + 1], scalar1=sc, scalar2=None, op0=ALU.mult),
                reads=['gcol'], writes=['gcol'])
    ph.op('sp', lambda e: e.dma_start(out=esink[:], in_=s.win_sink[l].partition_broadcast(128)), writes=['esink'])
    ph.op('act', lambda e: e.activation(out=esink[:], in_=esink[:], func=AF.Exp), reads=['esink'], writes=['esink'])
    ph.op('sp', lambda e: e.dma_start(out=lg[:], in_=s.ret_decay[l].partition_broadcast(128)), writes=['lg'])
    ph.op('sp', lambda e: e.dma_start(out=iot[:], in_=cst['iot']), writes=['iot'])
    for i, nm in enumerate(('dif_f', 'msk_f', 'dif_b', 'msk_b')):
        ph.op('sp', lambda e, i=i, nm=nm: e.dma_start(out=dcf[:, i, :], in_=cst[nm]), writes=['dcf'])
    ph.op('act', lambda e: e.activation(out=lg[:], in_=lg[:], func=AF.Exp, scale=-1.0), reads=['lg'], writes=['lg'])
    ph.op('act', lambda e: e.activation(out=lg[:], in_=lg[:], func=AF.Ln, bias=1.0, scale=1.0), reads=['lg'], writes=['lg'])
    ph.op('dve', lambda e: e.tensor_scalar(out=lg[:], in0=lg[:], scalar1=-1.0, scalar2=None, op0=ALU.mult),
          reads=['lg'], writes=['lg'])
    for dr in range(2):
        for g in range(2):
            for hf in range(2):
                ph.op('dve', lambda e, dr=dr, g=g, hf=hf: e.tensor_copy(
                    out=lgp[hf * 64:(hf + 1) * 64, dr * 2 + g:dr * 2 + g + 1],
                    in_=lg[hf * 64:(hf + 1) * 64, dr * 4 + 2 * g + hf:dr * 4 + 2 * g + hf + 1]),
                    reads=['lg'], writes=['lgp'])
    for h in range(4):
        ph.op('act', lambda e, h=h: e.activation(out=dsum[:, h, :], in_=dcf[:, 0, :], func=AF.Exp, scale=lg[:, h:h + 1]),
              reads=['lg', 'dcf'], writes=[('dsum', h)])
        ph.op('dve', lambda e, h=h: e.tensor_tensor(out=dsum[:, h, :], in0=dsum[:, h, :], in1=dcf[:, 1, :], op=ALU.mult),
              reads=[('dsum', h)], writes=[('dsum', h)])
        ph.op('act', lambda e, h=h: e.activation(out=tmpd[:], in_=dcf[:, 2, :], func=AF.Exp, scale=lg[:, 4 + h:5 + h]),
              reads=['lg', 'dcf'], writes=['tmpd'])
        ph.op('dve', lambda e, h=h: e.tensor_tensor(out=tmpd[:], in0=tmpd[:], in1=dcf[:, 3, :], op=ALU.mult),
              reads=['tmpd'], writes=['tmpd'])
        ph.op('dve', lambda e, h=h: e.tensor_tensor(out=dsum[:, h, :], in0=dsum[:, h, :], in1=tmpd[:], op=ALU.add),
              reads=[('dsum', h), 'tmpd'], writes=[('dsum', h)])
    for dr in range(2):
        for g in range(2):
            i = dr * 2 + g
            ph.op('act', lambda e, i=i, dr=dr: e.activation(
                out=xit[:, i, :], in_=iot[:, dr * 128:(dr + 1) * 128], func=AF.Exp, scale=lgp[:, i:i + 1]),
                reads=['lgp', 'iot'], writes=['xit'])
        for h in range(4):
            ph.op('act', lambda e, dr=dr, h=h: e.activation(
                out=z4[:, dr * 4 + h:dr * 4 + h + 1], in_=iot[:, 256 + dr * 128:257 + dr * 128], func=AF.Exp,
                scale=lg[:, dr * 4 + h:dr * 4 + h + 1]), reads=['lg', 'iot'], writes=['z4'])
            ph.op('dve', lambda e, dr=dr, h=h: e.tensor_scalar(
                out=zt[:, dr, h * 64:(h + 1) * 64], in0=s.ones_f[:, 0:64], scalar1=z4[:, dr * 4 + h:dr * 4 + h + 1],
                scalar2=0.125, op0=ALU.mult, op1=ALU.mult), reads=['z4', 'ones_f'], writes=['zt'])
    ph.op('act', lambda e: e.activation(out=gcp[:], in_=lgp[:], func=AF.Exp, scale=128.0), reads=['lgp'], writes=['gcp'])
    done(s, ph)


FMB = [(0, 0, 256), (256, 256, 256), (512, 1536, 256), (768, 1792, 64), (832, 1792, 64), (896, 1856, 64), (960, 1856, 64),
       (1024, 2048, 256), (1280, 2304, 64), (1344, 2304, 64), (1408, 2368, 64), (1472, 2368, 64)]
TMB = [(0, 256, 256), (256, 1920, 128), (384, 2432, 128), (512, 512, 512), (1024, 1024, 512)]


def phase_proj(s, l, b):
    T, L, NB = s.T, s.L, s.NB
    PS = s.PS
    with contextlib.ExitStack() as st:
        wfm = sb(s, "wfm", [128, KC, 1536], BF16, st)
        wtm = sb(s, "wtm", [128, KC, 1536], BF16, st)
        cosT = sb(s, "cosT", [128, L], F32, st)
        sinT = sb(s, "sinT", [128, L], F32, st)
        xsr = Rot([sb(s, f"pxs{i}", [128, KC, 512], F32, st) for i in range(1)], "pxs")
        hTr = Rot([sb(s, f"phT{i}", [128, KC, 512], BF16, st) for i in range(1)], "phT")
        sqr = Rot([sb(s, f"psq{i}", [128, 512], BF16, st) for i in range(2)], "psq")
        rstd = sb(s, "prstd", [128, 512], F32, st)
        tmpr = Rot([sb(s, f"ptmp{i}", [128, 512], F32, st) for i in range(2)], "ptmp")
        fmr = Rot([sb(s, f"pfm{i}", [128, 12, 512], BF16, st) for i in range(1)], "pfm")
        tokr = Rot([sb(s, f"ptok{i}", [128, 1536], BF16, st) for i in range(1)], "ptok")
        rr = Rot([sb(s, f"pr{i}", [128, 512], F32, st) for i in range(2)], "pr")
        qnr = Rot([sb(s, f"pqn{i}", [128, 512], BF16, st) for i in range(2)], "pqn")
        t1r = Rot([sb(s, f"pt1{i}", [128, 512], F32, st) for i in range(1)], "pt1")
        t2r = Rot([sb(s, f"pt2{i}", [128, 512], F32, st) for i in range(1)], "pt2")
        psr = Rot(PS[0:4], "ps")
        pxr = Rot(PS[4:6], "px")
        ph = new_phase(s, "a")
        wv = s.w_in[l].rearrange("(k p) n -> p k n", p=128)
        for (d0, s0, w) in FMB:
            ph.op('pq', lambda e, d0=d0, s0=s0, w=w: e.dma_start(out=wfm[:, :, d0:d0 + w], in_=wv[:, :, s0:s0 + w]), writes=['wfm'])
        for (d0, s0, w) in TMB:
            ph.op('pq', lambda e, d0=d0, s0=s0, w=w: e.dma_start(out=wtm[:, :, d0:d0 + w], in_=wv[:, :, s0:s0 + w]), writes=['wtm'])
        ph.op('sp', lambda e: e.dma_start(out=cosT[:], in_=s.cst['cosT']), writes=['cos'])
        ph.op('sp', lambda e: e.dma_start(out=sinT[:], in_=s.cst['sinT']), writes=['sin'])
        for (t0, n) in s.chunks:
            isx = t0 >= CTX
            bcol = b if isx else NB
            x0 = t0 - CTX
            xs, xk = xsr.next()
            ph.op('sp', lambda e, xs=xs, t0=t0, n=n: e.dma_start(
                out=xs[:, :, :n], in_=s.XT[b, :, :, t0:t0 + n].rearrange("k p t -> p k t")), writes=[xk])
            hT, hk = hTr.next()
            norm_mod(s, ph, xs, xk, n, s.gs1, 0, l, bcol, hT, hk, sqr, rstd, tmpr)
            fm, fk = fmr.next()
            for j in range(12):
                ps, pk = psr.next()
                for k in range(KC):
                    ph.op('pe', lambda e, ps=ps, j=j, k=k, hT=hT: e.matmul(
                        ps[:, :n], wfm[:, k, j * 128:(j + 1) * 128], hT[:, k, :n], start=(k == 0), stop=(k == KC - 1)),
                        reads=['wfm', hk], writes=[pk])
                if j < 2:
                    ph.op('act', lambda e, ps=ps, j=j, fm=fm: e.activation(out=fm[:, j, :n], in_=ps[:, :n], func=AF.Copy),
                          reads=[pk], writes=[(fk, j)])
                elif j < 4:
                    ph.op('act', lambda e, ps=ps, j=j, fm=fm: e.activation(out=fm[:, j, :n], in_=ps[:, :n], func=AF.Copy, scale=0.125),
                          reads=[pk], writes=[(fk, j)])
                else:
                    kind = (j - 4) // 2
                    sq, sqk = sqr.next()
                    ph.op('act', lambda e, ps=ps, sq=sq: e.activation(out=sq[:, :n], in_=ps[:, :n], func=AF.Square),
                          reads=[pk], writes=[sqk])
                    px, pxk = pxr.next()
                    ph.op('pe', lambda e, px=px, sq=sq: e.matmul(px[:, :n], s.blk64[:], sq[:, :n], start=True, stop=True),
                          reads=[sqk, 'blk64'], writes=[pxk])
                    r, rk = rr.next()
                    ph.op('act', lambda e, px=px, r=r: e.activation(out=r[:, :n], in_=px[:, :n], func=AF.Sqrt, bias=EPS, scale=1.0 / 64),
                          reads=[pxk], writes=[rk])
                    ph.op('dve', lambda e, r=r: e.reciprocal(out=r[:, :n], in_=r[:, :n]), reads=[rk], writes=[rk])
                    if not isx:
                        ph.op('dve', lambda e, ps=ps, r=r, j=j, fm=fm, kind=kind: e.scalar_tensor_tensor(
                            out=fm[:, j, :n], in0=ps[:, :n], scalar=s.gcol[:, kind:kind + 1], in1=r[:, :n],
                            op0=ALU.mult, op1=ALU.mult), reads=[pk, rk, 'gcol'], writes=[(fk, j)])
                    else:
                        qn, qk = qnr.next()
                        ph.op('dve', lambda e, ps=ps, r=r, qn=qn, kind=kind: e.scalar_tensor_tensor(
                            out=qn[:, :n], in0=ps[:, :n], scalar=s.gcol[:, kind:kind + 1], in1=r[:, :n],
                            op0=ALU.mult, op1=ALU.mult), reads=[pk, rk, 'gcol'], writes=[qk])
                        px2, px2k = pxr.next()
                        ph.op('pe', lambda e, px2=px2, qn=qn: e.matmul(px2[:, :n], s.prot[:], qn[:, :n], start=True, stop=True),
                              reads=[qk, 'prot'], writes=[px2k])
                        t1, t1k = t1r.next()
                        t2, t2k = t2r.next()
                        ph.op('pool', lambda e, t1=t1, qn=qn: e.tensor_tensor(
                            out=t1[:, :n], in0=qn[:, :n], in1=cosT[:, x0:x0 + n], op=ALU.mult), reads=[qk, 'cos'], writes=[t1k])
                        ph.op('dve', lambda e, t2=t2, px2=px2: e.tensor_tensor(
                            out=t2[:, :n], in0=px2[:, :n], in1=sinT[:, x0:x0 + n], op=ALU.mult), reads=[px2k, 'sin'], writes=[t2k])
                        ph.op('pool', lambda e, t1=t1, t2=t2, fm=fm, j=j: e.tensor_tensor(
                            out=fm[:, j, :n], in0=t1[:, :n], in1=t2[:, :n], op=ALU.add), reads=[t1k, t2k], writes=[(fk, j)])
            ph.op('sp', lambda e, fm=fm, t0=t0, n=n: e.dma_start(
                out=s.FM[:, :, t0:t0 + n].rearrange("j p t -> p j t"), in_=fm[:, :, :n]),
                reads=[(fk, j) for j in range(12)], writes=[('FM', t0)])
            for i in range(n // 128):
                tok, tkk = tokr.next()
                for g in range(3):
                    ps, pk = psr.next()
                    for k in range(KC):
                        ph.op('pe', lambda e, ps=ps, g=g, k=k, i=i, hT=hT: e.matmul(
                            ps[:], hT[:, k, i * 128:(i + 1) * 128], wtm[:, k, g * 512:(g + 1) * 512], start=(k == 0), stop=(k == KC - 1)),
                            reads=['wtm', hk], writes=[pk])
                    if g == 0:
                        ph.op('dve', lambda e, ps=ps, tok=tok: e.tensor_copy(out=tok[:, 0:512], in_=ps[:]),
                              reads=[pk], writes=[(tkk, 0)])
                    elif g == 1:
                        ph.op('act', lambda e, ps=ps, tok=tok: e.activation(out=tok[:, 512:1024], in_=ps[:], func=AF.Copy),
                              reads=[pk], writes=[(tkk, 1)])
                    else:
                        ph.op('act', lambda e, ps=ps, tok=tok: e.activation(out=tok[:, 1024:1536], in_=ps[:], func=AF.Silu),
                              reads=[pk], writes=[(tkk, 2)])
                ph.op('sp', lambda e, tok=tok, t0=t0, i=i: e.dma_start(out=s.TOK[t0 + i * 128:t0 + (i + 1) * 128, :], in_=tok[:]),
                      reads=[(tkk, 0), (tkk, 1), (tkk, 2)], writes=[('TOK', t0, i)])
        done(s, ph)


def phase_ret(s, l, b):
    T, NT = s.T, s.NT
    PS = s.PS
    with contextlib.ExitStack() as st:
        qt = sb(s, "rq", [128, 2, T], BF16, st)
        kt = sb(s, "rk", [128, 2, T], BF16, st)
        ktok = sb(s, "rktok", [128, NT, 256], BF16, st)
        v = sb(s, "rv", [128, NT, 512], BF16, st)
        sat = sb(s, "rsat", [128, 4, NT, 128], BF16, st)
        srun = sb(s, "rsrun", [128, 4, 128], F32, st)
        kzr = Rot([sb(s, f"rkz{i}", [128, 128], BF16, st) for i in range(3)], "rkz")
        attr = Rot([sb(s, f"ratt{i}", [128, 128], BF16, st) for i in range(3)], "ratt")
        qxr = Rot([sb(s, f"rqx{i}", [128, 2, 128], BF16, st) for i in range(2)], "rqx")
        gater = Rot([sb(s, f"rgate{i}", [128, 512], BF16, st) for i in range(2)], "rgate")
        ybr = Rot([sb(s, f"rybf{i}", [128, 512], BF16, st) for i in range(2)], "rybf")
        ytr = Rot([sb(s, f"ryt{i}", [128, 4, 128], BF16, st) for i in range(2)], "ryt")
        tmpr = Rot([sb(s, f"rtmp{i}", [128, 512], F32, st) for i in range(2)], "rtmp")
        junk = sb(s, "rjunk", [128, 128], F32, st)
        statr = Rot([sb(s, f"rstat{i}", [128, 12], F32, st) for i in range(2)], "rstat")
        ph = new_phase(s, "r")
        ph.op('sp', lambda e: e.dma_start(out=qt[:], in_=s.FM[0:2, :, :].rearrange("j p t -> p j t")), writes=['qt'])
        ph.op('sp', lambda e: e.dma_start(out=kt[:], in_=s.FM[2:4, :, :].rearrange("j p t -> p j t")), writes=['kt'])
        ph.op('sp', lambda e: e.dma_start(out=ktok[:], in_=s.TOK[:, 0:256].rearrange("(i p) c -> p i c", p=128)), writes=['ktok'])
        ph.op('sp', lambda e: e.dma_start(out=v[:], in_=s.TOK[:, 512:1024].rearrange("(i p) c -> p i c", p=128)), writes=['v'])
        ph.op('dve', lambda e: e.memset(srun[:], 0.0), writes=[('srun', i) for i in range(4)])
        psur = Rot(PS[0:2], "psu")
        orders = [list(range(NT)), [1, 0] + list(range(NT - 1, 1, -1))]
        for dr in range(2):
            for i in orders[dr]:
                for g in range(2):
                    idx = dr * 2 + g
                    kz, kzk = kzr.next()
                    ph.op('pool', lambda e, kz=kz, i=i, g=g, dr=dr: e.tensor_tensor(
                        out=kz[:], in0=ktok[:, i, g * 128:(g + 1) * 128], in1=s.zt[:, dr, g * 128:(g + 1) * 128], op=ALU.mult),
                        reads=['ktok', 'zt'], writes=[kzk])
                    pu, puk = psur.next()
                    for hh in range(2):
                        h = 2 * g + hh
                        ph.op('pe', lambda e, pu=pu, kz=kz, hh=hh, h=h, i=i: e.matmul(
                            pu[:, hh * 128:(hh + 1) * 128], kz[:], v[:, i, h * 128:(h + 1) * 128], start=True, stop=True),
                            reads=[kzk, 'v'], writes=[puk])
                    ph.op('act', lambda e, idx=idx, i=i: e.activation(out=sat[:, idx, i, :], in_=srun[:, idx, :], func=AF.Copy),
                          reads=[('srun', idx)], writes=[('sat', idx, i)])
                    for hh in range(2):
                        ph.op('dve', lambda e, pu=pu, hh=hh, idx=idx: e.scalar_tensor_tensor(
                            out=srun[hh * 64:(hh + 1) * 64, idx, :], in0=srun[hh * 64:(hh + 1) * 64, idx, :],
                            scalar=s.gcp[hh * 64:(hh + 1) * 64, idx:idx + 1],
                            in1=pu[hh * 64:(hh + 1) * 64, hh * 128:(hh + 1) * 128], op0=ALU.mult, op1=ALU.add),
                            reads=[puk, ('srun', idx), 'gcp'], writes=[('srun', idx)])
        pssr = Rot(PS[2:4], "pss")
        psor = Rot(PS[4:6], "pso")
        for i in range(NT):
            tc = slice(i * 128, (i + 1) * 128)
            po, pok = psor.next()
            for g in range(2):
                qx, qxk = qxr.next()
                for dr in range(2):
                    ph.op('pool', lambda e, qx=qx, g=g, dr=dr, tc=tc: e.tensor_tensor(
                        out=qx[:, dr, :], in0=qt[:, g, tc], in1=s.xit[:, dr * 2 + g, :], op=ALU.mult),
                        reads=['qt', 'xit'], writes=[qxk])
                for hh in range(2):
                    h = 2 * g + hh
                    rows = slice(hh * 64, (hh + 1) * 64)
                    pss, psk = pssr.next()
                    ph.op('pe', lambda e, pss=pss, rows=rows, g=g, tc=tc: e.matmul(
                        pss[:, 0:128], kt[rows, g, tc], qt[rows, g, tc], start=True, stop=True),
                        reads=['kt', 'qt'], writes=[psk])
                    att, atk = attr.next()
                    ph.op('dve', lambda e, pss=pss, att=att, h=h: e.tensor_tensor(
                        out=att[:], in0=pss[:, 0:128], in1=s.dsum[:, h, :], op=ALU.mult), reads=[psk, ('dsum', h)], writes=[atk])
                    oc = slice(h * 128, (h + 1) * 128)
                    ph.op('pe', lambda e, po=po, att=att, oc=oc, i=i: e.matmul(
                        po[:, oc], att[:], v[:, i, oc], start=True, stop=False), reads=[atk, 'v'], writes=[(pok, h)])
                    ph.op('pe', lambda e, po=po, qx=qx, rows=rows, oc=oc, g=g, i=i: e.matmul(
                        po[:, oc], qx[rows, 0, :], sat[rows, g, i, :], start=False, stop=False),
                        reads=[qxk, ('sat', g, i)], writes=[(pok, h)])
                    ph.op('pe', lambda e, po=po, qx=qx, rows=rows, oc=oc, g=g, i=i: e.matmul(
                        po[:, oc], qx[rows, 1, :], sat[rows, 2 + g, i, :], start=False, stop=True),
                        reads=[qxk, ('sat', 2 + g, i)], writes=[(pok, h)])
            pokeys = [(pok, h) for h in range(4)]
            stt_, stk = statr.next()
            ph.op('dve', lambda e, stt_=stt_: e.memset(stt_[:], 0.0), writes=[stk])
            ph.op('dve', lambda e, po=po, stt_=stt_: e.tensor_reduce(
                out=stt_[:, 0:4], in_=po[:].rearrange("p (h d) -> p h d", h=4), axis=AX.X, op=ALU.add),
                reads=pokeys + [stk], writes=[stk])
            ph.op('dve', lambda e, stt_=stt_: e.tensor_scalar(
                out=stt_[:, 0:4], in0=stt_[:, 0:4], scalar1=-1.0 / 128, scalar2=None, op0=ALU.mult), reads=[stk], writes=[stk])
            for h in range(4):
                ph.op('act', lambda e, po=po, stt_=stt_, h=h: e.activation(
                    out=junk[:], in_=po[:, h * 128:(h + 1) * 128], func=AF.Square, bias=stt_[:, h:h + 1], scale=1.0,
                    accum_out=stt_[:, 4 + h:5 + h]), reads=pokeys + [stk], writes=[stk, 'junk'])
            ph.op('act', lambda e, stt_=stt_: e.activation(
                out=stt_[:, 8:12], in_=stt_[:, 4:8], func=AF.Sqrt, bias=EPS, scale=1.0 / 128), reads=[stk], writes=[stk])
            ph.op('dve', lambda e, stt_=stt_: e.reciprocal(out=stt_[:, 8:12], in_=stt_[:, 8:12]), reads=[stk], writes=[stk])
            tmp, tmk = tmpr.next()
            for h in range(4):
                ph.op('dve', lambda e, po=po, stt_=stt_, tmp=tmp, h=h: e.tensor_scalar(
                    out=tmp[:, h * 128:(h + 1) * 128], in0=po[:, h * 128:(h + 1) * 128], scalar1=stt_[:, h:h + 1],
                    scalar2=stt_[:, 8 + h:9 + h], op0=ALU.add, op1=ALU.mult), reads=pokeys + [stk], writes=[tmk])
            gate, gk = gater.next()
            ph.op('sp', lambda e, gate=gate, tc=tc: e.dma_start(out=gate[:], in_=s.TOK[tc, 1024:1536]), writes=[gk])
            yb, ybk = ybr.next()
            ph.op('pool', lambda e, yb=yb, tmp=tmp, gate=gate: e.tensor_tensor(out=yb[:], in0=tmp[:], in1=gate[:], op=ALU.mult),
                  reads=[tmk, gk], writes=[ybk])
            for j in range(4):
                ph.op('pe', lambda e, yb=yb, j=j: e.transpose(
                    s.PSB[:, j * 128:(j + 1) * 128], yb[:, j * 128:(j + 1) * 128], s.ident_bf[:]),
                    reads=[ybk, 'ident_bf'], writes=['psb'])
            yt_, ytk = ytr.next()
            ph.op('act', lambda e, yt_=yt_: e.activation(
                out=yt_[:], in_=s.PSB[:, 0:512].rearrange("p (j t) -> p j t", j=4), func=AF.Copy), reads=['psb'], writes=[ytk])
            ph.op('sp', lambda e, yt_=yt_, tc=tc: e.dma_start(
                out=s.YT[0:4, :, tc].rearrange("j p t -> p j t"), in_=yt_[:]), reads=[ytk], writes=[('YT', i)])
        done(s, ph)


def phase_attn(s, l, b):
    T, NT = s.T, s.NT
    PS = s.PS
    for kind in (0, 1):
        with contextlib.ExitStack() as st:
            qc, kc, vcol, yrow = 4 + kind * 4, 6 + kind * 4, 256 + kind * 128, 4 + kind * 2
            q2 = sb(s, "aq2", [128, 2, T], BF16, st)
            k2 = sb(s, "ak2", [128, 2, T], BF16, st)
            vaug = sb(s, "avaug", [128, NT, 2, 65], BF16, st)
            wm = sb(s, "awm", [128, 6, 512], BF16, st)
            pr = Rot([sb(s, f"ap{i}", [128, 512], BF16, st) for i in range(3)], "ap")
            rden = sb(s, "arden", [128, 512], F32, st)
            osbr = Rot([sb(s, f"aosb{i}", [128, 512], F32, st) for i in range(2)], "aosb")
            ystr = Rot([sb(s, f"ayst{i}", [128, 512], BF16, st) for i in range(2)], "ayst")
            ph = new_phase(s, "w" if kind == 0 else "g")
            ph.op('sp', lambda e: e.dma_start(out=q2[:], in_=s.FM[qc:qc + 2, :, :].rearrange("j p t -> p j t")), writes=['q2'])
            ph.op('sp', lambda e: e.dma_start(out=k2[:], in_=s.FM[kc:kc + 2, :, :].rearrange("j p t -> p j t")), writes=['k2'])
            ph.op('dve', lambda e: e.memset(vaug[:], 1.0), writes=['vaug'])
            for gg in range(2):
                ph.op('sp', lambda e, gg=gg: e.dma_start(
                    out=vaug[:, :, gg, 0:64],
                    in_=s.TOK[:, vcol + gg * 64:vcol + (gg + 1) * 64].rearrange("(i p) d -> p i d", p=128)),
                    writes=['vaug'])
            if kind == 0:
                ph.op('pq', lambda e: e.dma_start(out=wm[:], in_=s.cst['wmask'].rearrange("o p q -> p o q")), writes=['wm'])
            else:
                if b == 0:
                    for ex in range(NE):
                        ph.op('pq', lambda e, ex=ex: e.dma_start(
                            out=s.WBgu[ex], in_=s.w_gu[l, ex].rearrange("(k p) n -> p k n", p=128)), writes=[('wbgu', ex)])
                if b == s.NB - 1:
                    for ex in range(NE):
                        ph.op('pq', lambda e, ex=ex: e.dma_start(
                            out=s.WBdn[ex], in_=s.w_dn[l, ex].rearrange("(k p) n -> p k n", p=128)), writes=[('wbdn', ex)])
            pssr = Rot(PS[0:4], "pss")
            psor = Rot(PS[4:6], "pso")
            for (t0, n) in s.chunks:
                isx = t0 >= CTX
                qt0 = t0 // 128
                if not isx:
                    keytiles = [0, 1]
                elif kind == 1:
                    keytiles = list(range(NT))
                else:
                    keytiles = [0, 1] + [j for j in range(qt0 - 1, qt0 + 5) if 2 <= j < NT]
                for h in range(4):
                    g = h // 2
                    rows = slice((h % 2) * 64, (h % 2) * 64 + 64)
                    po, pok = psor.next()
                    for idx, j in enumerate(keytiles):
                        pss, psk = pssr.next()
                        masked = (kind == 0) and isx and j >= 2
                        kcs = slice(j * 128, (j + 1) * 128)
                        ph.op('pe', lambda e, pss=pss, rows=rows, g=g, kcs=kcs, masked=masked: e.matmul(
                            pss[:, :n], k2[rows, g, kcs], q2[rows, g, t0:t0 + n], start=True, stop=(not masked)),
                            reads=['k2', 'q2'], writes=[psk])
                        if masked:
                            o = j - qt0 + 1
                            ph.op('pe', lambda e, pss=pss, o=o: e.matmul(
                                pss[:, :n], s.ident_bf[:], wm[:, o, :n], start=False, stop=True),
                                reads=['wm', 'ident_bf'], writes=[psk])
                        p_, pk_ = pr.next()
                        ph.op('act', lambda e, pss=pss, p_=p_: e.activation(out=p_[:, :n], in_=pss[:, :n], func=AF.Exp),
                              reads=[psk], writes=[pk_])
                        ph.op('pe', lambda e, po=po, p_=p_, j=j, g=g, idx=idx: e.matmul(
                            po[0:65, :n], vaug[:, j, g, :], p_[:, :n], start=(idx == 0), stop=(idx == len(keytiles) - 1)),
                            reads=['vaug', pk_], writes=[pok])
                    add = s.esink[64:65, h:h + 1] if kind == 0 else 0.0
                    ph.op('dve', lambda e, po=po, add=add: e.tensor_scalar(
                        out=rden[64:65, :n], in0=po[64:65, :n], scalar1=add, scalar2=None, op0=ALU.add),
                        reads=[pok, 'esink'], writes=['rden'])
                    ph.op('dve', lambda e: e.reciprocal(out=rden[64:65, :n], in_=rden[64:65, :n]), reads=['rden'], writes=['rden'])
                    ph.op('pe', lambda e: e.matmul(PS[6][0:64, :n], s.ones_f[64:65, 0:64], rden[64:65, :n], start=True, stop=True),
                          reads=['rden', 'ones_f'], writes=[('ps', 6)])
                    osb, osk = osbr.next()
                    ph.op('act', lambda e, po=po, osb=osb: e.activation(out=osb[0:64, :n], in_=po[0:64, :n], func=AF.Copy),
                          reads=[pok], writes=[osk])
                    yst, ysk = ystr.next()
                    ph.op('dve', lambda e, osb=osb, yst=yst: e.tensor_tensor(
                        out=yst[0:64, :n], in0=osb[0:64, :n], in1=PS[6][0:64, :n], op=ALU.mult),
                        reads=[osk, ('ps', 6)], writes=[ysk])
                    ph.op('sp', lambda e, yst=yst, g=g, rows=rows: e.dma_start(
                        out=s.YT[yrow + g, rows, t0:t0 + n], in_=yst[0:64, :n]), reads=[ysk], writes=[('YT', kind, h, t0)])
            done(s, ph)


def phase_out(s, l, b):
    T, NB = s.T, s.NB
    PS = s.PS
    with contextlib.ExitStack() as st:
        wo = sb(s, "owo", [128, KC, D], BF16, st)
        ytr = Rot([sb(s, f"oyt{i}", [128, KC, 512], BF16, st) for i in range(2)], "oyt")
        xsr = Rot([sb(s, f"oxs{i}", [128, KC, 512], F32, st) for i in range(2)], "oxs")
        x2r = Rot([sb(s, f"ox2{i}", [128, KC, 512], F32, st) for i in range(2)], "ox2")
        hTr = Rot([sb(s, f"ohT{i}", [128, KC, 512], BF16, st) for i in range(2)], "ohT")
        sqr = Rot([sb(s, f"osq{i}", [128, 512], BF16, st) for i in range(2)], "osq")
        rstd = sb(s, "orstd", [128, 512], F32, st)
        tmpr = Rot([sb(s, f"otmp{i}", [128, 512], F32, st) for i in range(2)], "otmp")
        rtr = Rot([sb(s, f"ort{i}", [128, 8, 16], F32, st) for i in range(2)], "ort")
        hrr = Rot([sb(s, f"ohr{i}", [128, D], BF16, st) for i in range(2)], "ohr")
        psr = Rot(PS[0:4], "ps")
        ph = new_phase(s, "o")
        ph.op('pq', lambda e: e.dma_start(out=wo[:], in_=s.w_out[l].rearrange("(k p) n -> p k n", p=128)), writes=['wo'])
        for (t0, n) in s.chunks:
            isx = t0 >= CTX
            bcol = b if isx else NB
            yt_, ytk = ytr.next()
            ph.op('sp', lambda e, yt_=yt_, t0=t0, n=n: e.dma_start(
                out=yt_[:, :, :n], in_=s.YT[:, :, t0:t0 + n].rearrange("k p t -> p k t")), writes=[ytk])
            xs, xk = xsr.next()
            ph.op('sp', lambda e, xs=xs, t0=t0, n=n: e.dma_start(
                out=xs[:, :, :n], in_=s.XT[b, :, :, t0:t0 + n].rearrange("k p t -> p k t")), writes=[xk])
            x2, x2k = x2r.next()
            for nn in range(KC):
                ps, pk = psr.next()
                for k in range(KC):
                    ph.op('pe', lambda e, ps=ps, nn=nn, k=k, yt_=yt_: e.matmul(
                        ps[:, :n], wo[:, k, nn * 128:(nn + 1) * 128], yt_[:, k, :n], start=(k == 0), stop=(k == KC - 1)),
                        reads=['wo', ytk], writes=[pk])
                ph.op('dve', lambda e, ps=ps, nn=nn, x2=x2, xs=xs: e.scalar_tensor_tensor(
                    out=x2[:, nn, :n], in0=ps[:, :n], scalar=s.modT[:, l, 16 + nn, bcol:bcol + 1], in1=xs[:, nn, :n],
                    op0=ALU.mult, op1=ALU.add), reads=[pk, xk], writes=[x2k])
            ph.op('sp', lambda e, x2=x2, t0=t0, n=n: e.dma_start(
                out=s.XT[b, :, :, t0:t0 + n].rearrange("k p t -> p k t"), in_=x2[:, :, :n]), reads=[x2k], writes=[('XT', t0)])
            hT, hk = hTr.next()
            norm_mod(s, ph, x2, x2k, n, s.gs2, 24, l, bcol, hT, hk, sqr, rstd, tmpr)
            for i in range(n // 128):
                ps, pk = psr.next()
                for k in range(KC):
                    ph.op('pe', lambda e, ps=ps, k=k, i=i, hT=hT: e.matmul(
                        ps[:, 0:NE], hT[:, k, i * 128:(i + 1) * 128], s.wr_bf[:, k, :], start=(k == 0), stop=(k == KC - 1)),
                        reads=[hk, 'wr'], writes=[pk])
                rt, rk = rtr.next()
                R = lambda j: rt[:, j, :]
                G = lambda j: rt[:, j, :].rearrange("p (g e) -> p g e", g=4)
                ph.op('act', lambda e, ps=ps, rt=rt: e.activation(out=rt[:, 0, :], in_=ps[:, 0:NE], func=AF.Sigmoid),
                      reads=[pk], writes=[rk])
                seqops = [
                    lambda e, rt=rt: e.tensor_tensor(out=rt[:, 1, :], in0=rt[:, 0, :], in1=s.br_bc[:], op=ALU.add),
                    lambda e, rt=rt: e.tensor_reduce(out=rt[:, 2, 0:4], in_=rt[:, 1, :].rearrange("p (g e) -> p g e", g=4),
                                                     axis=AX.X, op=ALU.max),
                    lambda e, rt=rt: e.tensor_tensor(out=rt[:, 3, :].rearrange("p (g e) -> p g e", g=4),
                                                     in0=rt[:, 1, :].rearrange("p (g e) -> p g e", g=4),
                                                     in1=rt[:, 2, 0:4].unsqueeze(2).broadcast_to([128, 4, 4]), op=ALU.is_equal),
                    lambda e, rt=rt: e.scalar_tensor_tensor(out=rt[:, 3, :], in0=rt[:, 3, :], scalar=-1e9, in1=rt[:, 1, :],
                                                            op0=ALU.mult, op1=ALU.add),
                    lambda e, rt=rt: e.tensor_reduce(out=rt[:, 2, 4:8], in_=rt[:, 3, :].rearrange("p (g e) -> p g e", g=4),
                                                     axis=AX.X, op=ALU.max),
                    lambda e, rt=rt: e.tensor_tensor(out=rt[:, 2, 8:12], in0=rt[:, 2, 0:4], in1=rt[:, 2, 4:8], op=ALU.add),
                    lambda e, rt=rt: e.tensor_reduce(out=rt[:, 2, 12:13], in_=rt[:, 2, 8:12], axis=AX.X, op=ALU.max),
                    lambda e, rt=rt: e.tensor_scalar(out=rt[:, 4, 0:4], in0=rt[:, 2, 8:12], scalar1=rt[:, 2, 12:13], scalar2=None,
                                                     op0=ALU.is_ge),
                    lambda e, rt=rt: e.tensor_tensor(out=rt[:, 5, :].rearrange("p (g e) -> p g e", g=4),
                                                     in0=rt[:, 1, :].rearrange("p (g e) -> p g e", g=4),
                                                     in1=rt[:, 2, 4:8].unsqueeze(2).broadcast_to([128, 4, 4]), op=ALU.is_ge),
                    lambda e, rt=rt: e.tensor_tensor(out=rt[:, 5, :].rearrange("p (g e) -> p g e", g=4),
                                                     in0=rt[:, 5, :].rearrange("p (g e) -> p g e", g=4),
                                                     in1=rt[:, 4, 0:4].unsqueeze(2).broadcast_to([128, 4, 4]), op=ALU.mult),
                    lambda e, rt=rt: e.tensor_tensor(out=rt[:, 6, :], in0=rt[:, 5, :], in1=rt[:, 0, :], op=ALU.mult),
                    lambda e, rt=rt: e.tensor_reduce(out=rt[:, 4, 4:5], in_=rt[:, 6, :], axis=AX.X, op=ALU.add),
                    lambda e, rt=rt: e.reciprocal(out=rt[:, 4, 4:5], in_=rt[:, 4, 4:5]),
                    lambda e, rt=rt: e.tensor_scalar(out=rt[:, 7, :], in0=rt[:, 6, :], scalar1=rt[:, 4, 4:5], scalar2=None,
                                                     op0=ALU.mult),
                ]
                for fn in seqops:
                    ph.op('dve', fn, reads=[rk], writes=[rk])
                ti = b * s.NT + t0 // 128 + i
                ph.op('dve', lambda e, rt=rt, ti=ti: e.tensor_copy(out=s.SELt[:, ti, :], in_=rt[:, 5, :]), reads=[rk], writes=[('selt', ti)])
                ph.op('dve', lambda e, rt=rt, ti=ti: e.tensor_copy(out=s.WGt[:, ti, :], in_=rt[:, 7, :]), reads=[rk], writes=[('wgt', ti)])
                pt, ptk = psr.next()
                ph.op('pe', lambda e, pt=pt, rt=rt: e.matmul(pt[:, 0:NE], s.ustrict[:], rt[:, 5, :], start=True, stop=True),
                      reads=[rk, 'ustrict'], writes=[ptk])
                ph.op('pe', lambda e, pt=pt, rt=rt: e.matmul(pt[:, NE:2 * NE], s.ones_f[:], rt[:, 5, :], start=True, stop=True),
                      reads=[rk, 'ones_f'], writes=[ptk])
                ph.op('dve', lambda e, pt=pt, ti=ti: e.tensor_tensor(out=s.RKt[:, ti, :], in0=pt[:, 0:NE], in1=s.cbase[:], op=ALU.add),
                      reads=[ptk, 'cbase'], writes=[('rkt', ti)])
                ph.op('dve', lambda e, pt=pt: e.tensor_tensor(out=s.cbase[:], in0=pt[:, NE:2 * NE], in1=s.cbase[:], op=ALU.add),
                      reads=[ptk, 'cbase'], writes=['cbase'])
                for c in range(KC):
                    ph.op('pe', lambda e, hT=hT, c=c, i=i: e.transpose(
                        s.PSB[:, c * 128:(c + 1) * 128], hT[:, c, i * 128:(i + 1) * 128], s.ident_bf[:]),
                        reads=[hk, 'ident_bf'], writes=['psb'])
                hr, hrk = hrr.next()
                ph.op('act', lambda e, hr=hr: e.activation(out=hr[:], in_=s.PSB[:], func=AF.Copy), reads=['psb'], writes=[hrk])
                gt = b * T + t0 + i * 128
                ph.op('sp', lambda e, hr=hr, gt=gt: e.dma_start(out=s.H2TOK[gt:gt + 128, :], in_=hr[:]), reads=[hrk], writes=[('H2TOK', gt)])
        done(s, ph)


def phase_moe(s, l):
    T, NB, DEPTH = s.T, s.NB, s.DEPTH
    PS = s.PS
    supers = []
    for b in range(NB):
        cur, tot = [], 0
        for (t0, n) in s.chunks:
            if tot + n > 1280:
                supers.append((b, cur))
                cur, tot = [], 0
            cur.append((t0, n))
            tot += n
        if cur:
            supers.append((b, cur))
    with contextlib.ExitStack() as st:
        acc = sb(s, "macc", [128, KC, 1280], F32, st)
        wgr = Rot([sb(s, f"mwg{i}", [128, KC, 2 * D], BF16, st) for i in range(2)], "mwg")
        wdr = Rot([sb(s, f"mwd{i}", [128, KC, D], BF16, st) for i in range(1)], "mwd")
        h2r = Rot([sb(s, f"mh2{i}", [128, KC, 512], BF16, st) for i in range(2)], "mh2")
        wtr = Rot([sb(s, f"mwt{i}", [16, 512], F32, st) for i in range(2)], "mwt")
        wbr = Rot([sb(s, f"mwb{i}", [128, 512], F32, st) for i in range(2)], "mwb")
        sar = Rot([sb(s, f"msa{i}", [128, 512], F32, st) for i in range(2)], "msa")
        ttr = Rot([sb(s, f"mtt{i}", [128, 512], F32, st) for i in range(2)], "mtt")
        aTr = Rot([sb(s, f"maT{i}", [128, KC, 512], BF16, st) for i in range(1)], "maT")
        xsr = Rot([sb(s, f"mxs{i}", [128, KC, 128], F32, st) for i in range(2)], "mxs")
        otr = Rot([sb(s, f"mot{i}", [128, D], F32, st) for i in range(2)], "mot")
        psr = Rot(PS[0:6], "ps")
        for (b, chs) in supers:
            ph = new_phase(s, "m")
            for ex in range(NE):
                wg, wgk = wgr.next()
                wd, wdk = wdr.next()
                for hf in range(2):
                    ph.op('pq', lambda e, wg=wg, ex=ex, hf=hf: e.dma_start(
                        out=wg[:, :, hf * D:(hf + 1) * D],
                        in_=s.w_gu[l, ex].rearrange("(k p) n -> p k n", p=128)[:, :, hf * D:(hf + 1) * D]), writes=[wgk])
                ph.op('pq', lambda e, wd=wd, ex=ex: e.dma_start(
                    out=wd[:], in_=s.w_dn[l, ex].rearrange("(k p) n -> p k n", p=128)), writes=[wdk])
                off = 0
                for (t0, n) in chs:
                    gt = b * T + t0
                    h2, h2k = h2r.next()
                    ph.op('sp', lambda e, h2=h2, gt=gt, n=n: e.dma_start(
                        out=h2[:, :, :n], in_=s.H2T[:, :, gt:gt + n].rearrange("k p t -> p k t")), writes=[h2k])
                    wt, wtk = wtr.next()
                    ph.op('sp', lambda e, wt=wt, gt=gt, n=n: e.dma_start(out=wt[0:NE, :n], in_=s.WGT[:, gt:gt + n]), writes=[wtk])
                    ph.op('pe', lambda e, wt=wt, ex=ex, n=n: e.matmul(
                        PS[6][:, :n], s.sel_sb[0:NE, ex * 128:(ex + 1) * 128], wt[0:NE, :n], start=True, stop=True),
                        reads=[wtk, 'sel'], writes=[('ps', 6)])
                    wb, wbk = wbr.next()
                    ph.op('act', lambda e, wb=wb, n=n: e.activation(out=wb[:, :n], in_=PS[6][:, :n], func=AF.Copy),
                          reads=[('ps', 6)], writes=[wbk])
                    aT, aTk = aTr.next()
                    for f in range(KC):
                        pa, pak = psr.next()
                        pu, puk = psr.next()
                        for k in range(KC):
                            ph.op('pe', lambda e, pa=pa, wg=wg, h2=h2, f=f, k=k, n=n: e.matmul(
                                pa[:, :n], wg[:, k, f * 128:(f + 1) * 128], h2[:, k, :n], start=(k == 0), stop=(k == KC - 1)),
                                reads=[wgk, h2k], writes=[pak])
                        for k in range(KC):
                            ph.op('pe', lambda e, pu=pu, wg=wg, h2=h2, f=f, k=k, n=n: e.matmul(
                                pu[:, :n], wg[:, k, D + f * 128:D + (f + 1) * 128], h2[:, k, :n], start=(k == 0), stop=(k == KC - 1)),
                                reads=[wgk, h2k], writes=[puk])
                        sa, sak = sar.next()
                        ph.op('act', lambda e, pa=pa, sa=sa, n=n: e.activation(out=sa[:, :n], in_=pa[:, :n], func=AF.Silu),
                              reads=[pak], writes=[sak])
                        tt, ttk = ttr.next()
                        ph.op('dve', lambda e, pu=pu, sa=sa, tt=tt, n=n: e.tensor_tensor(
                            out=tt[:, :n], in0=pu[:, :n], in1=sa[:, :n], op=ALU.mult), reads=[puk, sak], writes=[ttk])
                        ph.op('pool', lambda e, tt=tt, wb=wb, aT=aT, f=f, n=n: e.tensor_tensor(
                            out=aT[:, f, :n], in0=tt[:, :n], in1=wb[:, :n], op=ALU.mult), reads=[ttk, wbk], writes=[(aTk, f)])
                    for nn in range(KC):
                        py, pyk = psr.next()
                        for f in range(KC):
                            ph.op('pe', lambda e, py=py, wd=wd, aT=aT, f=f, nn=nn, n=n: e.matmul(
                                py[:, :n], wd[:, f, nn * 128:(nn + 1) * 128], aT[:, f, :n], start=(f == 0), stop=(f == KC - 1)),
                                reads=[wdk, (aTk, f)], writes=[pyk])
                        if ex == 0:
                            ph.op('dve', lambda e, py=py, nn=nn, off=off, n=n: e.tensor_copy(
                                out=acc[:, nn, off:off + n], in_=py[:, :n]), reads=[pyk], writes=[('acc', nn, off)])
                        else:
                            ph.op('dve', lambda e, py=py, nn=nn, off=off, n=n: e.tensor_tensor(
                                out=acc[:, nn, off:off + n], in0=acc[:, nn, off:off + n], in1=py[:, :n], op=ALU.add),
                                reads=[pyk, ('acc', nn, off)], writes=[('acc', nn, off)])
                    off += n
            off = 0
            for (t0, n) in chs:
                isx = t0 >= CTX
                bcol = b if isx else NB
                for i in range(n // 128):
                    tt0 = t0 + i * 128
                    xs, xk = xsr.next()
                    ph.op('sp', lambda e, xs=xs, tt0=tt0: e.dma_start(
                        out=xs[:], in_=s.XT[b, :, :, tt0:tt0 + 128].rearrange("k p t -> p k t")), writes=[xk])
                    for nn in range(KC):
                        ph.op('dve', lambda e, xs=xs, nn=nn, o=off + i * 128: e.scalar_tensor_tensor(
                            out=xs[:, nn, :], in0=acc[:, nn, o:o + 128], scalar=s.modT[:, l, 40 + nn, bcol:bcol + 1],
                            in1=xs[:, nn, :], op0=ALU.mult, op1=ALU.add),
                            reads=[xk] + [('acc', nn, oo) for oo in set([off])], writes=[xk])
                    if l < DEPTH - 1:
                        ph.op('sp', lambda e, xs=xs, tt0=tt0: e.dma_start(
                            out=s.XT[b, :, :, tt0:tt0 + 128].rearrange("k p t -> p k t"), in_=xs[:]), reads=[xk], writes=[('XTo', tt0)])
                    else:
                        ot, otk = otr.next()
                        for half in range(2):
                            pt, ptk = psr.next()
                            for j in range(4):
                                c = half * 4 + j
                                ph.op('pe', lambda e, pt=pt, xs=xs, j=j, c=c: e.transpose(
                                    pt[:, j * 128:(j + 1) * 128], xs[:, c, :], s.ident[:]), reads=[xk, 'ident'], writes=[ptk])
                            if half == 0:
                                ph.op('act', lambda e, pt=pt, ot=ot: e.activation(out=ot[:, 0:512], in_=pt[:], func=AF.Copy),
                                      reads=[ptk], writes=[(otk, 0)])
                            else:
                                ph.op('dve', lambda e, pt=pt, ot=ot: e.tensor_copy(out=ot[:, 512:1024], in_=pt[:]),
                                      reads=[ptk], writes=[(otk, 1)])
                        dst = s.out_x[b, tt0 - CTX:tt0 - CTX + 128, :] if isx else s.out_c[b, tt0:tt0 + 128, :]
                        ph.op('sp', lambda e, ot=ot, dst=dst: e.dma_start(out=dst, in_=ot[:]),
                              reads=[(otk, 0), (otk, 1)], writes=[('out', tt0)])
                off += n
            done(s, ph)


def phase_dest(s, l):
    NTA, NBLK = s.NTA, s.NBLK
    with contextlib.ExitStack() as st:
        r_ = sb(s, "dr", [128, NE], F32, st)
        g_ = sb(s, "dg", [128, NE], F32, st)
        pad = sb(s, "dpad", [128, NE], F32, st)
        pst_ = sb(s, "dpst", [128, NE], F32, st)
        pend = sb(s, "dpend", [128, NE], F32, st)
        t1r = Rot([sb(s, f"dt1{i}", [128, NE], F32, st) for i in range(2)], "dt1")
        t2r = Rot([sb(s, f"dt2{i}", [128, NE], F32, st) for i in range(2)], "dt2")
        dstr = Rot([sb(s, f"ddst{i}", [128, NE], F32, st) for i in range(2)], "ddst")
        m1r = Rot([sb(s, f"dm1{i}", [128, 2], F32, st) for i in range(2)], "dm1")
        eacc = sb(s, "deacc", [128, 64], F32, st)
        hrr = Rot([sb(s, f"dhr{i}", [128, D], BF16, st) for i in range(3)], "dhr")
        ph = new_phase(s, "d")
        ki = sb(s, "dki", [128, NE], I32, st)
        ph.op('dve', lambda e: e.tensor_scalar(out=r_[:], in0=s.cbase[:], scalar1=float(MB - 1), scalar2=1.0 / MB, op0=ALU.add, op1=ALU.mult),
              writes=['r'])
        ph.op('dve', lambda e: e.tensor_copy(out=ki[:], in_=r_[:]), reads=['r'], writes=['ki'])
        ph.op('dve', lambda e: e.tensor_copy(out=g_[:], in_=ki[:]), reads=['ki'], writes=['g'])
        ph.op('dve', lambda e: e.tensor_tensor(out=pad[:], in0=g_[:], in1=r_[:], op=ALU.is_gt), reads=['g', 'r'], writes=['pad'])
        ph.op('dve', lambda e: e.tensor_tensor(out=g_[:], in0=g_[:], in1=pad[:], op=ALU.subtract), reads=['g', 'pad'], writes=['g'])
        ph.op('dve', lambda e: e.tensor_scalar(out=pad[:], in0=g_[:], scalar1=float(MB), scalar2=None, op0=ALU.mult), reads=['g'], writes=['pad'])
        ph.op('dve', lambda e: e.memset(pst_[:], 0.0), writes=['pst'])
        for ex in range(1, NE):
            ph.op('dve', lambda e, ex=ex: e.tensor_tensor(out=pst_[:, ex:ex + 1], in0=pst_[:, ex - 1:ex], in1=pad[:, ex - 1:ex], op=ALU.add),
                  reads=['pst', 'pad'], writes=['pst'])
        ph.op('dve', lambda e: e.tensor_tensor(out=pend[:], in0=pst_[:], in1=pad[:], op=ALU.add), reads=['pst', 'pad'], writes=['pend'])
        for ti in range(NTA):
            dst, dk = dstr.next()
            ph.op('dve', lambda e, dst=dst, ti=ti: e.tensor_tensor(out=dst[:], in0=s.RKt[:, ti, :], in1=pst_[:], op=ALU.add),
                  reads=['pst'], writes=[dk])
            t1, t1k = t1r.next()
            ph.op('dve', lambda e, dst=dst, t1=t1, ti=ti: e.scalar_tensor_tensor(
                out=t1[:], in0=dst[:], scalar=1.0, in1=s.SELt[:, ti, :], op0=ALU.add, op1=ALU.mult), reads=[dk], writes=[t1k])
            m1, m1k = m1r.next()
            ph.op('dve', lambda e, t1=t1, m1=m1: e.tensor_reduce(out=m1[:, 0:1], in_=t1[:], axis=AX.X, op=ALU.max), reads=[t1k], writes=[m1k])
            t2, t2k = t2r.next()
            ph.op('dve', lambda e, dst=dst, t2=t2, ti=ti: e.scalar_tensor_tensor(
                out=t2[:], in0=s.SELt[:, ti, :], scalar=-1.0e6, in1=dst[:], op0=ALU.mult, op1=ALU.add), reads=[dk], writes=[t2k])
            ph.op('dve', lambda e, t2=t2, m1=m1: e.tensor_reduce(out=m1[:, 1:2], in_=t2[:], axis=AX.X, op=ALU.min), reads=[t2k, m1k], writes=[m1k])
            ph.op('dve', lambda e, m1=m1, ti=ti: e.tensor_scalar(out=s.DAf[:, ti:ti + 1], in0=m1[:, 0:1], scalar1=-1.0, scalar2=None, op0=ALU.add),
                  reads=[m1k], writes=['daf'])
            ph.op('dve', lambda e, m1=m1, ti=ti: e.tensor_scalar(out=s.DBf[:, ti:ti + 1], in0=m1[:, 1:2], scalar1=1.0e6, scalar2=None, op0=ALU.add),
                  reads=[m1k], writes=['dbf'])
            ph.op('dve', lambda e, t1=t1, m1=m1: e.tensor_scalar(out=t1[:], in0=t1[:], scalar1=m1[:, 0:1], scalar2=None, op0=ALU.is_equal),
                  reads=[t1k, m1k], writes=[t1k])
            ph.op('dve', lambda e, t1=t1, ti=ti: e.tensor_tensor(out=t1[:], in0=t1[:], in1=s.WGt[:, ti, :], op=ALU.mult), reads=[t1k], writes=[t1k])
            ph.op('dve', lambda e, t1=t1, ti=ti: e.tensor_reduce(out=s.WA[:, ti:ti + 1], in_=t1[:], axis=AX.X, op=ALU.add), reads=[t1k], writes=['wa'])
        ph.op('dve', lambda e: e.tensor_scalar(out=s.WB[:], in0=s.WA[:], scalar1=-1.0, scalar2=1.0, op0=ALU.mult, op1=ALU.add),
              reads=['wa'], writes=['wb'])
        ph.op('dve', lambda e: e.tensor_copy(out=s.DAi[:], in_=s.DAf[:]), reads=['daf'], writes=['dai'])
        ph.op('dve', lambda e: e.tensor_copy(out=s.DBi[:], in_=s.DBf[:]), reads=['dbf'], writes=['dbi'])
        ph.op('dve', lambda e: e.memset(eacc[:], 0.0), writes=['eacc'])
        for ex in range(NE):
            ph.op('dve', lambda e, ex=ex: e.scalar_tensor_tensor(
                out=eacc[:], in0=s.blkiota[:], scalar=pend[:, ex:ex + 1], in1=eacc[:], op0=ALU.is_ge, op1=ALU.add),
                reads=['pend', 'eacc'], writes=['eacc'])
        ph.op('dve', lambda e: e.tensor_scalar(out=eacc[:], in0=eacc[:], scalar1=float(NE - 1), scalar2=128.0, op0=ALU.min, op1=ALU.mult),
              reads=['eacc'], writes=['eacc'])
        ph.op('dve', lambda e: e.tensor_scalar(out=eacc[:], in0=eacc[:], scalar1=s.pidx[:, 0:1], scalar2=None, op0=ALU.add),
              reads=['eacc'], writes=['eacc'])
        ph.op('dve', lambda e: e.tensor_copy(out=s.IDXi[:], in_=eacc[:]), reads=['eacc'], writes=['idxi'])
        for ti in range(NTA):
            hr, hrk = hrr.next()
            ph.op('sp', lambda e, hr=hr, ti=ti: e.dma_start(out=hr[:], in_=s.H2TOK[ti * 128:(ti + 1) * 128, :]), writes=[hrk])
            for (dd, dn) in ((s.DAi, 'dai'), (s.DBi, 'dbi')):
                ph.op('pq', lambda e, hr=hr, ti=ti, dd=dd: e.indirect_dma_start(
                    out=s.XP, out_offset=bass.IndirectOffsetOnAxis(ap=dd[:, ti:ti + 1], axis=0), in_=hr[:, :], in_offset=None),
                    reads=[hrk, dn], writes=[('XPs', ti, dn)])
        done(s, ph)


def phase_moe_sparse(s, l):
    NBLK = s.NBLK
    PS = s.PS
    with contextlib.ExitStack() as st:
        wgr = Rot([sb(s, f"swg{i}", [128, KC, 2 * D], BF16, st) for i in range(2)], "swg")
        wdr = Rot([sb(s, f"swd{i}", [128, KC, D], BF16, st) for i in range(2)], "swd")
        xrr = Rot([sb(s, f"sxr{i}", [128, D], BF16, st) for i in range(4)], "sxr")
        xpr = Rot([sb(s, f"sxp{i}", [128, KC, MB], BF16, st) for i in range(2)], "sxp")
        sar = Rot([sb(s, f"ssa{i}", [128, MB], F32, st) for i in range(2)], "ssa")
        aTr = Rot([sb(s, f"saT{i}", [128, KC, MB], BF16, st) for i in range(2)], "saT")
        ypr = Rot([sb(s, f"syp{i}", [128, D], F32, st) for i in range(2)], "syp")
        psr = Rot(PS[0:7], "ps")
        ph = new_phase(s, "s")
        for j in range(NBLK):
            wg, wgk = wgr.next()
            wd, wdk = wdr.next()
            ph.op('pq', lambda e, wg=wg, j=j: e.indirect_dma_start(
                out=wg[:].rearrange("p k n -> p (k n)"), out_offset=None, in_=s.WBgu.rearrange("e p k n -> (e p) (k n)"),
                in_offset=bass.IndirectOffsetOnAxis(ap=s.IDXi[:, j:j + 1], axis=0)), writes=[wgk])
            ph.op('pq', lambda e, wd=wd, j=j: e.indirect_dma_start(
                out=wd[:].rearrange("p k n -> p (k n)"), out_offset=None, in_=s.WBdn.rearrange("e p k n -> (e p) (k n)"),
                in_offset=bass.IndirectOffsetOnAxis(ap=s.IDXi[:, j:j + 1], axis=0)), writes=[wdk])
            xp, xpk = xpr.next()
            for sub in range(MB // 128):
                xr, xrk = xrr.next()
                r0 = j * MB + sub * 128
                ph.op('sp', lambda e, xr=xr, r0=r0: e.dma_start(out=xr[:], in_=s.XP[r0:r0 + 128, :]), writes=[xrk])
                for c in range(KC):
                    ph.op('pe', lambda e, xr=xr, c=c: e.transpose(
                        s.PSB[:, c * 128:(c + 1) * 128], xr[:, c * 128:(c + 1) * 128], s.ident_bf[:]),
                        reads=[xrk, 'ident_bf'], writes=['psb'])
                if sub % 2 == 0:
                    ph.op('act', lambda e, xp=xp, sub=sub: e.activation(
                        out=xp[:, :, sub * 128:(sub + 1) * 128], in_=s.PSB[:].rearrange("p (c t) -> p c t", c=KC), func=AF.Copy),
                        reads=['psb'], writes=[(xpk, sub)])
                else:
                    ph.op('dve', lambda e, xp=xp, sub=sub: e.tensor_copy(
                        out=xp[:, :, sub * 128:(sub + 1) * 128], in_=s.PSB[:].rearrange("p (c t) -> p c t", c=KC)),
                        reads=['psb'], writes=[(xpk, sub)])
            xpkeys = [(xpk, sub) for sub in range(MB // 128)]
            aT, aTk = aTr.next()
            for f in range(KC):
                pa, pak = psr.next()
                pu, puk = psr.next()
                for k in range(KC):
                    ph.op('pe', lambda e, pa=pa, wg=wg, xp=xp, f=f, k=k: e.matmul(
                        pa[:], wg[:, k, f * 128:(f + 1) * 128], xp[:, k, :], start=(k == 0), stop=(k == KC - 1)),
                        reads=[wgk] + xpkeys, writes=[pak])
                for k in range(KC):
                    ph.op('pe', lambda e, pu=pu, wg=wg, xp=xp, f=f, k=k: e.matmul(
                        pu[:], wg[:, k, D + f * 128:D + (f + 1) * 128], xp[:, k, :], start=(k == 0), stop=(k == KC - 1)),
                        reads=[wgk] + xpkeys, writes=[puk])
                sa, sak = sar.next()
                ph.op('act', lambda e, pa=pa, sa=sa: e.activation(out=sa[:], in_=pa[:], func=AF.Silu), reads=[pak], writes=[sak])
                ph.op('dve', lambda e, pu=pu, sa=sa, aT=aT, f=f: e.tensor_tensor(out=aT[:, f, :], in0=pu[:], in1=sa[:], op=ALU.mult),
                      reads=[puk, sak], writes=[(aTk, f)])
            aTkeys = [(aTk, f) for f in range(KC)]
            for sub in range(MB // 128):
                yp, ypk = ypr.next()
                for nh in range(2):
                    py, pyk = psr.next()
                    for f in range(KC):
                        ph.op('pe', lambda e, py=py, wd=wd, aT=aT, f=f, nh=nh, sub=sub: e.matmul(
                            py[:], aT[:, f, sub * 128:(sub + 1) * 128], wd[:, f, nh * 512:(nh + 1) * 512], start=(f == 0), stop=(f == KC - 1)),
                            reads=[wdk] + aTkeys, writes=[pyk])
                    if nh == 0:
                        ph.op('act', lambda e, py=py, yp=yp: e.activation(out=yp[:, 0:512], in_=py[:], func=AF.Copy), reads=[pyk], writes=[(ypk, 0)])
                    else:
                        ph.op('dve', lambda e, py=py, yp=yp: e.tensor_copy(out=yp[:, 512:1024], in_=py[:]), reads=[pyk], writes=[(ypk, 1)])
                r0 = j * MB + sub * 128
                ph.op('sp', lambda e, yp=yp, r0=r0: e.dma_start(out=s.YP[r0:r0 + 128, :], in_=yp[:]), reads=[(ypk, 0), (ypk, 1)], writes=[('YP', r0)])
        done(s, ph)


def phase_comb(s, l):
    T, NB, NT, DEPTH = s.T, s.NB, s.NT, s.DEPTH
    PS = s.PS
    with contextlib.ExitStack() as st:
        yar = Rot([sb(s, f"cya{i}", [128, D], F32, st) for i in range(2)], "cya")
        ybr = Rot([sb(s, f"cyb{i}", [128, D], F32, st) for i in range(2)], "cyb")
        xsr = Rot([sb(s, f"cxs{i}", [128, KC, 128], F32, st) for i in range(2)], "cxs")
        otr = Rot([sb(s, f"cot{i}", [128, D], F32, st) for i in range(2)], "cot")
        psr = Rot(PS[0:6], "ps")
        ph = new_phase(s, "k")
        for b in range(NB):
            for i in range(NT):
                ti = b * NT + i
                tt0 = i * 128
                isx = tt0 >= CTX
                bcol = b if isx else NB
                ya, yak = yar.next()
                yb, ybk = ybr.next()
                ph.op('pq', lambda e, ya=ya, ti=ti: e.indirect_dma_start(
                    out=ya[:, :], out_offset=None, in_=s.YP, in_offset=bass.IndirectOffsetOnAxis(ap=s.DAi[:, ti:ti + 1], axis=0)), writes=[yak])
                ph.op('pq', lambda e, yb=yb, ti=ti: e.indirect_dma_start(
                    out=yb[:, :], out_offset=None, in_=s.YP, in_offset=bass.IndirectOffsetOnAxis(ap=s.DBi[:, ti:ti + 1], axis=0)), writes=[ybk])
                ph.op('pool', lambda e, ya=ya, ti=ti: e.tensor_scalar(out=ya[:], in0=ya[:], scalar1=s.WA[:, ti:ti + 1], scalar2=None, op0=ALU.mult),
                      reads=[yak], writes=[yak])
                ph.op('dve', lambda e, ya=ya, yb=yb, ti=ti: e.scalar_tensor_tensor(
                    out=ya[:], in0=yb[:], scalar=s.WB[:, ti:ti + 1], in1=ya[:], op0=ALU.mult, op1=ALU.add), reads=[yak, ybk], writes=[yak])
                xs, xk = xsr.next()
                ph.op('sp', lambda e, xs=xs, tt0=tt0, b=b: e.dma_start(
                    out=xs[:], in_=s.XT[b, :, :, tt0:tt0 + 128].rearrange("k p t -> p k t")), writes=[xk])
                for half in range(2):
                    pt, ptk = psr.next()
                    for jj in range(4):
                        c = half * 4 + jj
                        ph.op('pe', lambda e, pt=pt, ya=ya, jj=jj, c=c: e.transpose(
                            pt[:, jj * 128:(jj + 1) * 128], ya[:, c * 128:(c + 1) * 128], s.ident[:]), reads=[yak, 'ident'], writes=[ptk])
                    for jj in range(4):
                        c = half * 4 + jj
                        ph.op('dve', lambda e, pt=pt, xs=xs, jj=jj, c=c, bcol=bcol: e.scalar_tensor_tensor(
                            out=xs[:, c, :], in0=pt[:, jj * 128:(jj + 1) * 128], scalar=s.modT[:, l, 40 + c, bcol:bcol + 1],
                            in1=xs[:, c, :], op0=ALU.mult, op1=ALU.add), reads=[ptk, xk], writes=[xk])
                if l < DEPTH - 1:
                    ph.op('sp', lambda e, xs=xs, tt0=tt0, b=b: e.dma_start(
                        out=s.XT[b, :, :, tt0:tt0 + 128].rearrange("k p t -> p k t"), in_=xs[:]), reads=[xk], writes=[('XTo', b, tt0)])
                else:
                    ot, otk = otr.next()
                    for half in range(2):
                        pt, ptk = psr.next()
                        for jj in range(4):
                            c = half * 4 + jj
                            ph.op('pe', lambda e, pt=pt, xs=xs, jj=jj, c=c: e.transpose(
                                pt[:, jj * 128:(jj + 1) * 128], xs[:, c, :], s.ident[:]), reads=[xk, 'ident'], writes=[ptk])
                        if half == 0:
                            ph.op('act', lambda e, pt=pt, ot=ot: e.activation(out=ot[:, 0:512], in_=pt[:], func=AF.Copy), reads=[ptk], writes=[(otk, 0)])
                        else:
                            ph.op('dve', lambda e, pt=pt, ot=ot: e.tensor_copy(out=ot[:, 512:1024], in_=pt[:]), reads=[ptk], writes=[(otk, 1)])
                    dst = s.out_x[b, tt0 - CTX:tt0 - CTX + 128, :] if isx else s.out_c[b, tt0:tt0 + 128, :]
                    ph.op('sp', lambda e, ot=ot, dst=dst: e.dma_start(out=dst, in_=ot[:]), reads=[(otk, 0), (otk, 1)], writes=[('out', b, tt0)])
        done(s, ph)


def kernel_run(inputs, L, NB, DEPTH, n_cores, layers=None):
    nc, ninst = build(L, NB, DEPTH)
    consts = host_constants(L)
    in_maps = []
    for c in range(n_cores):
        m = {}
        bs = slice(c * NB, (c + 1) * NB)
        m["x"] = np.ascontiguousarray(inputs["x"][bs])
        m["c"] = np.ascontiguousarray(inputs["c"][bs])
        m["ctx"] = np.ascontiguousarray(inputs["ctx"][bs])
        for k in ("c_ctx", "ada_w", "ada_b", "norm1", "norm2", "w_in", "w_out", "win_qk_gain", "win_sink",
                  "glb_qk_gain", "w_router", "b_router", "w_gate_up", "w_down"):
            m[k] = np.ascontiguousarray(inputs[k])
        m["ret_decay"] = np.ascontiguousarray(inputs["ret_decay"]).reshape(DEPTH, 8)
        for k, v in consts.items():
            m["k_" + k] = v
        in_maps.append(m)
    res = run_bass_kernel_spmd(nc, in_maps, core_ids=list(range(n_cores)))
    if DEBUG:
        global DBG
        DBG = res.results
    xo = np.concatenate([r["out"] for r in res.results], axis=0)
    co = np.concatenate([r["ctx_out"] for r in res.results], axis=0)
    return xo, co


FUSED = True
DEBUG = False
DBG = None


def kernel(**inputs):
    inputs = {k: np.asarray(v) for k, v in inputs.items()}
    B, L, _ = inputs["x"].shape
    depth = inputs["ada_w"].shape[0]
    n_cores = 8
    NB = B // n_cores
    if FUSED:
        xo, _ = kernel_run(inputs, L, NB, depth, n_cores)
        return xo.astype(np.float32)
    x, ctx = inputs["x"], inputs["ctx"]
    for l in range(depth):
        li = dict(inputs)
        li["x"], li["ctx"] = x, ctx
        for k in ("ada_w", "ada_b", "norm1", "norm2", "w_in", "w_out", "ret_decay", "win_qk_gain", "win_sink",
                  "glb_qk_gain", "w_gate_up", "w_down"):
            li[k] = inputs[k][l:l + 1]
        x, ctx = kernel_run(li, L, NB, 1, n_cores)
    return x.astype(np.float32)
```

```python
import contextlib
import types
import numpy as np
import ml_dtypes
import concourse.bass as bass
import concourse.mybir as mybir
from concourse.bass_utils import run_bass_kernel_spmd

F32 = mybir.dt.float32
BF16 = mybir.dt.bfloat16
AF = mybir.ActivationFunctionType
ALU = mybir.AluOpType
AX = mybir.AxisListType

PHYS = {'pe': 'tensor', 'act': 'scalar', 'dve': 'vector', 'pool': 'gpsimd',
        'sp': 'sync', 'pq': 'gpsimd'}
IS_DMA = {'sp', 'pq'}


NS = 8


def freeze(fn):
    if fn.__closure__ is None:
        return fn
    cells = []
    for c in fn.__closure__:
        try:
            cells.append(types.CellType(c.cell_contents))
        except ValueError:
            cells.append(c)
    return types.FunctionType(fn.__code__, fn.__globals__, fn.__name__, fn.__defaults__, tuple(cells))


class Sync:
    def __init__(self, nc, stack):
        self.nc = nc
        self.sem = {}
        self.base = {}
        for e in PHYS:
            if e in IS_DMA:
                self.sem[e] = [stack.enter_context(nc.semaphore(f"s_{e}{k}")) for k in range(NS)]
                self.base[e] = [0] * NS
            else:
                self.sem[e] = stack.enter_context(nc.semaphore(f"s_{e}"))
                self.base[e] = 0


class Phase:
    def __init__(self, sync, name):
        self.sync = sync
        self.nc = sync.nc
        self.name = name
        self.ops = {p: [] for p in ('tensor', 'scalar', 'vector', 'gpsimd', 'sync')}
        self.seq = {e: 0 for e in PHYS}
        self.res = {}
        self.flag = {e: set() for e in PHYS}

    def op(self, eng, fn, reads=(), writes=()):
        deps = set()

        def need(d):
            if d is None:
                return
            if d[0] == 'pe' and eng == 'pe':
                return
            deps.add(d)

        for r in reads:
            st = self.res.get(r)
            if st is not None:
                need(st[0])
        for w in writes:
            st = self.res.get(w)
            if st is not None:
                need(st[0])
                for d in st[1]:
                    need(d)
        self.seq[eng] += 1
        me = (eng, self.seq[eng])
        for r in reads:
            st = self.res.get(r)
            if st is None:
                self.res[r] = [None, [me]]
            else:
                st[1].append(me)
        for w in writes:
            self.res[w] = [me, []]
        if eng in IS_DMA and self.seq[eng] > NS:
            deps.add((eng, self.seq[eng] - NS))
        dd = {}
        for e, sq in deps:
            key = (e, (sq - 1) % NS) if e in IS_DMA else (e, 0)
            dd[key] = max(dd.get(key, 0), sq)
        self.ops[PHYS[eng]].append((eng, self.seq[eng], dd, freeze(fn)))
        return me

    def emit(self):
        nc = self.nc
        sy = self.sync
        final_wait = {}
        for e in IS_DMA:
            for sq in range(max(1, self.seq[e] - NS + 1), self.seq[e] + 1):
                final_wait[(e, (sq - 1) % NS)] = sq
        for p, lst in self.ops.items():
            seen = {}
            for i, (eng, seq, dd, fn) in enumerate(lst):
                nd = {}
                for key, sq in dd.items():
                    if seen.get(key, 0) >= sq:
                        continue
                    seen[key] = sq
                    nd[key] = sq
                    if key[0] not in IS_DMA:
                        self.flag[key[0]].add(sq)
                lst[i] = (eng, seq, nd, fn)
        rank = {}
        for e in PHYS:
            if e in IS_DMA:
                continue
            fl = sorted(self.flag[e])
            rank[e] = {sq: i + 1 for i, sq in enumerate(fl)}

        def wait(engine, key, sq):
            e = key[0]
            if e in IS_DMA:
                slot = (sq - 1) % NS
                engine.wait_ge(sy.sem[e][slot], (sy.base[e][slot] + (sq - 1) // NS + 1) * 16)
            else:
                engine.wait_ge(sy.sem[e], sy.base[e] + rank[e][sq])

        with nc.Block() as block:
            for p, lst in self.ops.items():
                fw = final_wait if p == 'sync' else {}
                if not lst and not fw:
                    continue

                def body(engine, lst=lst, fw=fw):
                    for eng, seq, nd, fn in lst:
                        for key, sq in nd.items():
                            wait(engine, key, sq)
                        inst = fn(engine)
                        if eng in IS_DMA:
                            inst.then_inc(sy.sem[eng][(seq - 1) % NS], 16)
                        elif seq in self.flag[eng]:
                            inst.then_inc(sy.sem[eng], 1)
                    for key, sq in fw.items():
                        wait(engine, key, sq)

                getattr(block, p)(body)
        for e in PHYS:
            if e in IS_DMA:
                for q in range(1, self.seq[e] + 1):
                    sy.base[e][(q - 1) % NS] += 1
            else:
                sy.base[e] += len(self.flag[e])
        return sum(len(l) for l in self.ops.values())


class Rot:
    def __init__(self, tiles, name):
        self.t = tiles
        self.name = name
        self.i = 0

    def next(self):
        k = self.i % len(self.t)
        self.i += 1
        return self.t[k], (self.name, k)


D = 1024
KC = 8
CTX = 256
EPS = 1e-6
NE = 16
MB = 512
I32 = mybir.dt.int32


def host_constants(L):
    c = {}
    c['ident'] = np.eye(128, dtype=np.float32)
    blk = np.zeros((128, 128), np.float32)
    blk[:64, :64] = 1.0
    blk[64:, 64:] = 1.0
    c['blk64'] = blk
    prot = np.zeros((128, 128), np.float32)
    for m in range(128):
        if (m % 32) < 16:
            prot[m + 16, m] = -1.0
        else:
            prot[m - 16, m] = 1.0
    c['prot'] = prot
    t = np.arange(L)
    row = (t // 64).astype(np.float32)
    col = (t % 64).astype(np.float32)
    inv = np.power(np.float32(10000.0), -np.arange(0, 32, 2, dtype=np.float32) / np.float32(32)).astype(np.float32)
    cosT = np.zeros((128, L), np.float32)
    sinT = np.zeros((128, L), np.float32)
    for p in range(128):
        d = p % 64
        a = d // 32
        i = d % 16
        ang = (row if a == 0 else col) * inv[i]
        cosT[p] = np.cos(ang.astype(np.float32))
        sinT[p] = np.sin(ang.astype(np.float32))
    c['cosT'] = cosT
    c['sinT'] = sinT
    k = np.arange(128)[:, None].astype(np.float32)
    q = np.arange(128)[None, :].astype(np.float32)
    c['dif_f'] = np.maximum(q - k, 0.0).astype(np.float32)
    c['msk_f'] = (q >= k).astype(np.float32)
    c['dif_b'] = np.maximum(k - q, 0.0).astype(np.float32)
    c['msk_b'] = (k >= q).astype(np.float32)
    iot = np.zeros((128, 4 * 128), np.float32)
    iot[:, 0:128] = q + 1.0
    iot[:, 128:256] = 128.0 - q
    iot[:, 256:384] = 127.0 - k
    iot[:, 384:512] = k + 0.0 * q
    c['iot'] = iot
    wm = np.zeros((6, 128, 512), np.float32)
    for oi, o in enumerate(range(-1, 5)):
        kp = o * 128 + np.arange(128)[:, None]
        qp = np.arange(512)[None, :]
        wm[oi] = np.where(np.abs(kp - qp) <= 128, 0.0, -30000.0)
    c['wmask'] = wm
    sel = np.zeros((16, 16 * 128), np.float32)
    for e in range(16):
        sel[e, e * 128:(e + 1) * 128] = 1.0
    c['sel'] = sel
    c['ustrict'] = (np.arange(128)[:, None] < np.arange(128)[None, :]).astype(np.float32)
    c['pidx'] = (np.arange(8)[None, :] * 128 + np.arange(128)[:, None]).astype(np.float32)
    c['blkiota'] = np.broadcast_to((np.arange(64) * float(MB))[None, :], (128, 64)).astype(np.float32).copy()
    return c


CONST_SHAPES = lambda L: {'ident': [128, 128], 'blk64': [128, 128], 'prot': [128, 128], 'cosT': [128, L],
                          'sinT': [128, L], 'dif_f': [128, 128], 'msk_f': [128, 128], 'dif_b': [128, 128],
                          'msk_b': [128, 128], 'iot': [128, 512], 'wmask': [6, 128, 512], 'sel': [16, 2048],
                          'ustrict': [128, 128], 'blkiota': [128, 64], 'pidx': [128, 8]}


class K:
    pass


def build(L, NB, DEPTH):
    s = K()
    s.L, s.NB, s.DEPTH = L, NB, DEPTH
    T = s.T = CTX + L
    NT = s.NT = T // 128
    NBC = s.NBC = NB + 1
    TALL = s.TALL = NB * T
    nc = s.nc = bass.Bass("TRN2", target_bir_lowering=False)

    def din(name, shape):
        return nc.dram_tensor(name, list(shape), F32, kind="ExternalInput").ap()

    s.x_in = din("x", [NB, L, D])
    s.c_in = din("c", [NB, D])
    s.ctx_in = din("ctx", [NB, CTX, D])
    s.cctx_in = din("c_ctx", [D])
    s.ada_w = din("ada_w", [DEPTH, D, 6 * D])
    s.ada_b = din("ada_b", [DEPTH, 6 * D])
    s.norm1 = din("norm1", [DEPTH, D])
    s.norm2 = din("norm2", [DEPTH, D])
    s.w_in = din("w_in", [DEPTH, D, 2560])
    s.w_out = din("w_out", [DEPTH, D, D])
    s.ret_decay = din("ret_decay", [DEPTH, 8])
    s.win_gain = din("win_qk_gain", [DEPTH, 2, 64])
    s.win_sink = din("win_sink", [DEPTH, 4])
    s.glb_gain = din("glb_qk_gain", [DEPTH, 2, 64])
    s.w_router = din("w_router", [D, NE])
    s.b_router = din("b_router", [NE])
    s.w_gu = din("w_gate_up", [DEPTH, NE, D, 2 * D])
    s.w_dn = din("w_down", [DEPTH, NE, D, D])
    s.cst = {k: din("k_" + k, sh) for k, sh in CONST_SHAPES(L).items()}
    s.out_x = nc.dram_tensor("out", [NB, L, D], F32, kind="ExternalOutput").ap()
    s.out_c = nc.dram_tensor("ctx_out", [NB, CTX, D], F32, kind="ExternalOutput").ap()
    import os
    ext = os.environ.get("EXT", "").split(",")
    dkf = lambda nm: dict(kind="ExternalOutput") if (DEBUG or nm in ext) else {}
    dk = {}
    s.XT = nc.dram_tensor("XT", [NB, KC, 128, T], F32, **dkf("XT")).ap()
    s.FM = nc.dram_tensor("FMs", [12, 128, T], BF16, **dkf("FM")).ap()
    s.TOK = nc.dram_tensor("TOKs", [T, 1536], BF16, **dkf("TOK")).ap()
    s.YT = nc.dram_tensor("YTs", [KC, 128, T], BF16, **dkf("YT")).ap()
    s.H2T = nc.dram_tensor("H2Ts", [KC, 128, TALL], BF16, **dkf("H2T")).ap()
    s.WGT = nc.dram_tensor("WGTs", [NE, TALL], F32, **dkf("WGT")).ap()
    s.NTA = NB * NT
    s.NBLK = (2 * TALL + NE * (MB - 1) + MB - 1) // MB
    assert s.NBLK <= 64
    s.WBgu = [nc.dram_tensor(f"WBgu{i}", [NE, 128, KC, 2 * D], BF16).ap() for i in range(2)]
    s.WBdn = [nc.dram_tensor(f"WBdn{i}", [NE, 128, KC, D], BF16).ap() for i in range(2)]
    s.H2TOK = nc.dram_tensor("H2TOK", [TALL, D], BF16).ap()
    s.XP = nc.dram_tensor("XPs", [s.NBLK * MB, D], BF16).ap()
    s.YP = nc.dram_tensor("YPs", [s.NBLK * MB, D], BF16).ap()
    s.chunks = [(0, CTX)] + [(CTX + i, min(512, L - i)) for i in range(0, L, 512)]
    s.nphase = 0
    s.ninst = 0
    with contextlib.ExitStack() as gst:
        s.gst = gst
        s.sync = Sync(nc, gst)
        s.ident = sb(s, "ident", [128, 128], F32)
        s.ident_bf = sb(s, "ident_bf", [128, 128], BF16)
        s.ones_bf = sb(s, "ones_bf", [128, 128], BF16)
        s.ones_f = sb(s, "ones_f", [128, 128], F32)
        s.blk64 = sb(s, "blk64", [128, 128], BF16)
        s.prot = sb(s, "prot", [128, 128], BF16)
        s.modT = sb(s, "modT", [128, DEPTH, 48, NBC], F32)
        s.gs1 = sb(s, "gs1", [128, DEPTH, KC, NBC], F32)
        s.gs2 = sb(s, "gs2", [128, DEPTH, KC, NBC], F32)
        s.wr_bf = sb(s, "wr_bf", [128, KC, NE], BF16)
        s.br_bc = sb(s, "br_bc", [128, NE], F32)
        s.ustrict = sb(s, "ustrict", [128, 128], F32)
        s.blkiota = sb(s, "blkiota", [128, 64], F32)
        s.SELt = sb(s, "SELt", [128, s.NTA, NE], F32)
        s.WGt = sb(s, "WGt", [128, s.NTA, NE], F32)
        s.RKt = sb(s, "RKt", [128, s.NTA, NE], F32)
        s.cbase = sb(s, "cbase", [128, NE], F32)
        s.DAf = sb(s, "DAf", [128, s.NTA], F32)
        s.DBf = sb(s, "DBf", [128, s.NTA], F32)
        s.DAi = sb(s, "DAi", [128, s.NTA], I32)
        s.DBi = sb(s, "DBi", [128, s.NTA], I32)
        s.WA = sb(s, "WA", [128, s.NTA], F32)
        s.WB = sb(s, "WB", [128, s.NTA], F32)
        s.IDXi = sb(s, "IDXi", [128, 64], I32)
        s.pidx = sb(s, "pidx", [128, KC], F32)
        s.PS = [gst.enter_context(nc.psum_tensor(f"ps{i}", [128, 512], F32)) for i in range(7)]
        s.PSB = gst.enter_context(nc.psum_tensor("psb", [128, 1024], BF16))
        phase_consts(s)
        phase_input(s)
        for l in range(DEPTH):
            with contextlib.ExitStack() as stL:
                phase_tables(s, l, stL)
                for b in range(NB):
                    phase_proj(s, l, b)
                    phase_ret(s, l, b)
                    phase_attn(s, l, b)
                    phase_out(s, l, b)
            phase_dest(s, l)
            phase_moe_sparse(s, l)
            phase_comb(s, l)
    return nc, s.ninst


def sb(s, name, shape, dt, st=None):
    s.nsb = getattr(s, 'nsb', 0) + 1
    return (st or s.gst).enter_context(s.nc.sbuf_tensor(f"{name}_{s.nsb}", list(shape), dt))


def new_phase(s, tag):
    s.nphase += 1
    return Phase(s.sync, f"{tag}{s.nphase}")


STOP = 10 ** 9


def done(s, ph):
    if s.nphase > STOP:
        return
    s.ninst += ph.emit()


def phase_consts(s):
    nc, NB, NBC, DEPTH, cst = s.nc, s.NB, s.NBC, s.DEPTH, s.cst
    with contextlib.ExitStack() as st0:
        ph = new_phase(s, "c")
        ph.op('sp', lambda e: e.dma_start(out=s.ident[:], in_=cst['ident']), writes=['ident'])
        ph.op('pq', lambda e: e.dma_start(out=s.ident_bf[:], in_=cst['ident']), writes=['ident_bf'])
        ph.op('pq', lambda e: e.dma_start(out=s.blk64[:], in_=cst['blk64']), writes=['blk64'])
        ph.op('pq', lambda e: e.dma_start(out=s.prot[:], in_=cst['prot']), writes=['prot'])
        ph.op('sp', lambda e: e.dma_start(out=s.ustrict[:], in_=cst['ustrict']), writes=['ustrict'])
        ph.op('sp', lambda e: e.dma_start(out=s.blkiota[:], in_=cst['blkiota']), writes=['blkiota'])
        ph.op('sp', lambda e: e.dma_start(out=s.pidx[:], in_=cst['pidx']), writes=['pidx'])
        zrow = sb(s, "zrow", [128, D], BF16, st0)
        ph.op('dve', lambda e: e.memset(zrow[:], 0.0), writes=['zrow'])
        for r0 in range(0, s.NBLK * MB, 128):
            ph.op('sp', lambda e, r0=r0: e.dma_start(out=s.XP[r0:r0 + 128, :], in_=zrow[:]), reads=['zrow'], writes=[('XP', r0)])
        ph.op('dve', lambda e: e.memset(s.ones_bf[:], 1.0), writes=['ones_bf'])
        ph.op('dve', lambda e: e.memset(s.ones_f[:], 1.0), writes=['ones_f'])
        ph.op('pq', lambda e: e.dma_start(out=s.wr_bf[:], in_=s.w_router.rearrange("(k p) n -> p k n", p=128)), writes=['wr'])
        ph.op('sp', lambda e: e.dma_start(out=s.br_bc[:], in_=s.b_router.partition_broadcast(128)), writes=['br'])
        cT = sb(s, "cT", [128, KC, NBC], F32, st0)
        scT = sb(s, "scT", [128, KC, NBC], BF16, st0)
        for b in range(NB):
            ph.op('sp', lambda e, b=b: e.dma_start(out=cT[:, :, b], in_=s.c_in[b].rearrange("(k p) -> p k", p=128),
                                                    allow_slow_non_contiguous=True), writes=['cT'])
        ph.op('sp', lambda e: e.dma_start(out=cT[:, :, NB], in_=s.cctx_in.rearrange("(k p) -> p k", p=128),
                                          allow_slow_non_contiguous=True), writes=['cT'])
        ph.op('act', lambda e: e.activation(out=scT[:], in_=cT[:], func=AF.Silu), reads=['cT'], writes=['scT'])
        adab = sb(s, "adab", [128, DEPTH, 48], F32, st0)
        n1T = sb(s, "n1T", [128, DEPTH, KC], F32, st0)
        n2T = sb(s, "n2T", [128, DEPTH, KC], F32, st0)
        for l in range(DEPTH):
            for (dst, src) in ((adab, s.ada_b), (n1T, s.norm1), (n2T, s.norm2)):
                ph.op('sp', lambda e, l=l, dst=dst, src=src: e.dma_start(
                    out=dst[:, l, :], in_=src[l].rearrange("(c p) -> p c", p=128), allow_slow_non_contiguous=True),
                    writes=['smallT'])
        awr = Rot([sb(s, f"aw{i}", [128, KC, 1536], BF16, st0) for i in range(2)], "aw")
        for l in range(DEPTH):
            for qt in range(4):
                aw, awk = awr.next()
                ph.op('pq', lambda e, aw=aw, l=l, qt=qt: e.dma_start(
                    out=aw[:], in_=s.ada_w[l].rearrange("(k p) n -> p k n", p=128)[:, :, qt * 1536:(qt + 1) * 1536]),
                    writes=[awk])
                for f in range(12):
                    fc = qt * 12 + f
                    ps = s.PS[fc % 4]
                    for k in range(KC):
                        ph.op('pe', lambda e, ps=ps, aw=aw, f=f, k=k: e.matmul(
                            ps[:, 0:NBC], aw[:, k, f * 128:(f + 1) * 128], scT[:, k, :], start=(k == 0), stop=(k == KC - 1)),
                            reads=[awk, 'scT'], writes=[('ps', fc % 4)])
                    ph.op('dve', lambda e, ps=ps, l=l, fc=fc: e.tensor_scalar(
                        out=s.modT[:, l, fc, :], in0=ps[:, 0:NBC], scalar1=adab[:, l, fc:fc + 1], scalar2=None, op0=ALU.add),
                        reads=[('ps', fc % 4), 'smallT'], writes=['modT'])
            for (gs, nT, base) in ((s.gs1, n1T, 8), (s.gs2, n2T, 32)):
                for b in range(NBC):
                    ph.op('dve', lambda e, gs=gs, nT=nT, base=base, b=b, l=l: e.scalar_tensor_tensor(
                        out=gs[:, l, :, b], in0=s.modT[:, l, base:base + 8, b], scalar=1.0, in1=nT[:, l, :],
                        op0=ALU.add, op1=ALU.mult), reads=['modT', 'smallT'], writes=['gs'])
        done(s, ph)


def stage_weight(s, ph, l, i):
    buf = l % 2
    if i < NE:
        ph.op('pq', lambda e, ex=i: e.dma_start(
            out=s.WBgu[buf][ex], in_=s.w_gu[l, ex].rearrange("(k p) n -> p k n", p=128)), writes=[('wbgu', buf, i)])
    else:
        ph.op('pq', lambda e, ex=i - NE: e.dma_start(
            out=s.WBdn[buf][ex], in_=s.w_dn[l, ex].rearrange("(k p) n -> p k n", p=128)), writes=[('wbdn', buf, i)])


def phase_input(s):
    NB, NT = s.NB, s.NT
    with contextlib.ExitStack() as st1:
        ph = new_phase(s, "i")
        for i in range(2 * NE):
            stage_weight(s, ph, 0, i)
        xtr = Rot([sb(s, f"xin{i}", [128, D], F32, st1) for i in range(3)], "xin")
        str_ = Rot([sb(s, f"xstg{i}", [128, KC, 128], F32, st1) for i in range(3)], "xstg")
        for b in range(NB):
            for i in range(NT):
                t0 = i * 128
                src = s.ctx_in[b, t0:t0 + 128, :] if t0 < CTX else s.x_in[b, t0 - CTX:t0 - CTX + 128, :]
                xt, xk = xtr.next()
                ph.op('sp', lambda e, xt=xt, src=src: e.dma_start(out=xt[:], in_=src), writes=[xk])
                stg, sk = str_.next()
                for half in range(2):
                    ps = s.PS[half]
                    for j in range(4):
                        c = half * 4 + j
                        ph.op('pe', lambda e, ps=ps, j=j, c=c, xt=xt: e.transpose(
                            ps[:, j * 128:(j + 1) * 128], xt[:, c * 128:(c + 1) * 128], s.ident[:]),
                            reads=[xk, 'ident'], writes=[('ps', half)])
                    if half == 0:
                        ph.op('act', lambda e, ps=ps, stg=stg: e.activation(
                            out=stg[:, 0:4, :], in_=ps[:].rearrange("p (j t) -> p j t", j=4), func=AF.Copy),
                            reads=[('ps', half)], writes=[(sk, half)])
                    else:
                        ph.op('dve', lambda e, ps=ps, stg=stg: e.tensor_copy(
                            out=stg[:, 4:8, :], in_=ps[:].rearrange("p (j t) -> p j t", j=4)),
                            reads=[('ps', half)], writes=[(sk, half)])
                ph.op('sp', lambda e, stg=stg, b=b, t0=t0: e.dma_start(
                    out=s.XT[b, :, :, t0:t0 + 128].rearrange("k p t -> p k t"), in_=stg[:]),
                    reads=[(sk, 0), (sk, 1)], writes=[('XT', b, t0)])
        done(s, ph)


def norm_mod(s, ph, xs, xk, n, gs, sh, l, bcol, hT, hk, sqr, rstd, tmpr):
    psS = s.PS[6]
    for c in range(KC):
        sq, sqk = sqr.next()
        ph.op('act', lambda e, c=c, sq=sq: e.activation(out=sq[:, :n], in_=xs[:, c, :n], func=AF.Square),
              reads=[xk], writes=[sqk])
        ph.op('pe', lambda e, c=c, sq=sq: e.matmul(psS[:, :n], s.ones_bf[:], sq[:, :n], start=(c == 0), stop=(c == KC - 1)),
              reads=[sqk, 'ones_bf'], writes=[('ps', 6)])
    ph.op('act', lambda e: e.activation(out=rstd[:, :n], in_=psS[:, :n], func=AF.Sqrt, bias=EPS, scale=1.0 / D),
          reads=[('ps', 6)], writes=['rstd'])
    ph.op('dve', lambda e: e.reciprocal(out=rstd[:, :n], in_=rstd[:, :n]), reads=['rstd'], writes=['rstd'])
    for c in range(KC):
        tmp, tk = tmpr.next()
        ph.op('dve', lambda e, c=c, tmp=tmp: e.scalar_tensor_tensor(
            out=tmp[:, :n], in0=xs[:, c, :n], scalar=gs[:, l, c, bcol:bcol + 1], in1=rstd[:, :n],
            op0=ALU.mult, op1=ALU.mult), reads=[xk, 'rstd'], writes=[tk])
        ph.op('act', lambda e, c=c, tmp=tmp: e.activation(
            out=hT[:, c, :n], in_=tmp[:, :n], func=AF.Identity, bias=s.modT[:, l, sh + c, bcol:bcol + 1], scale=1.0),
            reads=[tk], writes=[hk])


def phase_tables(s, l, stL):
    cst = s.cst
    s.gcol = gcol = sb(s, "gcol", [128, 4], F32, stL)
    s.esink = esink = sb(s, "esink", [128, 4], F32, stL)
    lg = sb(s, "lg", [128, 8], F32, stL)
    lgp = sb(s, "lgp", [128, 4], F32, stL)
    s.dsum = dsum = sb(s, "dsum", [128, 4, 128], F32, stL)
    s.xit = xit = sb(s, "xit", [128, 4, 128], F32, stL)
    s.zt = zt = sb(s, "zt", [128, 2, 256], F32, stL)
    s.gcp = gcp = sb(s, "gcp", [128, 4], F32, stL)
    iot = sb(s, "iot", [128, 512], F32, stL)
    dcf = sb(s, "dcf", [128, 4, 128], F32, stL)
    z4 = sb(s, "z4", [128, 8], F32, stL)
    tmpd = sb(s, "tmpd", [128, 128], F32, stL)
    ph = new_phase(s, "t")
    ph.op('dve', lambda e: e.memset(s.cbase[:], 0.0), writes=['cbase'])
    for (i, (src, sc)) in enumerate(((s.win_gain[l, 0], 0.125), (s.win_gain[l, 1], 1.0),
                                     (s.glb_gain[l, 0], 0.125), (s.glb_gain[l, 1], 1.0))):
        for hf in range(2):
            ph.op('sp', lambda e, i=i, src=src, hf=hf: e.dma_start(
                out=gcol[hf * 64:(hf + 1) * 64, i:i + 1], in_=src.rearrange("(d o) -> d o", o=1)), writes=['gcol'])
        if sc != 1.0:
            ph.op('dve', lambda e, i=i, sc=sc: e.tensor_scalar(
                out=gcol[:, i:i + 1], in0=gcol[:, i:i + 1], scalar1=sc, scalar2=None, op0=ALU.mult),
                reads=['gcol'], writes=['gcol'])
    ph.op('sp', lambda e: e.dma_start(out=esink[:], in_=s.win_sink[l].partition_broadcast(128)), writes=['esink'])
    ph.op('act', lambda e: e.activation(out=esink[:], in_=esink[:], func=AF.Exp), reads=['esink'], writes=['esink'])
    ph.op('sp', lambda e: e.dma_start(out=lg[:], in_=s.ret_decay[l].partition_broadcast(128)), writes=['lg'])
    ph.op('sp', lambda e: e.dma_start(out=iot[:], in_=cst['iot']), writes=['iot'])
    for i, nm in enumerate(('dif_f', 'msk_f', 'dif_b', 'msk_b')):
        ph.op('sp', lambda e, i=i, nm=nm: e.dma_start(out=dcf[:, i, :], in_=cst[nm]), writes=['dcf'])
    ph.op('act', lambda e: e.activation(out=lg[:], in_=lg[:], func=AF.Exp, scale=-1.0), reads=['lg'], writes=['lg'])
    ph.op('act', lambda e: e.activation(out=lg[:], in_=lg[:], func=AF.Ln, bias=1.0, scale=1.0), reads=['lg'], writes=['lg'])
    ph.op('dve', lambda e: e.tensor_scalar(out=lg[:], in0=lg[:], scalar1=-1.0, scalar2=None, op0=ALU.mult),
          reads=['lg'], writes=['lg'])
    for dr in range(2):
        for g in range(2):
            for hf in range(2):
                ph.op('dve', lambda e, dr=dr, g=g, hf=hf: e.tensor_copy(
                    out=lgp[hf * 64:(hf + 1) * 64, dr * 2 + g:dr * 2 + g + 1],
                    in_=lg[hf * 64:(hf + 1) * 64, dr * 4 + 2 * g + hf:dr * 4 + 2 * g + hf + 1]),
                    reads=['lg'], writes=['lgp'])
    for h in range(4):
        ph.op('act', lambda e, h=h: e.activation(out=dsum[:, h, :], in_=dcf[:, 0, :], func=AF.Exp, scale=lg[:, h:h + 1]),
              reads=['lg', 'dcf'], writes=[('dsum', h)])
        ph.op('dve', lambda e, h=h: e.tensor_tensor(out=dsum[:, h, :], in0=dsum[:, h, :], in1=dcf[:, 1, :], op=ALU.mult),
              reads=[('dsum', h)], writes=[('dsum', h)])
        ph.op('act', lambda e, h=h: e.activation(out=tmpd[:], in_=dcf[:, 2, :], func=AF.Exp, scale=lg[:, 4 + h:5 + h]),
              reads=['lg', 'dcf'], writes=['tmpd'])
        ph.op('dve', lambda e, h=h: e.tensor_tensor(out=tmpd[:], in0=tmpd[:], in1=dcf[:, 3, :], op=ALU.mult),
              reads=['tmpd'], writes=['tmpd'])
        ph.op('dve', lambda e, h=h: e.tensor_tensor(out=dsum[:, h, :], in0=dsum[:, h, :], in1=tmpd[:], op=ALU.add),
              reads=[('dsum', h), 'tmpd'], writes=[('dsum', h)])
    for dr in range(2):
        for g in range(2):
            i = dr * 2 + g
            ph.op('act', lambda e, i=i, dr=dr: e.activation(
                out=xit[:, i, :], in_=iot[:, dr * 128:(dr + 1) * 128], func=AF.Exp, scale=lgp[:, i:i + 1]),
                reads=['lgp', 'iot'], writes=['xit'])
        for h in range(4):
            ph.op('act', lambda e, dr=dr, h=h: e.activation(
                out=z4[:, dr * 4 + h:dr * 4 + h + 1], in_=iot[:, 256 + dr * 128:257 + dr * 128], func=AF.Exp,
                scale=lg[:, dr * 4 + h:dr * 4 + h + 1]), reads=['lg', 'iot'], writes=['z4'])
            ph.op('dve', lambda e, dr=dr, h=h: e.tensor_scalar(
                out=zt[:, dr, h * 64:(h + 1) * 64], in0=s.ones_f[:, 0:64], scalar1=z4[:, dr * 4 + h:dr * 4 + h + 1],
                scalar2=0.125, op0=ALU.mult, op1=ALU.mult), reads=['z4', 'ones_f'], writes=['zt'])
    ph.op('act', lambda e: e.activation(out=gcp[:], in_=lgp[:], func=AF.Exp, scale=128.0), reads=['lgp'], writes=['gcp'])
    done(s, ph)


FMB = [(0, 0, 256), (256, 256, 256), (512, 1536, 256), (768, 1792, 64), (832, 1792, 64), (896, 1856, 64), (960, 1856, 64),
       (1024, 2048, 256), (1280, 2304, 64), (1344, 2304, 64), (1408, 2368, 64), (1472, 2368, 64)]
TMB = [(0, 256, 256), (256, 1920, 128), (384, 2432, 128), (512, 512, 512), (1024, 1024, 512)]


def phase_proj(s, l, b):
    T, L, NB = s.T, s.L, s.NB
    PS = s.PS
    with contextlib.ExitStack() as st:
        wfm = sb(s, "wfm", [128, KC, 1536], BF16, st)
        wtm = sb(s, "wtm", [128, KC, 1536], BF16, st)
        cosT = sb(s, "cosT", [128, L], F32, st)
        sinT = sb(s, "sinT", [128, L], F32, st)
        xsr = Rot([sb(s, f"pxs{i}", [128, KC, 512], F32, st) for i in range(1)], "pxs")
        hTr = Rot([sb(s, f"phT{i}", [128, KC, 512], BF16, st) for i in range(1)], "phT")
        sqr = Rot([sb(s, f"psq{i}", [128, 512], BF16, st) for i in range(3)], "psq")
        rstd = sb(s, "prstd", [128, 512], F32, st)
        tmpr = Rot([sb(s, f"ptmp{i}", [128, 512], F32, st) for i in range(2)], "ptmp")
        fmr = Rot([sb(s, f"pfm{i}", [128, 12, 512], BF16, st) for i in range(1)], "pfm")
        tokr = Rot([sb(s, f"ptok{i}", [128, 1536], BF16, st) for i in range(1)], "ptok")
        rr = Rot([sb(s, f"pr{i}", [128, 512], F32, st) for i in range(2)], "pr")
        qnr = Rot([sb(s, f"pqn{i}", [128, 512], BF16, st) for i in range(3)], "pqn")
        t1r = Rot([sb(s, f"pt1{i}", [128, 512], F32, st) for i in range(1)], "pt1")
        t2r = Rot([sb(s, f"pt2{i}", [128, 512], F32, st) for i in range(1)], "pt2")
        psr = Rot(PS[0:4], "ps")
        pxr = Rot(PS[4:6], "px")
        ph = new_phase(s, "a")
        wv = s.w_in[l].rearrange("(k p) n -> p k n", p=128)
        for (d0, s0, w) in FMB:
            ph.op('pq', lambda e, d0=d0, s0=s0, w=w: e.dma_start(out=wfm[:, :, d0:d0 + w], in_=wv[:, :, s0:s0 + w]), writes=['wfm'])
        for (d0, s0, w) in TMB:
            ph.op('pq', lambda e, d0=d0, s0=s0, w=w: e.dma_start(out=wtm[:, :, d0:d0 + w], in_=wv[:, :, s0:s0 + w]), writes=['wtm'])
        ph.op('sp', lambda e: e.dma_start(out=cosT[:], in_=s.cst['cosT']), writes=['cos'])
        ph.op('sp', lambda e: e.dma_start(out=sinT[:], in_=s.cst['sinT']), writes=['sin'])
        for (t0, n) in s.chunks:
            isx = t0 >= CTX
            bcol = b if isx else NB
            x0 = t0 - CTX
            xs, xk = xsr.next()
            ph.op('sp', lambda e, xs=xs, t0=t0, n=n: e.dma_start(
                out=xs[:, :, :n], in_=s.XT[b, :, :, t0:t0 + n].rearrange("k p t -> p k t")), writes=[xk])
            hT, hk = hTr.next()
            norm_mod(s, ph, xs, xk, n, s.gs1, 0, l, bcol, hT, hk, sqr, rstd, tmpr)
            fm, fk = fmr.next()
            stB, stC = {}, {}

            def stage_a(j):
                ps, pk = psr.next()
                for k in range(KC):
                    ph.op('pe', lambda e, ps=ps, j=j, k=k, hT=hT: e.matmul(
                        ps[:, :n], wfm[:, k, j * 128:(j + 1) * 128], hT[:, k, :n], start=(k == 0), stop=(k == KC - 1)),
                        reads=['wfm', hk], writes=[pk])
                if j < 2:
                    ph.op('act', lambda e, ps=ps, j=j, fm=fm: e.activation(out=fm[:, j, :n], in_=ps[:, :n], func=AF.Copy),
                          reads=[pk], writes=[(fk, j)])
                elif j < 4:
                    ph.op('act', lambda e, ps=ps, j=j, fm=fm: e.activation(out=fm[:, j, :n], in_=ps[:, :n], func=AF.Copy, scale=0.125),
                          reads=[pk], writes=[(fk, j)])
                else:
                    sq, sqk = sqr.next()
                    ph.op('act', lambda e, ps=ps, sq=sq: e.activation(out=sq[:, :n], in_=ps[:, :n], func=AF.Square),
                          reads=[pk], writes=[sqk])
                    stB[j] = (ps, pk, sq, sqk)

            def stage_b(j):
                if j not in stB:
                    return
                ps, pk, sq, sqk = stB.pop(j)
                kind = (j - 4) // 2
                px, pxk = pxr.next()
                ph.op('pe', lambda e, px=px, sq=sq: e.matmul(px[:, :n], s.blk64[:], sq[:, :n], start=True, stop=True),
                      reads=[sqk, 'blk64'], writes=[pxk])
                r, rk = rr.next()
                ph.op('act', lambda e, px=px, r=r: e.activation(out=r[:, :n], in_=px[:, :n], func=AF.Sqrt, bias=EPS, scale=1.0 / 64),
                      reads=[pxk], writes=[rk])
                ph.op('dve', lambda e, r=r: e.reciprocal(out=r[:, :n], in_=r[:, :n]), reads=[rk], writes=[rk])
                if not isx:
                    ph.op('dve', lambda e, ps=ps, r=r, j=j, fm=fm, kind=kind: e.scalar_tensor_tensor(
                        out=fm[:, j, :n], in0=ps[:, :n], scalar=s.gcol[:, kind:kind + 1], in1=r[:, :n],
                        op0=ALU.mult, op1=ALU.mult), reads=[pk, rk, 'gcol'], writes=[(fk, j)])
                else:
                    qn, qk = qnr.next()
                    ph.op('dve', lambda e, ps=ps, r=r, qn=qn, kind=kind: e.scalar_tensor_tensor(
                        out=qn[:, :n], in0=ps[:, :n], scalar=s.gcol[:, kind:kind + 1], in1=r[:, :n],
                        op0=ALU.mult, op1=ALU.mult), reads=[pk, rk, 'gcol'], writes=[qk])
                    stC[j] = (qn, qk)

            def stage_c(j):
                if j not in stC:
                    return
                qn, qk = stC.pop(j)
                px2, px2k = pxr.next()
                ph.op('pe', lambda e, px2=px2, qn=qn: e.matmul(px2[:, :n], s.prot[:], qn[:, :n], start=True, stop=True),
                      reads=[qk, 'prot'], writes=[px2k])
                t1, t1k = t1r.next()
                t2, t2k = t2r.next()
                ph.op('pool', lambda e, t1=t1, qn=qn: e.tensor_tensor(
                    out=t1[:, :n], in0=qn[:, :n], in1=cosT[:, x0:x0 + n], op=ALU.mult), reads=[qk, 'cos'], writes=[t1k])
                ph.op('dve', lambda e, t2=t2, px2=px2: e.tensor_tensor(
                    out=t2[:, :n], in0=px2[:, :n], in1=sinT[:, x0:x0 + n], op=ALU.mult), reads=[px2k, 'sin'], writes=[t2k])
                ph.op('pool', lambda e, t1=t1, t2=t2, fm=fm, j=j: e.tensor_tensor(
                    out=fm[:, j, :n], in0=t1[:, :n], in1=t2[:, :n], op=ALU.add), reads=[t1k, t2k], writes=[(fk, j)])

            for step in range(12 + 2):
                if step < 12:
                    stage_a(step)
                stage_b(step - 1)
                stage_c(step - 2)
            ph.op('sp', lambda e, fm=fm, t0=t0, n=n: e.dma_start(
                out=s.FM[:, :, t0:t0 + n].rearrange("j p t -> p j t"), in_=fm[:, :, :n]),
                reads=[(fk, j) for j in range(12)], writes=[('FM', t0)])
            for i in range(n // 128):
                tok, tkk = tokr.next()
                for g in range(3):
                    ps, pk = psr.next()
                    for k in range(KC):
                        ph.op('pe', lambda e, ps=ps, g=g, k=k, i=i, hT=hT: e.matmul(
                            ps[:], hT[:, k, i * 128:(i + 1) * 128], wtm[:, k, g * 512:(g + 1) * 512], start=(k == 0), stop=(k == KC - 1)),
                            reads=['wtm', hk], writes=[pk])
                    if g == 0:
                        ph.op('dve', lambda e, ps=ps, tok=tok: e.tensor_copy(out=tok[:, 0:512], in_=ps[:]),
                              reads=[pk], writes=[(tkk, 0)])
                    elif g == 1:
                        ph.op('act', lambda e, ps=ps, tok=tok: e.activation(out=tok[:, 512:1024], in_=ps[:], func=AF.Copy),
                              reads=[pk], writes=[(tkk, 1)])
                    else:
                        ph.op('act', lambda e, ps=ps, tok=tok: e.activation(out=tok[:, 1024:1536], in_=ps[:], func=AF.Silu),
                              reads=[pk], writes=[(tkk, 2)])
                ph.op('sp', lambda e, tok=tok, t0=t0, i=i: e.dma_start(out=s.TOK[t0 + i * 128:t0 + (i + 1) * 128, :], in_=tok[:]),
                      reads=[(tkk, 0), (tkk, 1), (tkk, 2)], writes=[('TOK', t0, i)])
        done(s, ph)


def phase_ret(s, l, b):
    T, NT = s.T, s.NT
    PS = s.PS
    with contextlib.ExitStack() as st:
        qt = sb(s, "rq", [128, 2, T], BF16, st)
        kt = sb(s, "rk", [128, 2, T], BF16, st)
        ktok = sb(s, "rktok", [128, NT, 256], BF16, st)
        v = sb(s, "rv", [128, NT, 512], BF16, st)
        sat = sb(s, "rsat", [128, 4, NT, 128], BF16, st)
        srun = sb(s, "rsrun", [128, 4, 128], F32, st)
        kzr = Rot([sb(s, f"rkz{i}", [128, 128], BF16, st) for i in range(3)], "rkz")
        attr = Rot([sb(s, f"ratt{i}", [128, 128], BF16, st) for i in range(3)], "ratt")
        qxr = Rot([sb(s, f"rqx{i}", [128, 2, 128], BF16, st) for i in range(2)], "rqx")
        gater = Rot([sb(s, f"rgate{i}", [128, 512], BF16, st) for i in range(2)], "rgate")
        ybr = Rot([sb(s, f"rybf{i}", [128, 512], BF16, st) for i in range(2)], "rybf")
        ytr = Rot([sb(s, f"ryt{i}", [128, 4, 128], BF16, st) for i in range(2)], "ryt")
        tmpr = Rot([sb(s, f"rtmp{i}", [128, 512], F32, st) for i in range(2)], "rtmp")
        junk = sb(s, "rjunk", [128, 128], F32, st)
        statr = Rot([sb(s, f"rstat{i}", [128, 12], F32, st) for i in range(2)], "rstat")
        ph = new_phase(s, "r")
        ph.op('sp', lambda e: e.dma_start(out=qt[:], in_=s.FM[0:2, :, :].rearrange("j p t -> p j t")), writes=['qt'])
        ph.op('sp', lambda e: e.dma_start(out=kt[:], in_=s.FM[2:4, :, :].rearrange("j p t -> p j t")), writes=['kt'])
        ph.op('sp', lambda e: e.dma_start(out=ktok[:], in_=s.TOK[:, 0:256].rearrange("(i p) c -> p i c", p=128)), writes=['ktok'])
        ph.op('sp', lambda e: e.dma_start(out=v[:], in_=s.TOK[:, 512:1024].rearrange("(i p) c -> p i c", p=128)), writes=['v'])
        ph.op('dve', lambda e: e.memset(srun[:], 0.0), writes=[('srun', i) for i in range(4)])
        psur = Rot(PS[0:2], "psu")
        orders = [list(range(NT)), [1, 0] + list(range(NT - 1, 1, -1))]
        for dr in range(2):
            for i in orders[dr]:
                for g in range(2):
                    idx = dr * 2 + g
                    kz, kzk = kzr.next()
                    ph.op('pool', lambda e, kz=kz, i=i, g=g, dr=dr: e.tensor_tensor(
                        out=kz[:], in0=ktok[:, i, g * 128:(g + 1) * 128], in1=s.zt[:, dr, g * 128:(g + 1) * 128], op=ALU.mult),
                        reads=['ktok', 'zt'], writes=[kzk])
                    pu, puk = psur.next()
                    for hh in range(2):
                        h = 2 * g + hh
                        ph.op('pe', lambda e, pu=pu, kz=kz, hh=hh, h=h, i=i: e.matmul(
                            pu[:, hh * 128:(hh + 1) * 128], kz[:], v[:, i, h * 128:(h + 1) * 128], start=True, stop=True),
                            reads=[kzk, 'v'], writes=[puk])
                    ph.op('act', lambda e, idx=idx, i=i: e.activation(out=sat[:, idx, i, :], in_=srun[:, idx, :], func=AF.Copy),
                          reads=[('srun', idx)], writes=[('sat', idx, i)])
                    for hh in range(2):
                        ph.op('dve', lambda e, pu=pu, hh=hh, idx=idx: e.scalar_tensor_tensor(
                            out=srun[hh * 64:(hh + 1) * 64, idx, :], in0=srun[hh * 64:(hh + 1) * 64, idx, :],
                            scalar=s.gcp[hh * 64:(hh + 1) * 64, idx:idx + 1],
                            in1=pu[hh * 64:(hh + 1) * 64, hh * 128:(hh + 1) * 128], op0=ALU.mult, op1=ALU.add),
                            reads=[puk, ('srun', idx), 'gcp'], writes=[('srun', idx)])
        pssr = Rot(PS[2:4], "pss")
        psor = Rot(PS[4:6], "pso")
        for i in range(NT):
            tc = slice(i * 128, (i + 1) * 128)
            po, pok = psor.next()
            for g in range(2):
                qx, qxk = qxr.next()
                for dr in range(2):
                    ph.op('pool', lambda e, qx=qx, g=g, dr=dr, tc=tc: e.tensor_tensor(
                        out=qx[:, dr, :], in0=qt[:, g, tc], in1=s.xit[:, dr * 2 + g, :], op=ALU.mult),
                        reads=['qt', 'xit'], writes=[qxk])
                for hh in range(2):
                    h = 2 * g + hh
                    rows = slice(hh * 64, (hh + 1) * 64)
                    pss, psk = pssr.next()
                    ph.op('pe', lambda e, pss=pss, rows=rows, g=g, tc=tc: e.matmul(
                        pss[:, 0:128], kt[rows, g, tc], qt[rows, g, tc], start=True, stop=True),
                        reads=['kt', 'qt'], writes=[psk])
                    att, atk = attr.next()
                    ph.op('dve', lambda e, pss=pss, att=att, h=h: e.tensor_tensor(
                        out=att[:], in0=pss[:, 0:128], in1=s.dsum[:, h, :], op=ALU.mult), reads=[psk, ('dsum', h)], writes=[atk])
                    oc = slice(h * 128, (h + 1) * 128)
                    ph.op('pe', lambda e, po=po, att=att, oc=oc, i=i: e.matmul(
                        po[:, oc], att[:], v[:, i, oc], start=True, stop=False), reads=[atk, 'v'], writes=[(pok, h)])
                    ph.op('pe', lambda e, po=po, qx=qx, rows=rows, oc=oc, g=g, i=i: e.matmul(
                        po[:, oc], qx[rows, 0, :], sat[rows, g, i, :], start=False, stop=False),
                        reads=[qxk, ('sat', g, i)], writes=[(pok, h)])
                    ph.op('pe', lambda e, po=po, qx=qx, rows=rows, oc=oc, g=g, i=i: e.matmul(
                        po[:, oc], qx[rows, 1, :], sat[rows, 2 + g, i, :], start=False, stop=True),
                        reads=[qxk, ('sat', 2 + g, i)], writes=[(pok, h)])
            pokeys = [(pok, h) for h in range(4)]
            stt_, stk = statr.next()
            ph.op('dve', lambda e, stt_=stt_: e.memset(stt_[:], 0.0), writes=[stk])
            ph.op('dve', lambda e, po=po, stt_=stt_: e.tensor_reduce(
                out=stt_[:, 0:4], in_=po[:].rearrange("p (h d) -> p h d", h=4), axis=AX.X, op=ALU.add),
                reads=pokeys + [stk], writes=[stk])
            ph.op('dve', lambda e, stt_=stt_: e.tensor_scalar(
                out=stt_[:, 0:4], in0=stt_[:, 0:4], scalar1=-1.0 / 128, scalar2=None, op0=ALU.mult), reads=[stk], writes=[stk])
            for h in range(4):
                ph.op('act', lambda e, po=po, stt_=stt_, h=h: e.activation(
                    out=junk[:], in_=po[:, h * 128:(h + 1) * 128], func=AF.Square, bias=stt_[:, h:h + 1], scale=1.0,
                    accum_out=stt_[:, 4 + h:5 + h]), reads=pokeys + [stk], writes=[stk, 'junk'])
            ph.op('act', lambda e, stt_=stt_: e.activation(
                out=stt_[:, 8:12], in_=stt_[:, 4:8], func=AF.Sqrt, bias=EPS, scale=1.0 / 128), reads=[stk], writes=[stk])
            ph.op('dve', lambda e, stt_=stt_: e.reciprocal(out=stt_[:, 8:12], in_=stt_[:, 8:12]), reads=[stk], writes=[stk])
            tmp, tmk = tmpr.next()
            for h in range(4):
                ph.op('dve', lambda e, po=po, stt_=stt_, tmp=tmp, h=h: e.tensor_scalar(
                    out=tmp[:, h * 128:(h + 1) * 128], in0=po[:, h * 128:(h + 1) * 128], scalar1=stt_[:, h:h + 1],
                    scalar2=stt_[:, 8 + h:9 + h], op0=ALU.add, op1=ALU.mult), reads=pokeys + [stk], writes=[tmk])
            gate, gk = gater.next()
            ph.op('sp', lambda e, gate=gate, tc=tc: e.dma_start(out=gate[:], in_=s.TOK[tc, 1024:1536]), writes=[gk])
            yb, ybk = ybr.next()
            ph.op('pool', lambda e, yb=yb, tmp=tmp, gate=gate: e.tensor_tensor(out=yb[:], in0=tmp[:], in1=gate[:], op=ALU.mult),
                  reads=[tmk, gk], writes=[ybk])
            for j in range(4):
                ph.op('pe', lambda e, yb=yb, j=j: e.transpose(
                    s.PSB[:, j * 128:(j + 1) * 128], yb[:, j * 128:(j + 1) * 128], s.ident_bf[:]),
                    reads=[ybk, 'ident_bf'], writes=['psb'])
            yt_, ytk = ytr.next()
            ph.op('act', lambda e, yt_=yt_: e.activation(
                out=yt_[:], in_=s.PSB[:, 0:512].rearrange("p (j t) -> p j t", j=4), func=AF.Copy), reads=['psb'], writes=[ytk])
            ph.op('sp', lambda e, yt_=yt_, tc=tc: e.dma_start(
                out=s.YT[0:4, :, tc].rearrange("j p t -> p j t"), in_=yt_[:]), reads=[ytk], writes=[('YT', i)])
        done(s, ph)


def phase_attn(s, l, b):
    T, NT = s.T, s.NT
    PS = s.PS
    for kind in (0, 1):
        with contextlib.ExitStack() as st:
            qc, kc, vcol, yrow = 4 + kind * 4, 6 + kind * 4, 256 + kind * 128, 4 + kind * 2
            q2 = sb(s, "aq2", [128, 2, T], BF16, st)
            k2 = sb(s, "ak2", [128, 2, T], BF16, st)
            vaug = sb(s, "avaug", [128, NT, 2, 65], BF16, st)
            wm = sb(s, "awm", [128, 6, 512], BF16, st)
            pr = Rot([sb(s, f"ap{i}", [128, 512], BF16, st) for i in range(3)], "ap")
            rden = sb(s, "arden", [128, 512], F32, st)
            osbr = Rot([sb(s, f"aosb{i}", [128, 512], F32, st) for i in range(2)], "aosb")
            ystr = Rot([sb(s, f"ayst{i}", [128, 512], BF16, st) for i in range(2)], "ayst")
            ph = new_phase(s, "w" if kind == 0 else "g")
            ph.op('sp', lambda e: e.dma_start(out=q2[:], in_=s.FM[qc:qc + 2, :, :].rearrange("j p t -> p j t")), writes=['q2'])
            ph.op('sp', lambda e: e.dma_start(out=k2[:], in_=s.FM[kc:kc + 2, :, :].rearrange("j p t -> p j t")), writes=['k2'])
            ph.op('dve', lambda e: e.memset(vaug[:], 1.0), writes=['vaug'])
            for gg in range(2):
                ph.op('sp', lambda e, gg=gg: e.dma_start(
                    out=vaug[:, :, gg, 0:64],
                    in_=s.TOK[:, vcol + gg * 64:vcol + (gg + 1) * 64].rearrange("(i p) d -> p i d", p=128)),
                    writes=['vaug'])
            if kind == 0:
                ph.op('pq', lambda e: e.dma_start(out=wm[:], in_=s.cst['wmask'].rearrange("o p q -> p o q")), writes=['wm'])
            pssr = Rot(PS[0:4], "pss")
            psor = Rot(PS[4:6], "pso")
            pending = []
            for (t0, n) in s.chunks:
                isx = t0 >= CTX
                qt0 = t0 // 128
                if not isx:
                    keytiles = [0, 1]
                elif kind == 1:
                    keytiles = list(range(NT))
                else:
                    keytiles = [0, 1] + [j for j in range(qt0 - 1, qt0 + 5) if 2 <= j < NT]
                for h in range(4):
                    g = h // 2
                    rows = slice((h % 2) * 64, (h % 2) * 64 + 64)
                    po, pok = psor.next()
                    nk = len(keytiles)
                    staged = []
                    LA = 2
                    for idx in range(nk + LA):
                        if idx < nk:
                            j = keytiles[idx]
                            pss, psk = pssr.next()
                            masked = (kind == 0) and isx and j >= 2
                            kcs = slice(j * 128, (j + 1) * 128)
                            ph.op('pe', lambda e, pss=pss, rows=rows, g=g, kcs=kcs, masked=masked: e.matmul(
                                pss[:, :n], k2[rows, g, kcs], q2[rows, g, t0:t0 + n], start=True, stop=(not masked)),
                                reads=['k2', 'q2'], writes=[psk])
                            if masked:
                                o = j - qt0 + 1
                                ph.op('pe', lambda e, pss=pss, o=o: e.matmul(
                                    pss[:, :n], s.ident_bf[:], wm[:, o, :n], start=False, stop=True),
                                    reads=['wm', 'ident_bf'], writes=[psk])
                            p_, pk_ = pr.next()
                            ph.op('act', lambda e, pss=pss, p_=p_: e.activation(out=p_[:, :n], in_=pss[:, :n], func=AF.Exp),
                                  reads=[psk], writes=[pk_])
                            staged.append((p_, pk_, j))
                        if idx == LA - 1 or (nk < LA and idx == nk - 1):
                            for fn_ in pending:
                                fn_()
                            pending.clear()
                        i2 = idx - LA
                        if i2 >= 0:
                            p2, pk2, j2 = staged[i2]
                            ph.op('pe', lambda e, po=po, p2=p2, j2=j2, g=g, i2=i2, nk=nk: e.matmul(
                                po[0:65, :n], vaug[:, j2, g, :], p2[:, :n], start=(i2 == 0), stop=(i2 == nk - 1)),
                                reads=['vaug', pk2], writes=[pok])
                    add = s.esink[64:65, h:h + 1] if kind == 0 else 0.0
                    ph.op('dve', lambda e, po=po, add=add: e.tensor_scalar(
                        out=rden[64:65, :n], in0=po[64:65, :n], scalar1=add, scalar2=None, op0=ALU.add),
                        reads=[pok, 'esink'], writes=['rden'])
                    ph.op('dve', lambda e: e.reciprocal(out=rden[64:65, :n], in_=rden[64:65, :n]), reads=['rden'], writes=['rden'])

                    def tail(po=po, pok=pok, g=g, rows=rows, h=h, t0=t0, n=n):
                        ph.op('pe', lambda e: e.matmul(PS[6][0:64, :n], s.ones_f[64:65, 0:64], rden[64:65, :n], start=True, stop=True),
                              reads=['rden', 'ones_f'], writes=[('ps', 6)])
                        osb, osk = osbr.next()
                        ph.op('act', lambda e, osb=osb: e.activation(out=osb[0:64, :n], in_=po[0:64, :n], func=AF.Copy),
                              reads=[pok], writes=[osk])
                        yst, ysk = ystr.next()
                        ph.op('dve', lambda e, osb=osb, yst=yst: e.tensor_tensor(
                            out=yst[0:64, :n], in0=osb[0:64, :n], in1=PS[6][0:64, :n], op=ALU.mult),
                            reads=[osk, ('ps', 6)], writes=[ysk])
                        ph.op('sp', lambda e, yst=yst: e.dma_start(
                            out=s.YT[yrow + g, rows, t0:t0 + n], in_=yst[0:64, :n]), reads=[ysk], writes=[('YT', kind, h, t0)])

                    pending.append(tail)
            for fn_ in pending:
                fn_()
            pending.clear()
            done(s, ph)


def phase_out(s, l, b):
    T, NB = s.T, s.NB
    PS = s.PS
    with contextlib.ExitStack() as st:
        wo = sb(s, "owo", [128, KC, D], BF16, st)
        ytr = Rot([sb(s, f"oyt{i}", [128, KC, 512], BF16, st) for i in range(2)], "oyt")
        xsr = Rot([sb(s, f"oxs{i}", [128, KC, 512], F32, st) for i in range(2)], "oxs")
        x2r = Rot([sb(s, f"ox2{i}", [128, KC, 512], F32, st) for i in range(2)], "ox2")
        hTr = Rot([sb(s, f"ohT{i}", [128, KC, 512], BF16, st) for i in range(2)], "ohT")
        sqr = Rot([sb(s, f"osq{i}", [128, 512], BF16, st) for i in range(2)], "osq")
        rstd = sb(s, "orstd", [128, 512], F32, st)
        tmpr = Rot([sb(s, f"otmp{i}", [128, 512], F32, st) for i in range(2)], "otmp")
        rtr = Rot([sb(s, f"ort{i}", [128, 8, 16], F32, st) for i in range(2)], "ort")
        hrr = Rot([sb(s, f"ohr{i}", [128, D], BF16, st) for i in range(2)], "ohr")
        psr = Rot(PS[0:4], "ps")
        ph = new_phase(s, "o")
        ph.op('pq', lambda e: e.dma_start(out=wo[:], in_=s.w_out[l].rearrange("(k p) n -> p k n", p=128)), writes=['wo'])
        for (t0, n) in s.chunks:
            isx = t0 >= CTX
            bcol = b if isx else NB
            yt_, ytk = ytr.next()
            ph.op('sp', lambda e, yt_=yt_, t0=t0, n=n: e.dma_start(
                out=yt_[:, :, :n], in_=s.YT[:, :, t0:t0 + n].rearrange("k p t -> p k t")), writes=[ytk])
            xs, xk = xsr.next()
            ph.op('sp', lambda e, xs=xs, t0=t0, n=n: e.dma_start(
                out=xs[:, :, :n], in_=s.XT[b, :, :, t0:t0 + n].rearrange("k p t -> p k t")), writes=[xk])
            x2, x2k = x2r.next()
            for nn in range(KC):
                ps, pk = psr.next()
                for k in range(KC):
                    ph.op('pe', lambda e, ps=ps, nn=nn, k=k, yt_=yt_: e.matmul(
                        ps[:, :n], wo[:, k, nn * 128:(nn + 1) * 128], yt_[:, k, :n], start=(k == 0), stop=(k == KC - 1)),
                        reads=['wo', ytk], writes=[pk])
                ph.op('dve', lambda e, ps=ps, nn=nn, x2=x2, xs=xs: e.scalar_tensor_tensor(
                    out=x2[:, nn, :n], in0=ps[:, :n], scalar=s.modT[:, l, 16 + nn, bcol:bcol + 1], in1=xs[:, nn, :n],
                    op0=ALU.mult, op1=ALU.add), reads=[pk, xk], writes=[x2k])
            ph.op('sp', lambda e, x2=x2, t0=t0, n=n: e.dma_start(
                out=s.XT[b, :, :, t0:t0 + n].rearrange("k p t -> p k t"), in_=x2[:, :, :n]), reads=[x2k], writes=[('XT', t0)])
            hT, hk = hTr.next()
            norm_mod(s, ph, x2, x2k, n, s.gs2, 24, l, bcol, hT, hk, sqr, rstd, tmpr)
            for i in range(n // 128):
                ps, pk = psr.next()
                for k in range(KC):
                    ph.op('pe', lambda e, ps=ps, k=k, i=i, hT=hT: e.matmul(
                        ps[:, 0:NE], hT[:, k, i * 128:(i + 1) * 128], s.wr_bf[:, k, :], start=(k == 0), stop=(k == KC - 1)),
                        reads=[hk, 'wr'], writes=[pk])
                rt, rk = rtr.next()
                R = lambda j: rt[:, j, :]
                G = lambda j: rt[:, j, :].rearrange("p (g e) -> p g e", g=4)
                ph.op('act', lambda e, ps=ps, rt=rt: e.activation(out=rt[:, 0, :], in_=ps[:, 0:NE], func=AF.Sigmoid),
                      reads=[pk], writes=[rk])
                seqops = [
                    lambda e, rt=rt: e.tensor_tensor(out=rt[:, 1, :], in0=rt[:, 0, :], in1=s.br_bc[:], op=ALU.add),
                    lambda e, rt=rt: e.tensor_reduce(out=rt[:, 2, 0:4], in_=rt[:, 1, :].rearrange("p (g e) -> p g e", g=4),
                                                     axis=AX.X, op=ALU.max),
                    lambda e, rt=rt: e.tensor_tensor(out=rt[:, 3, :].rearrange("p (g e) -> p g e", g=4),
                                                     in0=rt[:, 1, :].rearrange("p (g e) -> p g e", g=4),
                                                     in1=rt[:, 2, 0:4].unsqueeze(2).broadcast_to([128, 4, 4]), op=ALU.is_equal),
                    lambda e, rt=rt: e.scalar_tensor_tensor(out=rt[:, 3, :], in0=rt[:, 3, :], scalar=-1e9, in1=rt[:, 1, :],
                                                            op0=ALU.mult, op1=ALU.add),
                    lambda e, rt=rt: e.tensor_reduce(out=rt[:, 2, 4:8], in_=rt[:, 3, :].rearrange("p (g e) -> p g e", g=4),
                                                     axis=AX.X, op=ALU.max),
                    lambda e, rt=rt: e.tensor_tensor(out=rt[:, 2, 8:12], in0=rt[:, 2, 0:4], in1=rt[:, 2, 4:8], op=ALU.add),
                    lambda e, rt=rt: e.tensor_reduce(out=rt[:, 2, 12:13], in_=rt[:, 2, 8:12], axis=AX.X, op=ALU.max),
                    lambda e, rt=rt: e.tensor_scalar(out=rt[:, 4, 0:4], in0=rt[:, 2, 8:12], scalar1=rt[:, 2, 12:13], scalar2=None,
                                                     op0=ALU.is_ge),
                    lambda e, rt=rt: e.tensor_tensor(out=rt[:, 5, :].rearrange("p (g e) -> p g e", g=4),
                                                     in0=rt[:, 1, :].rearrange("p (g e) -> p g e", g=4),
                                                     in1=rt[:, 2, 4:8].unsqueeze(2).broadcast_to([128, 4, 4]), op=ALU.is_ge),
                    lambda e, rt=rt: e.tensor_tensor(out=rt[:, 5, :].rearrange("p (g e) -> p g e", g=4),
                                                     in0=rt[:, 5, :].rearrange("p (g e) -> p g e", g=4),
                                                     in1=rt[:, 4, 0:4].unsqueeze(2).broadcast_to([128, 4, 4]), op=ALU.mult),
                    lambda e, rt=rt: e.tensor_tensor(out=rt[:, 6, :], in0=rt[:, 5, :], in1=rt[:, 0, :], op=ALU.mult),
                    lambda e, rt=rt: e.tensor_reduce(out=rt[:, 4, 4:5], in_=rt[:, 6, :], axis=AX.X, op=ALU.add),
                    lambda e, rt=rt: e.reciprocal(out=rt[:, 4, 4:5], in_=rt[:, 4, 4:5]),
                    lambda e, rt=rt: e.tensor_scalar(out=rt[:, 7, :], in0=rt[:, 6, :], scalar1=rt[:, 4, 4:5], scalar2=None,
                                                     op0=ALU.mult),
                ]
                for fn in seqops:
                    ph.op('dve', fn, reads=[rk], writes=[rk])
                ti = b * s.NT + t0 // 128 + i
                ph.op('dve', lambda e, rt=rt, ti=ti: e.tensor_copy(out=s.SELt[:, ti, :], in_=rt[:, 5, :]), reads=[rk], writes=[('selt', ti)])
                ph.op('dve', lambda e, rt=rt, ti=ti: e.tensor_copy(out=s.WGt[:, ti, :], in_=rt[:, 7, :]), reads=[rk], writes=[('wgt', ti)])
                pt, ptk = psr.next()
                ph.op('pe', lambda e, pt=pt, rt=rt: e.matmul(pt[:, 0:NE], s.ustrict[:], rt[:, 5, :], start=True, stop=True),
                      reads=[rk, 'ustrict'], writes=[ptk])
                ph.op('pe', lambda e, pt=pt, rt=rt: e.matmul(pt[:, NE:2 * NE], s.ones_f[:], rt[:, 5, :], start=True, stop=True),
                      reads=[rk, 'ones_f'], writes=[ptk])
                ph.op('dve', lambda e, pt=pt, ti=ti: e.tensor_tensor(out=s.RKt[:, ti, :], in0=pt[:, 0:NE], in1=s.cbase[:], op=ALU.add),
                      reads=[ptk, 'cbase'], writes=[('rkt', ti)])
                ph.op('dve', lambda e, pt=pt: e.tensor_tensor(out=s.cbase[:], in0=pt[:, NE:2 * NE], in1=s.cbase[:], op=ALU.add),
                      reads=[ptk, 'cbase'], writes=['cbase'])
                for c in range(KC):
                    ph.op('pe', lambda e, hT=hT, c=c, i=i: e.transpose(
                        s.PSB[:, c * 128:(c + 1) * 128], hT[:, c, i * 128:(i + 1) * 128], s.ident_bf[:]),
                        reads=[hk, 'ident_bf'], writes=['psb'])
                hr, hrk = hrr.next()
                ph.op('act', lambda e, hr=hr: e.activation(out=hr[:], in_=s.PSB[:], func=AF.Copy), reads=['psb'], writes=[hrk])
                gt = b * T + t0 + i * 128
                ph.op('sp', lambda e, hr=hr, gt=gt: e.dma_start(out=s.H2TOK[gt:gt + 128, :], in_=hr[:]), reads=[hrk], writes=[('H2TOK', gt)])
        done(s, ph)


def phase_moe(s, l):
    T, NB, DEPTH = s.T, s.NB, s.DEPTH
    PS = s.PS
    supers = []
    for b in range(NB):
        cur, tot = [], 0
        for (t0, n) in s.chunks:
            if tot + n > 1280:
                supers.append((b, cur))
                cur, tot = [], 0
            cur.append((t0, n))
            tot += n
        if cur:
            supers.append((b, cur))
    with contextlib.ExitStack() as st:
        acc = sb(s, "macc", [128, KC, 1280], F32, st)
        wgr = Rot([sb(s, f"mwg{i}", [128, KC, 2 * D], BF16, st) for i in range(2)], "mwg")
        wdr = Rot([sb(s, f"mwd{i}", [128, KC, D], BF16, st) for i in range(1)], "mwd")
        h2r = Rot([sb(s, f"mh2{i}", [128, KC, 512], BF16, st) for i in range(2)], "mh2")
        wtr = Rot([sb(s, f"mwt{i}", [16, 512], F32, st) for i in range(2)], "mwt")
        wbr = Rot([sb(s, f"mwb{i}", [128, 512], F32, st) for i in range(2)], "mwb")
        sar = Rot([sb(s, f"msa{i}", [128, 512], F32, st) for i in range(2)], "msa")
        ttr = Rot([sb(s, f"mtt{i}", [128, 512], F32, st) for i in range(2)], "mtt")
        aTr = Rot([sb(s, f"maT{i}", [128, KC, 512], BF16, st) for i in range(1)], "maT")
        xsr = Rot([sb(s, f"mxs{i}", [128, KC, 128], F32, st) for i in range(2)], "mxs")
        otr = Rot([sb(s, f"mot{i}", [128, D], F32, st) for i in range(2)], "mot")
        psr = Rot(PS[0:6], "ps")
        for (b, chs) in supers:
            ph = new_phase(s, "m")
            for ex in range(NE):
                wg, wgk = wgr.next()
                wd, wdk = wdr.next()
                for hf in range(2):
                    ph.op('pq', lambda e, wg=wg, ex=ex, hf=hf: e.dma_start(
                        out=wg[:, :, hf * D:(hf + 1) * D],
                        in_=s.w_gu[l, ex].rearrange("(k p) n -> p k n", p=128)[:, :, hf * D:(hf + 1) * D]), writes=[wgk])
                ph.op('pq', lambda e, wd=wd, ex=ex: e.dma_start(
                    out=wd[:], in_=s.w_dn[l, ex].rearrange("(k p) n -> p k n", p=128)), writes=[wdk])
                off = 0
                for (t0, n) in chs:
                    gt = b * T + t0
                    h2, h2k = h2r.next()
                    ph.op('sp', lambda e, h2=h2, gt=gt, n=n: e.dma_start(
                        out=h2[:, :, :n], in_=s.H2T[:, :, gt:gt + n].rearrange("k p t -> p k t")), writes=[h2k])
                    wt, wtk = wtr.next()
                    ph.op('sp', lambda e, wt=wt, gt=gt, n=n: e.dma_start(out=wt[0:NE, :n], in_=s.WGT[:, gt:gt + n]), writes=[wtk])
                    ph.op('pe', lambda e, wt=wt, ex=ex, n=n: e.matmul(
                        PS[6][:, :n], s.sel_sb[0:NE, ex * 128:(ex + 1) * 128], wt[0:NE, :n], start=True, stop=True),
                        reads=[wtk, 'sel'], writes=[('ps', 6)])
                    wb, wbk = wbr.next()
                    ph.op('act', lambda e, wb=wb, n=n: e.activation(out=wb[:, :n], in_=PS[6][:, :n], func=AF.Copy),
                          reads=[('ps', 6)], writes=[wbk])
                    aT, aTk = aTr.next()
                    for f in range(KC):
                        pa, pak = psr.next()
                        pu, puk = psr.next()
                        for k in range(KC):
                            ph.op('pe', lambda e, pa=pa, wg=wg, h2=h2, f=f, k=k, n=n: e.matmul(
                                pa[:, :n], wg[:, k, f * 128:(f + 1) * 128], h2[:, k, :n], start=(k == 0), stop=(k == KC - 1)),
                                reads=[wgk, h2k], writes=[pak])
                        for k in range(KC):
                            ph.op('pe', lambda e, pu=pu, wg=wg, h2=h2, f=f, k=k, n=n: e.matmul(
                                pu[:, :n], wg[:, k, D + f * 128:D + (f + 1) * 128], h2[:, k, :n], start=(k == 0), stop=(k == KC - 1)),
                                reads=[wgk, h2k], writes=[puk])
                        sa, sak = sar.next()
                        ph.op('act', lambda e, pa=pa, sa=sa, n=n: e.activation(out=sa[:, :n], in_=pa[:, :n], func=AF.Silu),
                              reads=[pak], writes=[sak])
                        tt, ttk = ttr.next()
                        ph.op('dve', lambda e, pu=pu, sa=sa, tt=tt, n=n: e.tensor_tensor(
                            out=tt[:, :n], in0=pu[:, :n], in1=sa[:, :n], op=ALU.mult), reads=[puk, sak], writes=[ttk])
                        ph.op('pool', lambda e, tt=tt, wb=wb, aT=aT, f=f, n=n: e.tensor_tensor(
                            out=aT[:, f, :n], in0=tt[:, :n], in1=wb[:, :n], op=ALU.mult), reads=[ttk, wbk], writes=[(aTk, f)])
                    for nn in range(KC):
                        py, pyk = psr.next()
                        for f in range(KC):
                            ph.op('pe', lambda e, py=py, wd=wd, aT=aT, f=f, nn=nn, n=n: e.matmul(
                                py[:, :n], wd[:, f, nn * 128:(nn + 1) * 128], aT[:, f, :n], start=(f == 0), stop=(f == KC - 1)),
                                reads=[wdk, (aTk, f)], writes=[pyk])
                        if ex == 0:
                            ph.op('dve', lambda e, py=py, nn=nn, off=off, n=n: e.tensor_copy(
                                out=acc[:, nn, off:off + n], in_=py[:, :n]), reads=[pyk], writes=[('acc', nn, off)])
                        else:
                            ph.op('dve', lambda e, py=py, nn=nn, off=off, n=n: e.tensor_tensor(
                                out=acc[:, nn, off:off + n], in0=acc[:, nn, off:off + n], in1=py[:, :n], op=ALU.add),
                                reads=[pyk, ('acc', nn, off)], writes=[('acc', nn, off)])
                    off += n
            off = 0
            for (t0, n) in chs:
                isx = t0 >= CTX
                bcol = b if isx else NB
                for i in range(n // 128):
                    tt0 = t0 + i * 128
                    xs, xk = xsr.next()
                    ph.op('sp', lambda e, xs=xs, tt0=tt0: e.dma_start(
                        out=xs[:], in_=s.XT[b, :, :, tt0:tt0 + 128].rearrange("k p t -> p k t")), writes=[xk])
                    for nn in range(KC):
                        ph.op('dve', lambda e, xs=xs, nn=nn, o=off + i * 128: e.scalar_tensor_tensor(
                            out=xs[:, nn, :], in0=acc[:, nn, o:o + 128], scalar=s.modT[:, l, 40 + nn, bcol:bcol + 1],
                            in1=xs[:, nn, :], op0=ALU.mult, op1=ALU.add),
                            reads=[xk] + [('acc', nn, oo) for oo in set([off])], writes=[xk])
                    if l < DEPTH - 1:
                        ph.op('sp', lambda e, xs=xs, tt0=tt0: e.dma_start(
                            out=s.XT[b, :, :, tt0:tt0 + 128].rearrange("k p t -> p k t"), in_=xs[:]), reads=[xk], writes=[('XTo', tt0)])
                    else:
                        ot, otk = otr.next()
                        for half in range(2):
                            pt, ptk = psr.next()
                            for j in range(4):
                                c = half * 4 + j
                                ph.op('pe', lambda e, pt=pt, xs=xs, j=j, c=c: e.transpose(
                                    pt[:, j * 128:(j + 1) * 128], xs[:, c, :], s.ident[:]), reads=[xk, 'ident'], writes=[ptk])
                            if half == 0:
                                ph.op('act', lambda e, pt=pt, ot=ot: e.activation(out=ot[:, 0:512], in_=pt[:], func=AF.Copy),
                                      reads=[ptk], writes=[(otk, 0)])
                            else:
                                ph.op('dve', lambda e, pt=pt, ot=ot: e.tensor_copy(out=ot[:, 512:1024], in_=pt[:]),
                                      reads=[ptk], writes=[(otk, 1)])
                        dst = s.out_x[b, tt0 - CTX:tt0 - CTX + 128, :] if isx else s.out_c[b, tt0:tt0 + 128, :]
                        ph.op('sp', lambda e, ot=ot, dst=dst: e.dma_start(out=dst, in_=ot[:]),
                              reads=[(otk, 0), (otk, 1)], writes=[('out', tt0)])
                off += n
            done(s, ph)


def phase_dest(s, l):
    NTA, NBLK = s.NTA, s.NBLK
    with contextlib.ExitStack() as st:
        r_ = sb(s, "dr", [128, NE], F32, st)
        g_ = sb(s, "dg", [128, NE], F32, st)
        pad = sb(s, "dpad", [128, NE], F32, st)
        pst_ = sb(s, "dpst", [128, NE], F32, st)
        pend = sb(s, "dpend", [128, NE], F32, st)
        t1r = Rot([sb(s, f"dt1{i}", [128, NE], F32, st) for i in range(2)], "dt1")
        t2r = Rot([sb(s, f"dt2{i}", [128, NE], F32, st) for i in range(2)], "dt2")
        dstr = Rot([sb(s, f"ddst{i}", [128, NE], F32, st) for i in range(2)], "ddst")
        m1r = Rot([sb(s, f"dm1{i}", [128, 2], F32, st) for i in range(2)], "dm1")
        eacc = sb(s, "deacc", [128, 64], F32, st)
        hrr = Rot([sb(s, f"dhr{i}", [128, D], BF16, st) for i in range(3)], "dhr")
        ph = new_phase(s, "d")
        ki = sb(s, "dki", [128, NE], I32, st)
        ph.op('dve', lambda e: e.tensor_scalar(out=r_[:], in0=s.cbase[:], scalar1=float(MB - 1), scalar2=1.0 / MB, op0=ALU.add, op1=ALU.mult),
              writes=['r'])
        ph.op('dve', lambda e: e.tensor_copy(out=ki[:], in_=r_[:]), reads=['r'], writes=['ki'])
        ph.op('dve', lambda e: e.tensor_copy(out=g_[:], in_=ki[:]), reads=['ki'], writes=['g'])
        ph.op('dve', lambda e: e.tensor_tensor(out=pad[:], in0=g_[:], in1=r_[:], op=ALU.is_gt), reads=['g', 'r'], writes=['pad'])
        ph.op('dve', lambda e: e.tensor_tensor(out=g_[:], in0=g_[:], in1=pad[:], op=ALU.subtract), reads=['g', 'pad'], writes=['g'])
        ph.op('dve', lambda e: e.tensor_scalar(out=pad[:], in0=g_[:], scalar1=float(MB), scalar2=None, op0=ALU.mult), reads=['g'], writes=['pad'])
        ph.op('dve', lambda e: e.memset(pst_[:], 0.0), writes=['pst'])
        for ex in range(1, NE):
            ph.op('dve', lambda e, ex=ex: e.tensor_tensor(out=pst_[:, ex:ex + 1], in0=pst_[:, ex - 1:ex], in1=pad[:, ex - 1:ex], op=ALU.add),
                  reads=['pst', 'pad'], writes=['pst'])
        ph.op('dve', lambda e: e.tensor_tensor(out=pend[:], in0=pst_[:], in1=pad[:], op=ALU.add), reads=['pst', 'pad'], writes=['pend'])
        for ti in range(NTA):
            dst, dk = dstr.next()
            ph.op('dve', lambda e, dst=dst, ti=ti: e.tensor_tensor(out=dst[:], in0=s.RKt[:, ti, :], in1=pst_[:], op=ALU.add),
                  reads=['pst'], writes=[dk])
            t1, t1k = t1r.next()
            ph.op('dve', lambda e, dst=dst, t1=t1, ti=ti: e.scalar_tensor_tensor(
                out=t1[:], in0=dst[:], scalar=1.0, in1=s.SELt[:, ti, :], op0=ALU.add, op1=ALU.mult), reads=[dk], writes=[t1k])
            m1, m1k = m1r.next()
            ph.op('dve', lambda e, t1=t1, m1=m1: e.tensor_reduce(out=m1[:, 0:1], in_=t1[:], axis=AX.X, op=ALU.max), reads=[t1k], writes=[m1k])
            t2, t2k = t2r.next()
            ph.op('dve', lambda e, dst=dst, t2=t2, ti=ti: e.scalar_tensor_tensor(
                out=t2[:], in0=s.SELt[:, ti, :], scalar=-1.0e6, in1=dst[:], op0=ALU.mult, op1=ALU.add), reads=[dk], writes=[t2k])
            ph.op('dve', lambda e, t2=t2, m1=m1: e.tensor_reduce(out=m1[:, 1:2], in_=t2[:], axis=AX.X, op=ALU.min), reads=[t2k, m1k], writes=[m1k])
            ph.op('dve', lambda e, m1=m1, ti=ti: e.tensor_scalar(out=s.DAf[:, ti:ti + 1], in0=m1[:, 0:1], scalar1=-1.0, scalar2=None, op0=ALU.add),
                  reads=[m1k], writes=['daf'])
            ph.op('dve', lambda e, m1=m1, ti=ti: e.tensor_scalar(out=s.DBf[:, ti:ti + 1], in0=m1[:, 1:2], scalar1=1.0e6, scalar2=None, op0=ALU.add),
                  reads=[m1k], writes=['dbf'])
            ph.op('dve', lambda e, t1=t1, m1=m1: e.tensor_scalar(out=t1[:], in0=t1[:], scalar1=m1[:, 0:1], scalar2=None, op0=ALU.is_equal),
                  reads=[t1k, m1k], writes=[t1k])
            ph.op('dve', lambda e, t1=t1, ti=ti: e.tensor_tensor(out=t1[:], in0=t1[:], in1=s.WGt[:, ti, :], op=ALU.mult), reads=[t1k], writes=[t1k])
            ph.op('dve', lambda e, t1=t1, ti=ti: e.tensor_reduce(out=s.WA[:, ti:ti + 1], in_=t1[:], axis=AX.X, op=ALU.add), reads=[t1k], writes=['wa'])
        ph.op('dve', lambda e: e.tensor_scalar(out=s.WB[:], in0=s.WA[:], scalar1=-1.0, scalar2=1.0, op0=ALU.mult, op1=ALU.add),
              reads=['wa'], writes=['wb'])
        ph.op('dve', lambda e: e.tensor_copy(out=s.DAi[:], in_=s.DAf[:]), reads=['daf'], writes=['dai'])
        ph.op('dve', lambda e: e.tensor_copy(out=s.DBi[:], in_=s.DBf[:]), reads=['dbf'], writes=['dbi'])
        ph.op('dve', lambda e: e.memset(eacc[:], 0.0), writes=['eacc'])
        for ex in range(NE):
            ph.op('dve', lambda e, ex=ex: e.scalar_tensor_tensor(
                out=eacc[:], in0=s.blkiota[:], scalar=pend[:, ex:ex + 1], in1=eacc[:], op0=ALU.is_ge, op1=ALU.add),
                reads=['pend', 'eacc'], writes=['eacc'])
        ph.op('dve', lambda e: e.tensor_scalar(out=eacc[:], in0=eacc[:], scalar1=float(NE - 1), scalar2=128.0, op0=ALU.min, op1=ALU.mult),
              reads=['eacc'], writes=['eacc'])
        ph.op('dve', lambda e: e.tensor_scalar(out=eacc[:], in0=eacc[:], scalar1=s.pidx[:, 0:1], scalar2=None, op0=ALU.add),
              reads=['eacc'], writes=['eacc'])
        ph.op('dve', lambda e: e.tensor_copy(out=s.IDXi[:], in_=eacc[:]), reads=['eacc'], writes=['idxi'])
        for ti in range(NTA):
            hr, hrk = hrr.next()
            ph.op('sp', lambda e, hr=hr, ti=ti: e.dma_start(out=hr[:], in_=s.H2TOK[ti * 128:(ti + 1) * 128, :]), writes=[hrk])
            for (dd, dn) in ((s.DAi, 'dai'), (s.DBi, 'dbi')):
                ph.op('pq', lambda e, hr=hr, ti=ti, dd=dd: e.indirect_dma_start(
                    out=s.XP, out_offset=bass.IndirectOffsetOnAxis(ap=dd[:, ti:ti + 1], axis=0), in_=hr[:, :], in_offset=None),
                    reads=[hrk, dn], writes=[('XPs', ti, dn)])
        done(s, ph)


def phase_moe_sparse(s, l):
    NBLK = s.NBLK
    PS = s.PS
    with contextlib.ExitStack() as st:
        wgr = Rot([sb(s, f"swg{i}", [128, KC, 2 * D], BF16, st) for i in range(2)], "swg")
        wdr = Rot([sb(s, f"swd{i}", [128, KC, D], BF16, st) for i in range(2)], "swd")
        xrr = Rot([sb(s, f"sxr{i}", [128, D], BF16, st) for i in range(4)], "sxr")
        xpr = Rot([sb(s, f"sxp{i}", [128, KC, MB], BF16, st) for i in range(2)], "sxp")
        sar = Rot([sb(s, f"ssa{i}", [128, MB], F32, st) for i in range(2)], "ssa")
        aTr = Rot([sb(s, f"saT{i}", [128, KC, MB], BF16, st) for i in range(2)], "saT")
        ypr = Rot([sb(s, f"syp{i}", [128, D], BF16, st) for i in range(2)], "syp")
        psr = Rot(PS[0:7], "ps")
        ph = new_phase(s, "s")
        for j in range(NBLK):
            wg, wgk = wgr.next()
            wd, wdk = wdr.next()
            ph.op('pq', lambda e, wg=wg, j=j: e.indirect_dma_start(
                out=wg[:].rearrange("p k n -> p (k n)"), out_offset=None, in_=s.WBgu[l % 2].rearrange("e p k n -> (e p) (k n)"),
                in_offset=bass.IndirectOffsetOnAxis(ap=s.IDXi[:, j:j + 1], axis=0)), writes=[wgk])
            ph.op('pq', lambda e, wd=wd, j=j: e.indirect_dma_start(
                out=wd[:].rearrange("p k n -> p (k n)"), out_offset=None, in_=s.WBdn[l % 2].rearrange("e p k n -> (e p) (k n)"),
                in_offset=bass.IndirectOffsetOnAxis(ap=s.IDXi[:, j:j + 1], axis=0)), writes=[wdk])
            if l + 1 < s.DEPTH:
                per = -(-2 * NE // NBLK)
                for i in range(j * per, min(2 * NE, (j + 1) * per)):
                    stage_weight(s, ph, l + 1, i)
            xp, xpk = xpr.next()
            for sub in range(MB // 128):
                xr, xrk = xrr.next()
                r0 = j * MB + sub * 128
                ph.op('sp', lambda e, xr=xr, r0=r0: e.dma_start(out=xr[:], in_=s.XP[r0:r0 + 128, :]), writes=[xrk])
                for c in range(KC):
                    ph.op('pe', lambda e, xr=xr, c=c: e.transpose(
                        s.PSB[:, c * 128:(c + 1) * 128], xr[:, c * 128:(c + 1) * 128], s.ident_bf[:]),
                        reads=[xrk, 'ident_bf'], writes=['psb'])
                if sub % 2 == 0:
                    ph.op('act', lambda e, xp=xp, sub=sub: e.activation(
                        out=xp[:, :, sub * 128:(sub + 1) * 128], in_=s.PSB[:].rearrange("p (c t) -> p c t", c=KC), func=AF.Copy),
                        reads=['psb'], writes=[(xpk, sub)])
                else:
                    ph.op('dve', lambda e, xp=xp, sub=sub: e.tensor_copy(
                        out=xp[:, :, sub * 128:(sub + 1) * 128], in_=s.PSB[:].rearrange("p (c t) -> p c t", c=KC)),
                        reads=['psb'], writes=[(xpk, sub)])
            xpkeys = [(xpk, sub) for sub in range(MB // 128)]
            aT, aTk = aTr.next()
            for f in range(KC):
                pa, pak = psr.next()
                pu, puk = psr.next()
                for k in range(KC):
                    ph.op('pe', lambda e, pa=pa, wg=wg, xp=xp, f=f, k=k: e.matmul(
                        pa[:], wg[:, k, f * 128:(f + 1) * 128], xp[:, k, :], start=(k == 0), stop=(k == KC - 1)),
                        reads=[wgk] + xpkeys, writes=[pak])
                for k in range(KC):
                    ph.op('pe', lambda e, pu=pu, wg=wg, xp=xp, f=f, k=k: e.matmul(
                        pu[:], wg[:, k, D + f * 128:D + (f + 1) * 128], xp[:, k, :], start=(k == 0), stop=(k == KC - 1)),
                        reads=[wgk] + xpkeys, writes=[puk])
                sa, sak = sar.next()
                ph.op('act', lambda e, pa=pa, sa=sa: e.activation(out=sa[:], in_=pa[:], func=AF.Silu), reads=[pak], writes=[sak])
                ph.op('dve', lambda e, pu=pu, sa=sa, aT=aT, f=f: e.tensor_tensor(out=aT[:, f, :], in0=pu[:], in1=sa[:], op=ALU.mult),
                      reads=[puk, sak], writes=[(aTk, f)])
            aTkeys = [(aTk, f) for f in range(KC)]
            for sub in range(MB // 128):
                yp, ypk = ypr.next()
                for nh in range(2):
                    py, pyk = psr.next()
                    for f in range(KC):
                        ph.op('pe', lambda e, py=py, wd=wd, aT=aT, f=f, nh=nh, sub=sub: e.matmul(
                            py[:], aT[:, f, sub * 128:(sub + 1) * 128], wd[:, f, nh * 512:(nh + 1) * 512], start=(f == 0), stop=(f == KC - 1)),
                            reads=[wdk] + aTkeys, writes=[pyk])
                    if nh == 0:
                        ph.op('act', lambda e, py=py, yp=yp: e.activation(out=yp[:, 0:512], in_=py[:], func=AF.Copy), reads=[pyk], writes=[(ypk, 0)])
                    else:
                        ph.op('dve', lambda e, py=py, yp=yp: e.tensor_copy(out=yp[:, 512:1024], in_=py[:]), reads=[pyk], writes=[(ypk, 1)])
                r0 = j * MB + sub * 128
                ph.op('sp', lambda e, yp=yp, r0=r0: e.dma_start(out=s.YP[r0:r0 + 128, :], in_=yp[:]), reads=[(ypk, 0), (ypk, 1)], writes=[('YP', r0)])
        done(s, ph)


def phase_comb(s, l):
    T, NB, NT, DEPTH = s.T, s.NB, s.NT, s.DEPTH
    PS = s.PS
    with contextlib.ExitStack() as st:
        yar = Rot([sb(s, f"cya{i}", [128, D], BF16, st) for i in range(3)], "cya")
        ybr = Rot([sb(s, f"cyb{i}", [128, D], BF16, st) for i in range(3)], "cyb")
        ysr = Rot([sb(s, f"cys{i}", [128, D], F32, st) for i in range(2)], "cys")
        xsr = Rot([sb(s, f"cxs{i}", [128, KC, 128], F32, st) for i in range(2)], "cxs")
        otr = Rot([sb(s, f"cot{i}", [128, D], F32, st) for i in range(2)], "cot")
        psr = Rot(PS[0:6], "ps")
        ph = new_phase(s, "k")
        for b in range(NB):
            for i in range(NT):
                ti = b * NT + i
                tt0 = i * 128
                isx = tt0 >= CTX
                bcol = b if isx else NB
                ya, yak = yar.next()
                yb, ybk = ybr.next()
                ph.op('pq', lambda e, ya=ya, ti=ti: e.indirect_dma_start(
                    out=ya[:, :], out_offset=None, in_=s.YP, in_offset=bass.IndirectOffsetOnAxis(ap=s.DAi[:, ti:ti + 1], axis=0)), writes=[yak])
                ph.op('pq', lambda e, yb=yb, ti=ti: e.indirect_dma_start(
                    out=yb[:, :], out_offset=None, in_=s.YP, in_offset=bass.IndirectOffsetOnAxis(ap=s.DBi[:, ti:ti + 1], axis=0)), writes=[ybk])
                ys, ysk = ysr.next()
                ph.op('pool', lambda e, ya=ya, ys=ys, ti=ti: e.tensor_scalar(out=ys[:], in0=ya[:], scalar1=s.WA[:, ti:ti + 1], scalar2=None, op0=ALU.mult),
                      reads=[yak], writes=[ysk])
                ph.op('dve', lambda e, ys=ys, yb=yb, ti=ti: e.scalar_tensor_tensor(
                    out=ys[:], in0=yb[:], scalar=s.WB[:, ti:ti + 1], in1=ys[:], op0=ALU.mult, op1=ALU.add), reads=[ysk, ybk], writes=[ysk])
                xs, xk = xsr.next()
                ph.op('sp', lambda e, xs=xs, tt0=tt0, b=b: e.dma_start(
                    out=xs[:], in_=s.XT[b, :, :, tt0:tt0 + 128].rearrange("k p t -> p k t")), writes=[xk])
                for half in range(2):
                    pt, ptk = psr.next()
                    for jj in range(4):
                        c = half * 4 + jj
                        ph.op('pe', lambda e, pt=pt, ys=ys, jj=jj, c=c: e.transpose(
                            pt[:, jj * 128:(jj + 1) * 128], ys[:, c * 128:(c + 1) * 128], s.ident[:]), reads=[ysk, 'ident'], writes=[ptk])
                    for jj in range(4):
                        c = half * 4 + jj
                        ph.op('dve', lambda e, pt=pt, xs=xs, jj=jj, c=c, bcol=bcol: e.scalar_tensor_tensor(
                            out=xs[:, c, :], in0=pt[:, jj * 128:(jj + 1) * 128], scalar=s.modT[:, l, 40 + c, bcol:bcol + 1],
                            in1=xs[:, c, :], op0=ALU.mult, op1=ALU.add), reads=[ptk, xk], writes=[xk])
                if l < DEPTH - 1:
                    ph.op('sp', lambda e, xs=xs, tt0=tt0, b=b: e.dma_start(
                        out=s.XT[b, :, :, tt0:tt0 + 128].rearrange("k p t -> p k t"), in_=xs[:]), reads=[xk], writes=[('XTo', b, tt0)])
                else:
                    ot, otk = otr.next()
                    for half in range(2):
                        pt, ptk = psr.next()
                        for jj in range(4):
                            c = half * 4 + jj
                            ph.op('pe', lambda e, pt=pt, xs=xs, jj=jj, c=c: e.transpose(
                                pt[:, jj * 128:(jj + 1) * 128], xs[:, c, :], s.ident[:]), reads=[xk, 'ident'], writes=[ptk])
                        if half == 0:
                            ph.op('act', lambda e, pt=pt, ot=ot: e.activation(out=ot[:, 0:512], in_=pt[:], func=AF.Copy), reads=[ptk], writes=[(otk, 0)])
                        else:
                            ph.op('dve', lambda e, pt=pt, ot=ot: e.tensor_copy(out=ot[:, 512:1024], in_=pt[:]), reads=[ptk], writes=[(otk, 1)])
                    dst = s.out_x[b, tt0 - CTX:tt0 - CTX + 128, :] if isx else s.out_c[b, tt0:tt0 + 128, :]
                    ph.op('sp', lambda e, ot=ot, dst=dst: e.dma_start(out=dst, in_=ot[:]), reads=[(otk, 0), (otk, 1)], writes=[('out', b, tt0)])
        done(s, ph)


def kernel_run(inputs, L, NB, DEPTH, n_cores, layers=None):
    nc, ninst = build(L, NB, DEPTH)
    consts = host_constants(L)
    in_maps = []
    for c in range(n_cores):
        m = {}
        bs = slice(c * NB, (c + 1) * NB)
        m["x"] = np.ascontiguousarray(inputs["x"][bs])
        m["c"] = np.ascontiguousarray(inputs["c"][bs])
        m["ctx"] = np.ascontiguousarray(inputs["ctx"][bs])
        for k in ("c_ctx", "ada_w", "ada_b", "norm1", "norm2", "w_in", "w_out", "win_qk_gain", "win_sink",
                  "glb_qk_gain", "w_router", "b_router", "w_gate_up", "w_down"):
            m[k] = np.ascontiguousarray(inputs[k])
        m["ret_decay"] = np.ascontiguousarray(inputs["ret_decay"]).reshape(DEPTH, 8)
        for k, v in consts.items():
            m["k_" + k] = v
        in_maps.append(m)
    res = run_bass_kernel_spmd(nc, in_maps, core_ids=list(range(n_cores)))
    if DEBUG:
        global DBG
        DBG = res.results
    xo = np.concatenate([r["out"] for r in res.results], axis=0)
    co = np.concatenate([r["ctx_out"] for r in res.results], axis=0)
    return xo, co


FUSED = True
DEBUG = False
DBG = None


def kernel(**inputs):
    inputs = {k: np.asarray(v) for k, v in inputs.items()}
    B, L, _ = inputs["x"].shape
    depth = inputs["ada_w"].shape[0]
    n_cores = 8
    NB = B // n_cores
    if FUSED:
        xo, _ = kernel_run(inputs, L, NB, depth, n_cores)
        return xo.astype(np.float32)
    x, ctx = inputs["x"], inputs["ctx"]
    for l in range(depth):
        li = dict(inputs)
        li["x"], li["ctx"] = x, ctx
        for k in ("ada_w", "ada_b", "norm1", "norm2", "w_in", "w_out", "ret_decay", "win_qk_gain", "win_sink",
                  "glb_qk_gain", "w_gate_up", "w_down"):
            li[k] = inputs[k][l:l + 1]
        x, ctx = kernel_run(li, L, NB, 1, n_cores)
    return x.astype(np.float32)
```

```python
import contextlib
import types
import numpy as np
import ml_dtypes
import concourse.bass as bass
import concourse.mybir as mybir
from concourse.bass_utils import run_bass_kernel_spmd

F32 = mybir.dt.float32
BF16 = mybir.dt.bfloat16
AF = mybir.ActivationFunctionType
ALU = mybir.AluOpType
AX = mybir.AxisListType

PHYS = {'pe': 'tensor', 'act': 'scalar', 'dve': 'vector', 'pool': 'gpsimd',
        'sp': 'sync', 'pq': 'gpsimd'}
IS_DMA = {'sp', 'pq'}


NS = 8


def freeze(fn):
    if fn.__closure__ is None:
        return fn
    cells = []
    for c in fn.__closure__:
        try:
            cells.append(types.CellType(c.cell_contents))
        except ValueError:
            cells.append(c)
    return types.FunctionType(fn.__code__, fn.__globals__, fn.__name__, fn.__defaults__, tuple(cells))


class Sync:
    def __init__(self, nc, stack):
        self.nc = nc
        self.sem = {}
        self.base = {}
        for e in PHYS:
            if e in IS_DMA:
                self.sem[e] = [stack.enter_context(nc.semaphore(f"s_{e}{k}")) for k in range(NS)]
                self.base[e] = [0] * NS
            else:
                self.sem[e] = stack.enter_context(nc.semaphore(f"s_{e}"))
                self.base[e] = 0


class Phase:
    def __init__(self, sync, name):
        self.sync = sync
        self.nc = sync.nc
        self.name = name
        self.ops = {p: [] for p in ('tensor', 'scalar', 'vector', 'gpsimd', 'sync')}
        self.seq = {e: 0 for e in PHYS}
        self.res = {}
        self.flag = {e: set() for e in PHYS}

    def op(self, eng, fn, reads=(), writes=()):
        deps = set()

        def need(d):
            if d is None:
                return
            if d[0] == 'pe' and eng == 'pe':
                return
            deps.add(d)

        for r in reads:
            st = self.res.get(r)
            if st is not None:
                need(st[0])
        for w in writes:
            st = self.res.get(w)
            if st is not None:
                need(st[0])
                for d in st[1]:
                    need(d)
        self.seq[eng] += 1
        me = (eng, self.seq[eng])
        for r in reads:
            st = self.res.get(r)
            if st is None:
                self.res[r] = [None, [me]]
            else:
                st[1].append(me)
        for w in writes:
            self.res[w] = [me, []]
        if eng in IS_DMA and self.seq[eng] > NS:
            deps.add((eng, self.seq[eng] - NS))
        dd = {}
        for e, sq in deps:
            key = (e, (sq - 1) % NS) if e in IS_DMA else (e, 0)
            dd[key] = max(dd.get(key, 0), sq)
        self.ops[PHYS[eng]].append((eng, self.seq[eng], dd, freeze(fn)))
        return me

    def emit(self):
        nc = self.nc
        sy = self.sync
        final_wait = {}
        for e in IS_DMA:
            for sq in range(max(1, self.seq[e] - NS + 1), self.seq[e] + 1):
                final_wait[(e, (sq - 1) % NS)] = sq
        for p, lst in self.ops.items():
            seen = {}
            for i, (eng, seq, dd, fn) in enumerate(lst):
                nd = {}
                for key, sq in dd.items():
                    if seen.get(key, 0) >= sq:
                        continue
                    seen[key] = sq
                    nd[key] = sq
                    if key[0] not in IS_DMA:
                        self.flag[key[0]].add(sq)
                lst[i] = (eng, seq, nd, fn)
        rank = {}
        for e in PHYS:
            if e in IS_DMA:
                continue
            fl = sorted(self.flag[e])
            rank[e] = {sq: i + 1 for i, sq in enumerate(fl)}

        def wait(engine, key, sq):
            e = key[0]
            if e in IS_DMA:
                slot = (sq - 1) % NS
                engine.wait_ge(sy.sem[e][slot], (sy.base[e][slot] + (sq - 1) // NS + 1) * 16)
            else:
                engine.wait_ge(sy.sem[e], sy.base[e] + rank[e][sq])

        with nc.Block() as block:
            for p, lst in self.ops.items():
                fw = final_wait if p == 'sync' else {}
                if not lst and not fw:
                    continue

                def body(engine, lst=lst, fw=fw):
                    for eng, seq, nd, fn in lst:
                        for key, sq in nd.items():
                            wait(engine, key, sq)
                        inst = fn(engine)
                        if eng in IS_DMA:
                            inst.then_inc(sy.sem[eng][(seq - 1) % NS], 16)
                        elif seq in self.flag[eng]:
                            inst.then_inc(sy.sem[eng], 1)
                    for key, sq in fw.items():
                        wait(engine, key, sq)

                getattr(block, p)(body)
        for e in PHYS:
            if e in IS_DMA:
                for q in range(1, self.seq[e] + 1):
                    sy.base[e][(q - 1) % NS] += 1
            else:
                sy.base[e] += len(self.flag[e])
        return sum(len(l) for l in self.ops.values())


class Rot:
    def __init__(self, tiles, name):
        self.t = tiles
        self.name = name
        self.i = 0

    def next(self):
        k = self.i % len(self.t)
        self.i += 1
        return self.t[k], (self.name, k)


D = 1024
KC = 8
CTX = 256
EPS = 1e-6
NE = 16
MB = 512
I32 = mybir.dt.int32


def host_constants(L):
    c = {}
    c['ident'] = np.eye(128, dtype=np.float32)
    blk = np.zeros((128, 128), np.float32)
    blk[:64, :64] = 1.0
    blk[64:, 64:] = 1.0
    c['blk64'] = blk
    prot = np.zeros((128, 128), np.float32)
    for m in range(128):
        if (m % 32) < 16:
            prot[m + 16, m] = -1.0
        else:
            prot[m - 16, m] = 1.0
    c['prot'] = prot
    t = np.arange(L)
    row = (t // 64).astype(np.float32)
    col = (t % 64).astype(np.float32)
    inv = np.power(np.float32(10000.0), -np.arange(0, 32, 2, dtype=np.float32) / np.float32(32)).astype(np.float32)
    cosT = np.zeros((128, L), np.float32)
    sinT = np.zeros((128, L), np.float32)
    for p in range(128):
        d = p % 64
        a = d // 32
        i = d % 16
        ang = (row if a == 0 else col) * inv[i]
        cosT[p] = np.cos(ang.astype(np.float32))
        sinT[p] = np.sin(ang.astype(np.float32))
    c['cosT'] = cosT
    c['sinT'] = sinT
    k = np.arange(128)[:, None].astype(np.float32)
    q = np.arange(128)[None, :].astype(np.float32)
    c['dif_f'] = np.maximum(q - k, 0.0).astype(np.float32)
    c['msk_f'] = (q >= k).astype(np.float32)
    c['dif_b'] = np.maximum(k - q, 0.0).astype(np.float32)
    c['msk_b'] = (k >= q).astype(np.float32)
    iot = np.zeros((128, 4 * 128), np.float32)
    iot[:, 0:128] = q + 1.0
    iot[:, 128:256] = 128.0 - q
    iot[:, 256:384] = 127.0 - k
    iot[:, 384:512] = k + 0.0 * q
    c['iot'] = iot
    wm = np.zeros((6, 128, 512), np.float32)
    for oi, o in enumerate(range(-1, 5)):
        kp = o * 128 + np.arange(128)[:, None]
        qp = np.arange(512)[None, :]
        wm[oi] = np.where(np.abs(kp - qp) <= 128, 0.0, -30000.0)
    c['wmask'] = wm
    sel = np.zeros((16, 16 * 128), np.float32)
    for e in range(16):
        sel[e, e * 128:(e + 1) * 128] = 1.0
    c['sel'] = sel
    c['ustrict'] = (np.arange(128)[:, None] < np.arange(128)[None, :]).astype(np.float32)
    c['pidx'] = (np.arange(8)[None, :] * 128 + np.arange(128)[:, None]).astype(np.float32)
    c['blkiota'] = np.broadcast_to((np.arange(64) * float(MB))[None, :], (128, 64)).astype(np.float32).copy()
    return c


CONST_SHAPES = lambda L: {'ident': [128, 128], 'blk64': [128, 128], 'prot': [128, 128], 'cosT': [128, L],
                          'sinT': [128, L], 'dif_f': [128, 128], 'msk_f': [128, 128], 'dif_b': [128, 128],
                          'msk_b': [128, 128], 'iot': [128, 512], 'wmask': [6, 128, 512], 'sel': [16, 2048],
                          'ustrict': [128, 128], 'blkiota': [128, 64], 'pidx': [128, 8]}


class K:
    pass


def build(L, NB, DEPTH):
    s = K()
    s.L, s.NB, s.DEPTH = L, NB, DEPTH
    T = s.T = CTX + L
    NT = s.NT = T // 128
    NBC = s.NBC = NB + 1
    TALL = s.TALL = NB * T
    nc = s.nc = bass.Bass("TRN2", target_bir_lowering=False)

    def din(name, shape):
        return nc.dram_tensor(name, list(shape), F32, kind="ExternalInput").ap()

    s.x_in = din("x", [NB, L, D])
    s.c_in = din("c", [NB, D])
    s.ctx_in = din("ctx", [NB, CTX, D])
    s.cctx_in = din("c_ctx", [D])
    s.ada_w = din("ada_w", [DEPTH, D, 6 * D])
    s.ada_b = din("ada_b", [DEPTH, 6 * D])
    s.norm1 = din("norm1", [DEPTH, D])
    s.norm2 = din("norm2", [DEPTH, D])
    s.w_in = din("w_in", [DEPTH, D, 2560])
    s.w_out = din("w_out", [DEPTH, D, D])
    s.ret_decay = din("ret_decay", [DEPTH, 8])
    s.win_gain = din("win_qk_gain", [DEPTH, 2, 64])
    s.win_sink = din("win_sink", [DEPTH, 4])
    s.glb_gain = din("glb_qk_gain", [DEPTH, 2, 64])
    s.w_router = din("w_router", [D, NE])
    s.b_router = din("b_router", [NE])
    s.w_gu = din("w_gate_up", [DEPTH, NE, D, 2 * D])
    s.w_dn = din("w_down", [DEPTH, NE, D, D])
    s.cst = {k: din("k_" + k, sh) for k, sh in CONST_SHAPES(L).items()}
    s.out_x = nc.dram_tensor("out", [NB, L, D], F32, kind="ExternalOutput").ap()
    s.out_c = nc.dram_tensor("ctx_out", [NB, CTX, D], F32, kind="ExternalOutput").ap()
    import os
    ext = os.environ.get("EXT", "").split(",")
    dkf = lambda nm: dict(kind="ExternalOutput") if (DEBUG or nm in ext) else {}
    dk = {}
    s.XT = nc.dram_tensor("XT", [NB, KC, 128, T], F32, **dkf("XT")).ap()
    s.FM = nc.dram_tensor("FMs", [12, 128, T], BF16, **dkf("FM")).ap()
    s.TOK = nc.dram_tensor("TOKs", [T, 1536], BF16, **dkf("TOK")).ap()
    s.YT = nc.dram_tensor("YTs", [KC, 128, T], BF16, **dkf("YT")).ap()
    s.H2T = nc.dram_tensor("H2Ts", [KC, 128, TALL], BF16, **dkf("H2T")).ap()
    s.WGT = nc.dram_tensor("WGTs", [NE, TALL], F32, **dkf("WGT")).ap()
    s.NTA = NB * NT
    s.NBLK = (2 * TALL + NE * (MB - 1) + MB - 1) // MB
    assert s.NBLK <= 64
    s.WBgu = [nc.dram_tensor(f"WBgu{i}", [NE, 128, KC, 2 * D], BF16).ap() for i in range(2)]
    s.WBdn = [nc.dram_tensor(f"WBdn{i}", [NE, 128, KC, D], BF16).ap() for i in range(2)]
    s.H2TOK = nc.dram_tensor("H2TOK", [TALL, D], BF16).ap()
    s.XP = nc.dram_tensor("XPs", [s.NBLK * MB, D], BF16).ap()
    s.YP = nc.dram_tensor("YPs", [s.NBLK * MB, D], BF16).ap()
    s.chunks = [(0, CTX)] + [(CTX + i, min(512, L - i)) for i in range(0, L, 512)]
    s.nphase = 0
    s.ninst = 0
    with contextlib.ExitStack() as gst:
        s.gst = gst
        s.sync = Sync(nc, gst)
        s.ident = sb(s, "ident", [128, 128], F32)
        s.ident_bf = sb(s, "ident_bf", [128, 128], BF16)
        s.ones_bf = sb(s, "ones_bf", [128, 128], BF16)
        s.ones_f = sb(s, "ones_f", [128, 128], F32)
        s.blk64 = sb(s, "blk64", [128, 128], BF16)
        s.prot = sb(s, "prot", [128, 128], BF16)
        s.modT = sb(s, "modT", [128, DEPTH, 48, NBC], F32)
        s.gs1 = sb(s, "gs1", [128, DEPTH, KC, NBC], F32)
        s.gs2 = sb(s, "gs2", [128, DEPTH, KC, NBC], F32)
        s.wr_bf = sb(s, "wr_bf", [128, KC, NE], BF16)
        s.br_bc = sb(s, "br_bc", [128, NE], F32)
        s.ustrict = sb(s, "ustrict", [128, 128], F32)
        s.blkiota = sb(s, "blkiota", [128, 64], F32)
        s.SELt = sb(s, "SELt", [128, s.NTA, NE], F32)
        s.WGt = sb(s, "WGt", [128, s.NTA, NE], F32)
        s.RKt = sb(s, "RKt", [128, s.NTA, NE], F32)
        s.cbase = sb(s, "cbase", [128, NE], F32)
        s.DAf = sb(s, "DAf", [128, s.NTA], F32)
        s.DBf = sb(s, "DBf", [128, s.NTA], F32)
        s.DAi = sb(s, "DAi", [128, s.NTA], I32)
        s.DBi = sb(s, "DBi", [128, s.NTA], I32)
        s.WA = sb(s, "WA", [128, s.NTA], F32)
        s.WB = sb(s, "WB", [128, s.NTA], F32)
        s.IDXi = sb(s, "IDXi", [128, 64], I32)
        s.pidx = sb(s, "pidx", [128, KC], F32)
        s.PS = [gst.enter_context(nc.psum_tensor(f"ps{i}", [128, 512], F32)) for i in range(7)]
        s.PSB = gst.enter_context(nc.psum_tensor("psb", [128, 1024], BF16))
        phase_consts(s)
        phase_input(s)
        for l in range(DEPTH):
            with contextlib.ExitStack() as stL:
                phase_tables(s, l, stL)
                for b in range(NB):
                    phase_proj(s, l, b)
                    phase_ret(s, l, b)
                    phase_attn(s, l, b)
                    phase_out(s, l, b)
            phase_dest(s, l)
            phase_moe_sparse(s, l)
            phase_comb(s, l)
    return nc, s.ninst


def sb(s, name, shape, dt, st=None):
    s.nsb = getattr(s, 'nsb', 0) + 1
    return (st or s.gst).enter_context(s.nc.sbuf_tensor(f"{name}_{s.nsb}", list(shape), dt))


def new_phase(s, tag):
    s.nphase += 1
    return Phase(s.sync, f"{tag}{s.nphase}")


STOP = 10 ** 9


def done(s, ph):
    if s.nphase > STOP:
        return
    s.ninst += ph.emit()


def phase_consts(s):
    nc, NB, NBC, DEPTH, cst = s.nc, s.NB, s.NBC, s.DEPTH, s.cst
    with contextlib.ExitStack() as st0:
        ph = new_phase(s, "c")
        ph.op('sp', lambda e: e.dma_start(out=s.ident[:], in_=cst['ident']), writes=['ident'])
        ph.op('pq', lambda e: e.dma_start(out=s.ident_bf[:], in_=cst['ident']), writes=['ident_bf'])
        ph.op('pq', lambda e: e.dma_start(out=s.blk64[:], in_=cst['blk64']), writes=['blk64'])
        ph.op('pq', lambda e: e.dma_start(out=s.prot[:], in_=cst['prot']), writes=['prot'])
        ph.op('sp', lambda e: e.dma_start(out=s.ustrict[:], in_=cst['ustrict']), writes=['ustrict'])
        ph.op('sp', lambda e: e.dma_start(out=s.blkiota[:], in_=cst['blkiota']), writes=['blkiota'])
        ph.op('sp', lambda e: e.dma_start(out=s.pidx[:], in_=cst['pidx']), writes=['pidx'])
        zrow = sb(s, "zrow", [128, D], BF16, st0)
        ph.op('dve', lambda e: e.memset(zrow[:], 0.0), writes=['zrow'])
        for r0 in range(0, s.NBLK * MB, 128):
            ph.op('sp', lambda e, r0=r0: e.dma_start(out=s.XP[r0:r0 + 128, :], in_=zrow[:]), reads=['zrow'], writes=[('XP', r0)])
        ph.op('dve', lambda e: e.memset(s.ones_bf[:], 1.0), writes=['ones_bf'])
        ph.op('dve', lambda e: e.memset(s.ones_f[:], 1.0), writes=['ones_f'])
        ph.op('pq', lambda e: e.dma_start(out=s.wr_bf[:], in_=s.w_router.rearrange("(k p) n -> p k n", p=128)), writes=['wr'])
        ph.op('sp', lambda e: e.dma_start(out=s.br_bc[:], in_=s.b_router.partition_broadcast(128)), writes=['br'])
        cT = sb(s, "cT", [128, KC, NBC], F32, st0)
        scT = sb(s, "scT", [128, KC, NBC], BF16, st0)
        for b in range(NB):
            ph.op('sp', lambda e, b=b: e.dma_start(out=cT[:, :, b], in_=s.c_in[b].rearrange("(k p) -> p k", p=128),
                                                    allow_slow_non_contiguous=True), writes=['cT'])
        ph.op('sp', lambda e: e.dma_start(out=cT[:, :, NB], in_=s.cctx_in.rearrange("(k p) -> p k", p=128),
                                          allow_slow_non_contiguous=True), writes=['cT'])
        ph.op('act', lambda e: e.activation(out=scT[:], in_=cT[:], func=AF.Silu), reads=['cT'], writes=['scT'])
        adab = sb(s, "adab", [128, DEPTH, 48], F32, st0)
        n1T = sb(s, "n1T", [128, DEPTH, KC], F32, st0)
        n2T = sb(s, "n2T", [128, DEPTH, KC], F32, st0)
        for l in range(DEPTH):
            for (dst, src) in ((adab, s.ada_b), (n1T, s.norm1), (n2T, s.norm2)):
                ph.op('sp', lambda e, l=l, dst=dst, src=src: e.dma_start(
                    out=dst[:, l, :], in_=src[l].rearrange("(c p) -> p c", p=128), allow_slow_non_contiguous=True),
                    writes=['smallT'])
        awr = Rot([sb(s, f"aw{i}", [128, KC, 1536], BF16, st0) for i in range(2)], "aw")
        for l in range(DEPTH):
            for qt in range(4):
                aw, awk = awr.next()
                ph.op('pq', lambda e, aw=aw, l=l, qt=qt: e.dma_start(
                    out=aw[:], in_=s.ada_w[l].rearrange("(k p) n -> p k n", p=128)[:, :, qt * 1536:(qt + 1) * 1536]),
                    writes=[awk])
                for f in range(12):
                    fc = qt * 12 + f
                    ps = s.PS[fc % 4]
                    for k in range(KC):
                        ph.op('pe', lambda e, ps=ps, aw=aw, f=f, k=k: e.matmul(
                            ps[:, 0:NBC], aw[:, k, f * 128:(f + 1) * 128], scT[:, k, :], start=(k == 0), stop=(k == KC - 1)),
                            reads=[awk, 'scT'], writes=[('ps', fc % 4)])
                    ph.op('dve', lambda e, ps=ps, l=l, fc=fc: e.tensor_scalar(
                        out=s.modT[:, l, fc, :], in0=ps[:, 0:NBC], scalar1=adab[:, l, fc:fc + 1], scalar2=None, op0=ALU.add),
                        reads=[('ps', fc % 4), 'smallT'], writes=['modT'])
            for (gs, nT, base) in ((s.gs1, n1T, 8), (s.gs2, n2T, 32)):
                for b in range(NBC):
                    ph.op('dve', lambda e, gs=gs, nT=nT, base=base, b=b, l=l: e.scalar_tensor_tensor(
                        out=gs[:, l, :, b], in0=s.modT[:, l, base:base + 8, b], scalar=1.0, in1=nT[:, l, :],
                        op0=ALU.add, op1=ALU.mult), reads=['modT', 'smallT'], writes=['gs'])
        done(s, ph)


def stage_weight(s, ph, l, i):
    buf = l % 2
    if i < NE:
        ph.op('pq', lambda e, ex=i: e.dma_start(
            out=s.WBgu[buf][ex], in_=s.w_gu[l, ex].rearrange("(k p) n -> p k n", p=128)), writes=[('wbgu', buf, i)])
    else:
        ph.op('pq', lambda e, ex=i - NE: e.dma_start(
            out=s.WBdn[buf][ex], in_=s.w_dn[l, ex].rearrange("(k p) n -> p k n", p=128)), writes=[('wbdn', buf, i)])


def phase_input(s):
    NB, NT = s.NB, s.NT
    with contextlib.ExitStack() as st1:
        ph = new_phase(s, "i")
        for i in range(2 * NE):
            stage_weight(s, ph, 0, i)
        xtr = Rot([sb(s, f"xin{i}", [128, D], F32, st1) for i in range(3)], "xin")
        str_ = Rot([sb(s, f"xstg{i}", [128, KC, 128], F32, st1) for i in range(3)], "xstg")
        for b in range(NB):
            for i in range(NT):
                t0 = i * 128
                src = s.ctx_in[b, t0:t0 + 128, :] if t0 < CTX else s.x_in[b, t0 - CTX:t0 - CTX + 128, :]
                xt, xk = xtr.next()
                ph.op('sp', lambda e, xt=xt, src=src: e.dma_start(out=xt[:], in_=src), writes=[xk])
                stg, sk = str_.next()
                for half in range(2):
                    ps = s.PS[half]
                    for j in range(4):
                        c = half * 4 + j
                        ph.op('pe', lambda e, ps=ps, j=j, c=c, xt=xt: e.transpose(
                            ps[:, j * 128:(j + 1) * 128], xt[:, c * 128:(c + 1) * 128], s.ident[:]),
                            reads=[xk, 'ident'], writes=[('ps', half)])
                    if half == 0:
                        ph.op('act', lambda e, ps=ps, stg=stg: e.activation(
                            out=stg[:, 0:4, :], in_=ps[:].rearrange("p (j t) -> p j t", j=4), func=AF.Copy),
                            reads=[('ps', half)], writes=[(sk, half)])
                    else:
                        ph.op('dve', lambda e, ps=ps, stg=stg: e.tensor_copy(
                            out=stg[:, 4:8, :], in_=ps[:].rearrange("p (j t) -> p j t", j=4)),
                            reads=[('ps', half)], writes=[(sk, half)])
                ph.op('sp', lambda e, stg=stg, b=b, t0=t0: e.dma_start(
                    out=s.XT[b, :, :, t0:t0 + 128].rearrange("k p t -> p k t"), in_=stg[:]),
                    reads=[(sk, 0), (sk, 1)], writes=[('XT', b, t0)])
        done(s, ph)


def norm_mod(s, ph, xs, xk, n, gs, sh, l, bcol, hT, hk, sqr, rstd, tmpr):
    psS = s.PS[6]
    for c in range(KC):
        sq, sqk = sqr.next()
        ph.op('act', lambda e, c=c, sq=sq: e.activation(out=sq[:, :n], in_=xs[:, c, :n], func=AF.Square),
              reads=[xk], writes=[sqk])
        ph.op('pe', lambda e, c=c, sq=sq: e.matmul(psS[:, :n], s.ones_bf[:], sq[:, :n], start=(c == 0), stop=(c == KC - 1)),
              reads=[sqk, 'ones_bf'], writes=[('ps', 6)])
    ph.op('act', lambda e: e.activation(out=rstd[:, :n], in_=psS[:, :n], func=AF.Sqrt, bias=EPS, scale=1.0 / D),
          reads=[('ps', 6)], writes=['rstd'])
    ph.op('dve', lambda e: e.reciprocal(out=rstd[:, :n], in_=rstd[:, :n]), reads=['rstd'], writes=['rstd'])
    for c in range(KC):
        tmp, tk = tmpr.next()
        ph.op('dve', lambda e, c=c, tmp=tmp: e.scalar_tensor_tensor(
            out=tmp[:, :n], in0=xs[:, c, :n], scalar=gs[:, l, c, bcol:bcol + 1], in1=rstd[:, :n],
            op0=ALU.mult, op1=ALU.mult), reads=[xk, 'rstd'], writes=[tk])
        ph.op('act', lambda e, c=c, tmp=tmp: e.activation(
            out=hT[:, c, :n], in_=tmp[:, :n], func=AF.Identity, bias=s.modT[:, l, sh + c, bcol:bcol + 1], scale=1.0),
            reads=[tk], writes=[hk])


def phase_tables(s, l, stL):
    cst = s.cst
    s.gcol = gcol = sb(s, "gcol", [128, 4], F32, stL)
    s.esink = esink = sb(s, "esink", [128, 4], F32, stL)
    lg = sb(s, "lg", [128, 8], F32, stL)
    lgp = sb(s, "lgp", [128, 4], F32, stL)
    s.dsum = dsum = sb(s, "dsum", [128, 4, 128], F32, stL)
    s.xit = xit = sb(s, "xit", [128, 4, 128], F32, stL)
    s.zt = zt = sb(s, "zt", [128, 2, 256], F32, stL)
    s.gcp = gcp = sb(s, "gcp", [128, 4], F32, stL)
    iot = sb(s, "iot", [128, 512], F32, stL)
    dcf = sb(s, "dcf", [128, 4, 128], F32, stL)
    z4 = sb(s, "z4", [128, 8], F32, stL)
    tmpd = sb(s, "tmpd", [128, 128], F32, stL)
    ph = new_phase(s, "t")
    ph.op('dve', lambda e: e.memset(s.cbase[:], 0.0), writes=['cbase'])
    for (i, (src, sc)) in enumerate(((s.win_gain[l, 0], 0.125), (s.win_gain[l, 1], 1.0),
                                     (s.glb_gain[l, 0], 0.125), (s.glb_gain[l, 1], 1.0))):
        for hf in range(2):
            ph.op('sp', lambda e, i=i, src=src, hf=hf: e.dma_start(
                out=gcol[hf * 64:(hf + 1) * 64, i:i + 1], in_=src.rearrange("(d o) -> d o", o=1)), writes=['gcol'])
        if sc != 1.0:
            ph.op('dve', lambda e, i=i, sc=sc: e.tensor_scalar(
                out=gcol[:, i:i + 1], in0=gcol[:, i:i + 1], scalar1=sc, scalar2=None, op0=ALU.mult),
                reads=['gcol'], writes=['gcol'])
    ph.op('sp', lambda e: e.dma_start(out=esink[:], in_=s.win_sink[l].partition_broadcast(128)), writes=['esink'])
    ph.op('act', lambda e: e.activation(out=esink[:], in_=esink[:], func=AF.Exp), reads=['esink'], writes=['esink'])
    ph.op('sp', lambda e: e.dma_start(out=lg[:], in_=s.ret_decay[l].partition_broadcast(128)), writes=['lg'])
    ph.op('sp', lambda e: e.dma_start(out=iot[:], in_=cst['iot']), writes=['iot'])
    for i, nm in enumerate(('dif_f', 'msk_f', 'dif_b', 'msk_b')):
        ph.op('sp', lambda e, i=i, nm=nm: e.dma_start(out=dcf[:, i, :], in_=cst[nm]), writes=['dcf'])
    ph.op('act', lambda e: e.activation(out=lg[:], in_=lg[:], func=AF.Exp, scale=-1.0), reads=['lg'], writes=['lg'])
    ph.op('act', lambda e: e.activation(out=lg[:], in_=lg[:], func=AF.Ln, bias=1.0, scale=1.0), reads=['lg'], writes=['lg'])
    ph.op('dve', lambda e: e.tensor_scalar(out=lg[:], in0=lg[:], scalar1=-1.0, scalar2=None, op0=ALU.mult),
          reads=['lg'], writes=['lg'])
    for dr in range(2):
        for g in range(2):
            for hf in range(2):
                ph.op('dve', lambda e, dr=dr, g=g, hf=hf: e.tensor_copy(
                    out=lgp[hf * 64:(hf + 1) * 64, dr * 2 + g:dr * 2 + g + 1],
                    in_=lg[hf * 64:(hf + 1) * 64, dr * 4 + 2 * g + hf:dr * 4 + 2 * g + hf + 1]),
                    reads=['lg'], writes=['lgp'])
    for h in range(4):
        ph.op('act', lambda e, h=h: e.activation(out=dsum[:, h, :], in_=dcf[:, 0, :], func=AF.Exp, scale=lg[:, h:h + 1]),
              reads=['lg', 'dcf'], writes=[('dsum', h)])
        ph.op('dve', lambda e, h=h: e.tensor_tensor(out=dsum[:, h, :], in0=dsum[:, h, :], in1=dcf[:, 1, :], op=ALU.mult),
              reads=[('dsum', h)], writes=[('dsum', h)])
        ph.op('act', lambda e, h=h: e.activation(out=tmpd[:], in_=dcf[:, 2, :], func=AF.Exp, scale=lg[:, 4 + h:5 + h]),
              reads=['lg', 'dcf'], writes=['tmpd'])
        ph.op('dve', lambda e, h=h: e.tensor_tensor(out=tmpd[:], in0=tmpd[:], in1=dcf[:, 3, :], op=ALU.mult),
              reads=['tmpd'], writes=['tmpd'])
        ph.op('dve', lambda e, h=h: e.tensor_tensor(out=dsum[:, h, :], in0=dsum[:, h, :], in1=tmpd[:], op=ALU.add),
              reads=[('dsum', h), 'tmpd'], writes=[('dsum', h)])
    for dr in range(2):
        for g in range(2):
            i = dr * 2 + g
            ph.op('act', lambda e, i=i, dr=dr: e.activation(
                out=xit[:, i, :], in_=iot[:, dr * 128:(dr + 1) * 128], func=AF.Exp, scale=lgp[:, i:i + 1]),
                reads=['lgp', 'iot'], writes=['xit'])
        for h in range(4):
            ph.op('act', lambda e, dr=dr, h=h: e.activation(
                out=z4[:, dr * 4 + h:dr * 4 + h + 1], in_=iot[:, 256 + dr * 128:257 + dr * 128], func=AF.Exp,
                scale=lg[:, dr * 4 + h:dr * 4 + h + 1]), reads=['lg', 'iot'], writes=['z4'])
            ph.op('dve', lambda e, dr=dr, h=h: e.tensor_scalar(
                out=zt[:, dr, h * 64:(h + 1) * 64], in0=s.ones_f[:, 0:64], scalar1=z4[:, dr * 4 + h:dr * 4 + h + 1],
                scalar2=0.125, op0=ALU.mult, op1=ALU.mult), reads=['z4', 'ones_f'], writes=['zt'])
    ph.op('act', lambda e: e.activation(out=gcp[:], in_=lgp[:], func=AF.Exp, scale=128.0), reads=['lgp'], writes=['gcp'])
    done(s, ph)


FMB = [(0, 0, 256), (256, 256, 256), (512, 1536, 256), (768, 1792, 64), (832, 1792, 64), (896, 1856, 64), (960, 1856, 64),
       (1024, 2048, 256), (1280, 2304, 64), (1344, 2304, 64), (1408, 2368, 64), (1472, 2368, 64)]
TMB = [(0, 256, 256), (256, 1920, 128), (384, 2432, 128), (512, 512, 512), (1024, 1024, 512)]


def phase_proj(s, l, b):
    T, L, NB = s.T, s.L, s.NB
    PS = s.PS
    with contextlib.ExitStack() as st:
        wfm = sb(s, "wfm", [128, KC, 1536], BF16, st)
        wtm = sb(s, "wtm", [128, KC, 1536], BF16, st)
        cosT = sb(s, "cosT", [128, L], F32, st)
        sinT = sb(s, "sinT", [128, L], F32, st)
        xsr = Rot([sb(s, f"pxs{i}", [128, KC, 512], F32, st) for i in range(1)], "pxs")
        hTr = Rot([sb(s, f"phT{i}", [128, KC, 512], BF16, st) for i in range(1)], "phT")
        sqr = Rot([sb(s, f"psq{i}", [128, 512], BF16, st) for i in range(3)], "psq")
        rstd = sb(s, "prstd", [128, 512], F32, st)
        tmpr = Rot([sb(s, f"ptmp{i}", [128, 512], F32, st) for i in range(2)], "ptmp")
        fmr = Rot([sb(s, f"pfm{i}", [128, 12, 512], BF16, st) for i in range(1)], "pfm")
        tokr = Rot([sb(s, f"ptok{i}", [128, 1536], BF16, st) for i in range(1)], "ptok")
        rr = Rot([sb(s, f"pr{i}", [128, 512], F32, st) for i in range(2)], "pr")
        qnr = Rot([sb(s, f"pqn{i}", [128, 512], BF16, st) for i in range(3)], "pqn")
        t1r = Rot([sb(s, f"pt1{i}", [128, 512], F32, st) for i in range(1)], "pt1")
        t2r = Rot([sb(s, f"pt2{i}", [128, 512], F32, st) for i in range(1)], "pt2")
        psr = Rot(PS[0:4], "ps")
        pxr = Rot(PS[4:6], "px")
        ph = new_phase(s, "a")
        wv = s.w_in[l].rearrange("(k p) n -> p k n", p=128)
        for (d0, s0, w) in FMB:
            ph.op('pq', lambda e, d0=d0, s0=s0, w=w: e.dma_start(out=wfm[:, :, d0:d0 + w], in_=wv[:, :, s0:s0 + w]), writes=['wfm'])
        for (d0, s0, w) in TMB:
            ph.op('pq', lambda e, d0=d0, s0=s0, w=w: e.dma_start(out=wtm[:, :, d0:d0 + w], in_=wv[:, :, s0:s0 + w]), writes=['wtm'])
        ph.op('sp', lambda e: e.dma_start(out=cosT[:], in_=s.cst['cosT']), writes=['cos'])
        ph.op('sp', lambda e: e.dma_start(out=sinT[:], in_=s.cst['sinT']), writes=['sin'])
        for (t0, n) in s.chunks:
            isx = t0 >= CTX
            bcol = b if isx else NB
            x0 = t0 - CTX
            xs, xk = xsr.next()
            ph.op('sp', lambda e, xs=xs, t0=t0, n=n: e.dma_start(
                out=xs[:, :, :n], in_=s.XT[b, :, :, t0:t0 + n].rearrange("k p t -> p k t")), writes=[xk])
            hT, hk = hTr.next()
            norm_mod(s, ph, xs, xk, n, s.gs1, 0, l, bcol, hT, hk, sqr, rstd, tmpr)
            fm, fk = fmr.next()
            stB, stC = {}, {}

            def stage_a(j):
                ps, pk = psr.next()
                for k in range(KC):
                    ph.op('pe', lambda e, ps=ps, j=j, k=k, hT=hT: e.matmul(
                        ps[:, :n], wfm[:, k, j * 128:(j + 1) * 128], hT[:, k, :n], start=(k == 0), stop=(k == KC - 1)),
                        reads=['wfm', hk], writes=[pk])
                if j < 2:
                    ph.op('act', lambda e, ps=ps, j=j, fm=fm: e.activation(out=fm[:, j, :n], in_=ps[:, :n], func=AF.Copy),
                          reads=[pk], writes=[(fk, j)])
                elif j < 4:
                    ph.op('act', lambda e, ps=ps, j=j, fm=fm: e.activation(out=fm[:, j, :n], in_=ps[:, :n], func=AF.Copy, scale=0.125),
                          reads=[pk], writes=[(fk, j)])
                else:
                    sq, sqk = sqr.next()
                    ph.op('act', lambda e, ps=ps, sq=sq: e.activation(out=sq[:, :n], in_=ps[:, :n], func=AF.Square),
                          reads=[pk], writes=[sqk])
                    stB[j] = (ps, pk, sq, sqk)

            def stage_b(j):
                if j not in stB:
                    return
                ps, pk, sq, sqk = stB.pop(j)
                kind = (j - 4) // 2
                px, pxk = pxr.next()
                ph.op('pe', lambda e, px=px, sq=sq: e.matmul(px[:, :n], s.blk64[:], sq[:, :n], start=True, stop=True),
                      reads=[sqk, 'blk64'], writes=[pxk])
                r, rk = rr.next()
                ph.op('act', lambda e, px=px, r=r: e.activation(out=r[:, :n], in_=px[:, :n], func=AF.Sqrt, bias=EPS, scale=1.0 / 64),
                      reads=[pxk], writes=[rk])
                ph.op('dve', lambda e, r=r: e.reciprocal(out=r[:, :n], in_=r[:, :n]), reads=[rk], writes=[rk])
                if not isx:
                    ph.op('dve', lambda e, ps=ps, r=r, j=j, fm=fm, kind=kind: e.scalar_tensor_tensor(
                        out=fm[:, j, :n], in0=ps[:, :n], scalar=s.gcol[:, kind:kind + 1], in1=r[:, :n],
                        op0=ALU.mult, op1=ALU.mult), reads=[pk, rk, 'gcol'], writes=[(fk, j)])
                else:
                    qn, qk = qnr.next()
                    ph.op('dve', lambda e, ps=ps, r=r, qn=qn, kind=kind: e.scalar_tensor_tensor(
                        out=qn[:, :n], in0=ps[:, :n], scalar=s.gcol[:, kind:kind + 1], in1=r[:, :n],
                        op0=ALU.mult, op1=ALU.mult), reads=[pk, rk, 'gcol'], writes=[qk])
                    stC[j] = (qn, qk)

            def stage_c(j):
                if j not in stC:
                    return
                qn, qk = stC.pop(j)
                px2, px2k = pxr.next()
                ph.op('pe', lambda e, px2=px2, qn=qn: e.matmul(px2[:, :n], s.prot[:], qn[:, :n], start=True, stop=True),
                      reads=[qk, 'prot'], writes=[px2k])
                t1, t1k = t1r.next()
                t2, t2k = t2r.next()
                ph.op('pool', lambda e, t1=t1, qn=qn: e.tensor_tensor(
                    out=t1[:, :n], in0=qn[:, :n], in1=cosT[:, x0:x0 + n], op=ALU.mult), reads=[qk, 'cos'], writes=[t1k])
                ph.op('dve', lambda e, t2=t2, px2=px2: e.tensor_tensor(
                    out=t2[:, :n], in0=px2[:, :n], in1=sinT[:, x0:x0 + n], op=ALU.mult), reads=[px2k, 'sin'], writes=[t2k])
                ph.op('pool', lambda e, t1=t1, t2=t2, fm=fm, j=j: e.tensor_tensor(
                    out=fm[:, j, :n], in0=t1[:, :n], in1=t2[:, :n], op=ALU.add), reads=[t1k, t2k], writes=[(fk, j)])

            for step in range(12 + 2):
                if step < 12:
                    stage_a(step)
                stage_b(step - 1)
                stage_c(step - 2)
            ph.op('sp', lambda e, fm=fm, t0=t0, n=n: e.dma_start(
                out=s.FM[:, :, t0:t0 + n].rearrange("j p t -> p j t"), in_=fm[:, :, :n]),
                reads=[(fk, j) for j in range(12)], writes=[('FM', t0)])
            for i in range(n // 128):
                tok, tkk = tokr.next()
                for g in range(3):
                    ps, pk = psr.next()
                    for k in range(KC):
                        ph.op('pe', lambda e, ps=ps, g=g, k=k, i=i, hT=hT: e.matmul(
                            ps[:], hT[:, k, i * 128:(i + 1) * 128], wtm[:, k, g * 512:(g + 1) * 512], start=(k == 0), stop=(k == KC - 1)),
                            reads=['wtm', hk], writes=[pk])
                    if g == 0:
                        ph.op('dve', lambda e, ps=ps, tok=tok: e.tensor_copy(out=tok[:, 0:512], in_=ps[:]),
                              reads=[pk], writes=[(tkk, 0)])
                    elif g == 1:
                        ph.op('act', lambda e, ps=ps, tok=tok: e.activation(out=tok[:, 512:1024], in_=ps[:], func=AF.Copy),
                              reads=[pk], writes=[(tkk, 1)])
                    else:
                        ph.op('act', lambda e, ps=ps, tok=tok: e.activation(out=tok[:, 1024:1536], in_=ps[:], func=AF.Silu),
                              reads=[pk], writes=[(tkk, 2)])
                ph.op('sp', lambda e, tok=tok, t0=t0, i=i: e.dma_start(out=s.TOK[t0 + i * 128:t0 + (i + 1) * 128, :], in_=tok[:]),
                      reads=[(tkk, 0), (tkk, 1), (tkk, 2)], writes=[('TOK', t0, i)])
        done(s, ph)


def phase_ret(s, l, b):
    T, NT = s.T, s.NT
    PS = s.PS
    with contextlib.ExitStack() as st:
        qt = sb(s, "rq", [128, 2, T], BF16, st)
        kt = sb(s, "rk", [128, 2, T], BF16, st)
        ktok = sb(s, "rktok", [128, NT, 256], BF16, st)
        v = sb(s, "rv", [128, NT, 512], BF16, st)
        sat = sb(s, "rsat", [128, 4, NT, 128], BF16, st)
        srun = sb(s, "rsrun", [128, 4, 128], F32, st)
        kzr = Rot([sb(s, f"rkz{i}", [128, 128], BF16, st) for i in range(3)], "rkz")
        attr = Rot([sb(s, f"ratt{i}", [128, 128], BF16, st) for i in range(3)], "ratt")
        qxr = Rot([sb(s, f"rqx{i}", [128, 2, 128], BF16, st) for i in range(2)], "rqx")
        gater = Rot([sb(s, f"rgate{i}", [128, 512], BF16, st) for i in range(2)], "rgate")
        ybr = Rot([sb(s, f"rybf{i}", [128, 512], BF16, st) for i in range(2)], "rybf")
        ytr = Rot([sb(s, f"ryt{i}", [128, 4, 128], BF16, st) for i in range(2)], "ryt")
        tmpr = Rot([sb(s, f"rtmp{i}", [128, 512], F32, st) for i in range(2)], "rtmp")
        junk = sb(s, "rjunk", [128, 128], F32, st)
        statr = Rot([sb(s, f"rstat{i}", [128, 12], F32, st) for i in range(2)], "rstat")
        ph = new_phase(s, "r")
        ph.op('sp', lambda e: e.dma_start(out=qt[:], in_=s.FM[0:2, :, :].rearrange("j p t -> p j t")), writes=['qt'])
        ph.op('sp', lambda e: e.dma_start(out=kt[:], in_=s.FM[2:4, :, :].rearrange("j p t -> p j t")), writes=['kt'])
        ph.op('sp', lambda e: e.dma_start(out=ktok[:], in_=s.TOK[:, 0:256].rearrange("(i p) c -> p i c", p=128)), writes=['ktok'])
        ph.op('sp', lambda e: e.dma_start(out=v[:], in_=s.TOK[:, 512:1024].rearrange("(i p) c -> p i c", p=128)), writes=['v'])
        ph.op('dve', lambda e: e.memset(srun[:], 0.0), writes=[('srun', i) for i in range(4)])
        psur = Rot(PS[0:2], "psu")
        orders = [list(range(NT)), [1, 0] + list(range(NT - 1, 1, -1))]
        for dr in range(2):
            for i in orders[dr]:
                for g in range(2):
                    idx = dr * 2 + g
                    kz, kzk = kzr.next()
                    ph.op('pool', lambda e, kz=kz, i=i, g=g, dr=dr: e.tensor_tensor(
                        out=kz[:], in0=ktok[:, i, g * 128:(g + 1) * 128], in1=s.zt[:, dr, g * 128:(g + 1) * 128], op=ALU.mult),
                        reads=['ktok', 'zt'], writes=[kzk])
                    pu, puk = psur.next()
                    for hh in range(2):
                        h = 2 * g + hh
                        ph.op('pe', lambda e, pu=pu, kz=kz, hh=hh, h=h, i=i: e.matmul(
                            pu[:, hh * 128:(hh + 1) * 128], kz[:], v[:, i, h * 128:(h + 1) * 128], start=True, stop=True),
                            reads=[kzk, 'v'], writes=[puk])
                    ph.op('act', lambda e, idx=idx, i=i: e.activation(out=sat[:, idx, i, :], in_=srun[:, idx, :], func=AF.Copy),
                          reads=[('srun', idx)], writes=[('sat', idx, i)])
                    for hh in range(2):
                        ph.op('dve', lambda e, pu=pu, hh=hh, idx=idx: e.scalar_tensor_tensor(
                            out=srun[hh * 64:(hh + 1) * 64, idx, :], in0=srun[hh * 64:(hh + 1) * 64, idx, :],
                            scalar=s.gcp[hh * 64:(hh + 1) * 64, idx:idx + 1],
                            in1=pu[hh * 64:(hh + 1) * 64, hh * 128:(hh + 1) * 128], op0=ALU.mult, op1=ALU.add),
                            reads=[puk, ('srun', idx), 'gcp'], writes=[('srun', idx)])
        pssr = Rot(PS[2:4], "pss")
        psor = Rot(PS[4:6], "pso")
        for i in range(NT):
            tc = slice(i * 128, (i + 1) * 128)
            po, pok = psor.next()
            for g in range(2):
                qx, qxk = qxr.next()
                for dr in range(2):
                    ph.op('pool', lambda e, qx=qx, g=g, dr=dr, tc=tc: e.tensor_tensor(
                        out=qx[:, dr, :], in0=qt[:, g, tc], in1=s.xit[:, dr * 2 + g, :], op=ALU.mult),
                        reads=['qt', 'xit'], writes=[qxk])
                for hh in range(2):
                    h = 2 * g + hh
                    rows = slice(hh * 64, (hh + 1) * 64)
                    pss, psk = pssr.next()
                    ph.op('pe', lambda e, pss=pss, rows=rows, g=g, tc=tc: e.matmul(
                        pss[:, 0:128], kt[rows, g, tc], qt[rows, g, tc], start=True, stop=True),
                        reads=['kt', 'qt'], writes=[psk])
                    att, atk = attr.next()
                    ph.op('dve', lambda e, pss=pss, att=att, h=h: e.tensor_tensor(
                        out=att[:], in0=pss[:, 0:128], in1=s.dsum[:, h, :], op=ALU.mult), reads=[psk, ('dsum', h)], writes=[atk])
                    oc = slice(h * 128, (h + 1) * 128)
                    ph.op('pe', lambda e, po=po, att=att, oc=oc, i=i: e.matmul(
                        po[:, oc], att[:], v[:, i, oc], start=True, stop=False), reads=[atk, 'v'], writes=[(pok, h)])
                    ph.op('pe', lambda e, po=po, qx=qx, rows=rows, oc=oc, g=g, i=i: e.matmul(
                        po[:, oc], qx[rows, 0, :], sat[rows, g, i, :], start=False, stop=False),
                        reads=[qxk, ('sat', g, i)], writes=[(pok, h)])
                    ph.op('pe', lambda e, po=po, qx=qx, rows=rows, oc=oc, g=g, i=i: e.matmul(
                        po[:, oc], qx[rows, 1, :], sat[rows, 2 + g, i, :], start=False, stop=True),
                        reads=[qxk, ('sat', 2 + g, i)], writes=[(pok, h)])
            pokeys = [(pok, h) for h in range(4)]
            stt_, stk = statr.next()
            ph.op('dve', lambda e, stt_=stt_: e.memset(stt_[:], 0.0), writes=[stk])
            ph.op('dve', lambda e, po=po, stt_=stt_: e.tensor_reduce(
                out=stt_[:, 0:4], in_=po[:].rearrange("p (h d) -> p h d", h=4), axis=AX.X, op=ALU.add),
                reads=pokeys + [stk], writes=[stk])
            ph.op('dve', lambda e, stt_=stt_: e.tensor_scalar(
                out=stt_[:, 0:4], in0=stt_[:, 0:4], scalar1=-1.0 / 128, scalar2=None, op0=ALU.mult), reads=[stk], writes=[stk])
            for h in range(4):
                ph.op('act', lambda e, po=po, stt_=stt_, h=h: e.activation(
                    out=junk[:], in_=po[:, h * 128:(h + 1) * 128], func=AF.Square, bias=stt_[:, h:h + 1], scale=1.0,
                    accum_out=stt_[:, 4 + h:5 + h]), reads=pokeys + [stk], writes=[stk, 'junk'])
            ph.op('act', lambda e, stt_=stt_: e.activation(
                out=stt_[:, 8:12], in_=stt_[:, 4:8], func=AF.Sqrt, bias=EPS, scale=1.0 / 128), reads=[stk], writes=[stk])
            ph.op('dve', lambda e, stt_=stt_: e.reciprocal(out=stt_[:, 8:12], in_=stt_[:, 8:12]), reads=[stk], writes=[stk])
            tmp, tmk = tmpr.next()
            for h in range(4):
                ph.op('dve', lambda e, po=po, stt_=stt_, tmp=tmp, h=h: e.tensor_scalar(
                    out=tmp[:, h * 128:(h + 1) * 128], in0=po[:, h * 128:(h + 1) * 128], scalar1=stt_[:, h:h + 1],
                    scalar2=stt_[:, 8 + h:9 + h], op0=ALU.add, op1=ALU.mult), reads=pokeys + [stk], writes=[tmk])
            gate, gk = gater.next()
            ph.op('sp', lambda e, gate=gate, tc=tc: e.dma_start(out=gate[:], in_=s.TOK[tc, 1024:1536]), writes=[gk])
            yb, ybk = ybr.next()
            ph.op('pool', lambda e, yb=yb, tmp=tmp, gate=gate: e.tensor_tensor(out=yb[:], in0=tmp[:], in1=gate[:], op=ALU.mult),
                  reads=[tmk, gk], writes=[ybk])
            for j in range(4):
                ph.op('pe', lambda e, yb=yb, j=j: e.transpose(
                    s.PSB[:, j * 128:(j + 1) * 128], yb[:, j * 128:(j + 1) * 128], s.ident_bf[:]),
                    reads=[ybk, 'ident_bf'], writes=['psb'])
            yt_, ytk = ytr.next()
            ph.op('act', lambda e, yt_=yt_: e.activation(
                out=yt_[:], in_=s.PSB[:, 0:512].rearrange("p (j t) -> p j t", j=4), func=AF.Copy), reads=['psb'], writes=[ytk])
            ph.op('sp', lambda e, yt_=yt_, tc=tc: e.dma_start(
                out=s.YT[0:4, :, tc].rearrange("j p t -> p j t"), in_=yt_[:]), reads=[ytk], writes=[('YT', i)])
        done(s, ph)


def phase_attn(s, l, b):
    T, NT = s.T, s.NT
    PS = s.PS
    for kind in (0, 1):
        with contextlib.ExitStack() as st:
            qc, kc, vcol, yrow = 4 + kind * 4, 6 + kind * 4, 256 + kind * 128, 4 + kind * 2
            q2 = sb(s, "aq2", [128, 2, T], BF16, st)
            k2 = sb(s, "ak2", [128, 2, T], BF16, st)
            vaug = sb(s, "avaug", [128, NT, 2, 65], BF16, st)
            wm = sb(s, "awm", [128, 6, 512], BF16, st)
            pr = Rot([sb(s, f"ap{i}", [128, 512], BF16, st) for i in range(4)], "ap")
            rden = sb(s, "arden", [128, 512], F32, st)
            osbr = Rot([sb(s, f"aosb{i}", [128, 512], F32, st) for i in range(2)], "aosb")
            ystr = Rot([sb(s, f"ayst{i}", [128, 512], BF16, st) for i in range(2)], "ayst")
            ph = new_phase(s, "w" if kind == 0 else "g")
            ph.op('sp', lambda e: e.dma_start(out=q2[:], in_=s.FM[qc:qc + 2, :, :].rearrange("j p t -> p j t")), writes=['q2'])
            ph.op('sp', lambda e: e.dma_start(out=k2[:], in_=s.FM[kc:kc + 2, :, :].rearrange("j p t -> p j t")), writes=['k2'])
            ph.op('dve', lambda e: e.memset(vaug[:], 1.0), writes=['vaug'])
            for gg in range(2):
                ph.op('sp', lambda e, gg=gg: e.dma_start(
                    out=vaug[:, :, gg, 0:64],
                    in_=s.TOK[:, vcol + gg * 64:vcol + (gg + 1) * 64].rearrange("(i p) d -> p i d", p=128)),
                    writes=['vaug'])
            if kind == 0:
                ph.op('pq', lambda e: e.dma_start(out=wm[:], in_=s.cst['wmask'].rearrange("o p q -> p o q")), writes=['wm'])
            pssr = Rot(PS[0:4], "pss")
            psor = Rot(PS[4:6], "pso")
            pending = []
            for (t0, n) in s.chunks:
                isx = t0 >= CTX
                qt0 = t0 // 128
                if not isx:
                    keytiles = [0, 1]
                elif kind == 1:
                    keytiles = list(range(NT))
                else:
                    keytiles = [0, 1] + [j for j in range(qt0 - 1, qt0 + 5) if 2 <= j < NT]
                for h in range(4):
                    g = h // 2
                    rows = slice((h % 2) * 64, (h % 2) * 64 + 64)
                    po, pok = psor.next()
                    nk = len(keytiles)
                    staged = []
                    LA = 3
                    for idx in range(nk + LA):
                        if idx < nk:
                            j = keytiles[idx]
                            pss, psk = pssr.next()
                            masked = (kind == 0) and isx and j >= 2
                            kcs = slice(j * 128, (j + 1) * 128)
                            ph.op('pe', lambda e, pss=pss, rows=rows, g=g, kcs=kcs, masked=masked: e.matmul(
                                pss[:, :n], k2[rows, g, kcs], q2[rows, g, t0:t0 + n], start=True, stop=(not masked)),
                                reads=['k2', 'q2'], writes=[psk])
                            if masked:
                                o = j - qt0 + 1
                                ph.op('pe', lambda e, pss=pss, o=o: e.matmul(
                                    pss[:, :n], s.ident_bf[:], wm[:, o, :n], start=False, stop=True),
                                    reads=['wm', 'ident_bf'], writes=[psk])
                            p_, pk_ = pr.next()
                            ph.op('act', lambda e, pss=pss, p_=p_: e.activation(out=p_[:, :n], in_=pss[:, :n], func=AF.Exp),
                                  reads=[psk], writes=[pk_])
                            staged.append((p_, pk_, j))
                        if idx == LA - 1 or (nk < LA and idx == nk - 1):
                            for fn_ in pending:
                                fn_()
                            pending.clear()
                        i2 = idx - LA
                        if i2 >= 0:
                            p2, pk2, j2 = staged[i2]
                            ph.op('pe', lambda e, po=po, p2=p2, j2=j2, g=g, i2=i2, nk=nk: e.matmul(
                                po[0:65, :n], vaug[:, j2, g, :], p2[:, :n], start=(i2 == 0), stop=(i2 == nk - 1)),
                                reads=['vaug', pk2], writes=[pok])
                    add = s.esink[64:65, h:h + 1] if kind == 0 else 0.0
                    ph.op('dve', lambda e, po=po, add=add: e.tensor_scalar(
                        out=rden[64:65, :n], in0=po[64:65, :n], scalar1=add, scalar2=None, op0=ALU.add),
                        reads=[pok, 'esink'], writes=['rden'])
                    ph.op('dve', lambda e: e.reciprocal(out=rden[64:65, :n], in_=rden[64:65, :n]), reads=['rden'], writes=['rden'])

                    def tail(po=po, pok=pok, g=g, rows=rows, h=h, t0=t0, n=n):
                        ph.op('pe', lambda e: e.matmul(PS[6][0:64, :n], s.ones_f[64:65, 0:64], rden[64:65, :n], start=True, stop=True),
                              reads=['rden', 'ones_f'], writes=[('ps', 6)])
                        osb, osk = osbr.next()
                        ph.op('act', lambda e, osb=osb: e.activation(out=osb[0:64, :n], in_=po[0:64, :n], func=AF.Copy),
                              reads=[pok], writes=[osk])
                        yst, ysk = ystr.next()
                        ph.op('dve', lambda e, osb=osb, yst=yst: e.tensor_tensor(
                            out=yst[0:64, :n], in0=osb[0:64, :n], in1=PS[6][0:64, :n], op=ALU.mult),
                            reads=[osk, ('ps', 6)], writes=[ysk])
                        ph.op('sp', lambda e, yst=yst: e.dma_start(
                            out=s.YT[yrow + g, rows, t0:t0 + n], in_=yst[0:64, :n]), reads=[ysk], writes=[('YT', kind, h, t0)])

                    pending.append(tail)
            for fn_ in pending:
                fn_()
            pending.clear()
            done(s, ph)


def phase_out(s, l, b):
    T, NB = s.T, s.NB
    PS = s.PS
    with contextlib.ExitStack() as st:
        wo = sb(s, "owo", [128, KC, D], BF16, st)
        ytr = Rot([sb(s, f"oyt{i}", [128, KC, 512], BF16, st) for i in range(2)], "oyt")
        xsr = Rot([sb(s, f"oxs{i}", [128, KC, 512], F32, st) for i in range(2)], "oxs")
        x2r = Rot([sb(s, f"ox2{i}", [128, KC, 512], F32, st) for i in range(2)], "ox2")
        hTr = Rot([sb(s, f"ohT{i}", [128, KC, 512], BF16, st) for i in range(2)], "ohT")
        sqr = Rot([sb(s, f"osq{i}", [128, 512], BF16, st) for i in range(2)], "osq")
        rstd = sb(s, "orstd", [128, 512], F32, st)
        tmpr = Rot([sb(s, f"otmp{i}", [128, 512], F32, st) for i in range(2)], "otmp")
        rtr = Rot([sb(s, f"ort{i}", [128, 8, 16], F32, st) for i in range(2)], "ort")
        hrr = Rot([sb(s, f"ohr{i}", [128, D], BF16, st) for i in range(2)], "ohr")
        psr = Rot(PS[0:4], "ps")
        ph = new_phase(s, "o")
        ph.op('pq', lambda e: e.dma_start(out=wo[:], in_=s.w_out[l].rearrange("(k p) n -> p k n", p=128)), writes=['wo'])
        for (t0, n) in s.chunks:
            isx = t0 >= CTX
            bcol = b if isx else NB
            yt_, ytk = ytr.next()
            ph.op('sp', lambda e, yt_=yt_, t0=t0, n=n: e.dma_start(
                out=yt_[:, :, :n], in_=s.YT[:, :, t0:t0 + n].rearrange("k p t -> p k t")), writes=[ytk])
            xs, xk = xsr.next()
            ph.op('sp', lambda e, xs=xs, t0=t0, n=n: e.dma_start(
                out=xs[:, :, :n], in_=s.XT[b, :, :, t0:t0 + n].rearrange("k p t -> p k t")), writes=[xk])
            x2, x2k = x2r.next()
            for nn in range(KC):
                ps, pk = psr.next()
                for k in range(KC):
                    ph.op('pe', lambda e, ps=ps, nn=nn, k=k, yt_=yt_: e.matmul(
                        ps[:, :n], wo[:, k, nn * 128:(nn + 1) * 128], yt_[:, k, :n], start=(k == 0), stop=(k == KC - 1)),
                        reads=['wo', ytk], writes=[pk])
                ph.op('dve', lambda e, ps=ps, nn=nn, x2=x2, xs=xs: e.scalar_tensor_tensor(
                    out=x2[:, nn, :n], in0=ps[:, :n], scalar=s.modT[:, l, 16 + nn, bcol:bcol + 1], in1=xs[:, nn, :n],
                    op0=ALU.mult, op1=ALU.add), reads=[pk, xk], writes=[x2k])
            ph.op('sp', lambda e, x2=x2, t0=t0, n=n: e.dma_start(
                out=s.XT[b, :, :, t0:t0 + n].rearrange("k p t -> p k t"), in_=x2[:, :, :n]), reads=[x2k], writes=[('XT', t0)])
            hT, hk = hTr.next()
            norm_mod(s, ph, x2, x2k, n, s.gs2, 24, l, bcol, hT, hk, sqr, rstd, tmpr)
            for i in range(n // 128):
                ps, pk = psr.next()
                for k in range(KC):
                    ph.op('pe', lambda e, ps=ps, k=k, i=i, hT=hT: e.matmul(
                        ps[:, 0:NE], hT[:, k, i * 128:(i + 1) * 128], s.wr_bf[:, k, :], start=(k == 0), stop=(k == KC - 1)),
                        reads=[hk, 'wr'], writes=[pk])
                rt, rk = rtr.next()
                R = lambda j: rt[:, j, :]
                G = lambda j: rt[:, j, :].rearrange("p (g e) -> p g e", g=4)
                ph.op('act', lambda e, ps=ps, rt=rt: e.activation(out=rt[:, 0, :], in_=ps[:, 0:NE], func=AF.Sigmoid),
                      reads=[pk], writes=[rk])
                seqops = [
                    lambda e, rt=rt: e.tensor_tensor(out=rt[:, 1, :], in0=rt[:, 0, :], in1=s.br_bc[:], op=ALU.add),
                    lambda e, rt=rt: e.tensor_reduce(out=rt[:, 2, 0:4], in_=rt[:, 1, :].rearrange("p (g e) -> p g e", g=4),
                                                     axis=AX.X, op=ALU.max),
                    lambda e, rt=rt: e.tensor_tensor(out=rt[:, 3, :].rearrange("p (g e) -> p g e", g=4),
                                                     in0=rt[:, 1, :].rearrange("p (g e) -> p g e", g=4),
                                                     in1=rt[:, 2, 0:4].unsqueeze(2).broadcast_to([128, 4, 4]), op=ALU.is_equal),
                    lambda e, rt=rt: e.scalar_tensor_tensor(out=rt[:, 3, :], in0=rt[:, 3, :], scalar=-1e9, in1=rt[:, 1, :],
                                                            op0=ALU.mult, op1=ALU.add),
                    lambda e, rt=rt: e.tensor_reduce(out=rt[:, 2, 4:8], in_=rt[:, 3, :].rearrange("p (g e) -> p g e", g=4),
                                                     axis=AX.X, op=ALU.max),
                    lambda e, rt=rt: e.tensor_tensor(out=rt[:, 2, 8:12], in0=rt[:, 2, 0:4], in1=rt[:, 2, 4:8], op=ALU.add),
                    lambda e, rt=rt: e.tensor_reduce(out=rt[:, 2, 12:13], in_=rt[:, 2, 8:12], axis=AX.X, op=ALU.max),
                    lambda e, rt=rt: e.tensor_scalar(out=rt[:, 4, 0:4], in0=rt[:, 2, 8:12], scalar1=rt[:, 2, 12:13], scalar2=None,
                                                     op0=ALU.is_ge),
                    lambda e, rt=rt: e.tensor_tensor(out=rt[:, 5, :].rearrange("p (g e) -> p g e", g=4),
                                                     in0=rt[:, 1, :].rearrange("p (g e) -> p g e", g=4),
                                                     in1=rt[:, 2, 4:8].unsqueeze(2).broadcast_to([128, 4, 4]), op=ALU.is_ge),
                    lambda e, rt=rt: e.tensor_tensor(out=rt[:, 5, :].rearrange("p (g e) -> p g e", g=4),
                                                     in0=rt[:, 5, :].rearrange("p (g e) -> p g e", g=4),
                                                     in1=rt[:, 4, 0:4].unsqueeze(2).broadcast_to([128, 4, 4]), op=ALU.mult),
                    lambda e, rt=rt: e.tensor_tensor(out=rt[:, 6, :], in0=rt[:, 5, :], in1=rt[:, 0, :], op=ALU.mult),
                    lambda e, rt=rt: e.tensor_reduce(out=rt[:, 4, 4:5], in_=rt[:, 6, :], axis=AX.X, op=ALU.add),
                    lambda e, rt=rt: e.reciprocal(out=rt[:, 4, 4:5], in_=rt[:, 4, 4:5]),
                    lambda e, rt=rt: e.tensor_scalar(out=rt[:, 7, :], in0=rt[:, 6, :], scalar1=rt[:, 4, 4:5], scalar2=None,
                                                     op0=ALU.mult),
                ]
                for fn in seqops:
                    ph.op('dve', fn, reads=[rk], writes=[rk])
                ti = b * s.NT + t0 // 128 + i
                ph.op('dve', lambda e, rt=rt, ti=ti: e.tensor_copy(out=s.SELt[:, ti, :], in_=rt[:, 5, :]), reads=[rk], writes=[('selt', ti)])
                ph.op('dve', lambda e, rt=rt, ti=ti: e.tensor_copy(out=s.WGt[:, ti, :], in_=rt[:, 7, :]), reads=[rk], writes=[('wgt', ti)])
                for c in range(KC):
                    ph.op('pe', lambda e, hT=hT, c=c, i=i: e.transpose(
                        s.PSB[:, c * 128:(c + 1) * 128], hT[:, c, i * 128:(i + 1) * 128], s.ident_bf[:]),
                        reads=[hk, 'ident_bf'], writes=['psb'])
                hr, hrk = hrr.next()
                ph.op('act', lambda e, hr=hr: e.activation(out=hr[:], in_=s.PSB[:], func=AF.Copy), reads=['psb'], writes=[hrk])
                gt = b * T + t0 + i * 128
                ph.op('sp', lambda e, hr=hr, gt=gt: e.dma_start(out=s.H2TOK[gt:gt + 128, :], in_=hr[:]), reads=[hrk], writes=[('H2TOK', gt)])
                pt, ptk = psr.next()
                ph.op('pe', lambda e, pt=pt, rt=rt: e.matmul(pt[:, 0:NE], s.ustrict[:], rt[:, 5, :], start=True, stop=True),
                      reads=[rk, 'ustrict'], writes=[ptk])
                ph.op('pe', lambda e, pt=pt, rt=rt: e.matmul(pt[:, NE:2 * NE], s.ones_f[:], rt[:, 5, :], start=True, stop=True),
                      reads=[rk, 'ones_f'], writes=[ptk])
                ph.op('dve', lambda e, pt=pt, ti=ti: e.tensor_tensor(out=s.RKt[:, ti, :], in0=pt[:, 0:NE], in1=s.cbase[:], op=ALU.add),
                      reads=[ptk, 'cbase'], writes=[('rkt', ti)])
                ph.op('dve', lambda e, pt=pt: e.tensor_tensor(out=s.cbase[:], in0=pt[:, NE:2 * NE], in1=s.cbase[:], op=ALU.add),
                      reads=[ptk, 'cbase'], writes=['cbase'])
        done(s, ph)


def phase_moe(s, l):
    T, NB, DEPTH = s.T, s.NB, s.DEPTH
    PS = s.PS
    supers = []
    for b in range(NB):
        cur, tot = [], 0
        for (t0, n) in s.chunks:
            if tot + n > 1280:
                supers.append((b, cur))
                cur, tot = [], 0
            cur.append((t0, n))
            tot += n
        if cur:
            supers.append((b, cur))
    with contextlib.ExitStack() as st:
        acc = sb(s, "macc", [128, KC, 1280], F32, st)
        wgr = Rot([sb(s, f"mwg{i}", [128, KC, 2 * D], BF16, st) for i in range(2)], "mwg")
        wdr = Rot([sb(s, f"mwd{i}", [128, KC, D], BF16, st) for i in range(1)], "mwd")
        h2r = Rot([sb(s, f"mh2{i}", [128, KC, 512], BF16, st) for i in range(2)], "mh2")
        wtr = Rot([sb(s, f"mwt{i}", [16, 512], F32, st) for i in range(2)], "mwt")
        wbr = Rot([sb(s, f"mwb{i}", [128, 512], F32, st) for i in range(2)], "mwb")
        sar = Rot([sb(s, f"msa{i}", [128, 512], F32, st) for i in range(2)], "msa")
        ttr = Rot([sb(s, f"mtt{i}", [128, 512], F32, st) for i in range(2)], "mtt")
        aTr = Rot([sb(s, f"maT{i}", [128, KC, 512], BF16, st) for i in range(1)], "maT")
        xsr = Rot([sb(s, f"mxs{i}", [128, KC, 128], F32, st) for i in range(2)], "mxs")
        otr = Rot([sb(s, f"mot{i}", [128, D], F32, st) for i in range(2)], "mot")
        psr = Rot(PS[0:6], "ps")
        for (b, chs) in supers:
            ph = new_phase(s, "m")
            for ex in range(NE):
                wg, wgk = wgr.next()
                wd, wdk = wdr.next()
                for hf in range(2):
                    ph.op('pq', lambda e, wg=wg, ex=ex, hf=hf: e.dma_start(
                        out=wg[:, :, hf * D:(hf + 1) * D],
                        in_=s.w_gu[l, ex].rearrange("(k p) n -> p k n", p=128)[:, :, hf * D:(hf + 1) * D]), writes=[wgk])
                ph.op('pq', lambda e, wd=wd, ex=ex: e.dma_start(
                    out=wd[:], in_=s.w_dn[l, ex].rearrange("(k p) n -> p k n", p=128)), writes=[wdk])
                off = 0
                for (t0, n) in chs:
                    gt = b * T + t0
                    h2, h2k = h2r.next()
                    ph.op('sp', lambda e, h2=h2, gt=gt, n=n: e.dma_start(
                        out=h2[:, :, :n], in_=s.H2T[:, :, gt:gt + n].rearrange("k p t -> p k t")), writes=[h2k])
                    wt, wtk = wtr.next()
                    ph.op('sp', lambda e, wt=wt, gt=gt, n=n: e.dma_start(out=wt[0:NE, :n], in_=s.WGT[:, gt:gt + n]), writes=[wtk])
                    ph.op('pe', lambda e, wt=wt, ex=ex, n=n: e.matmul(
                        PS[6][:, :n], s.sel_sb[0:NE, ex * 128:(ex + 1) * 128], wt[0:NE, :n], start=True, stop=True),
                        reads=[wtk, 'sel'], writes=[('ps', 6)])
                    wb, wbk = wbr.next()
                    ph.op('act', lambda e, wb=wb, n=n: e.activation(out=wb[:, :n], in_=PS[6][:, :n], func=AF.Copy),
                          reads=[('ps', 6)], writes=[wbk])
                    aT, aTk = aTr.next()
                    for f in range(KC):
                        pa, pak = psr.next()
                        pu, puk = psr.next()
                        for k in range(KC):
                            ph.op('pe', lambda e, pa=pa, wg=wg, h2=h2, f=f, k=k, n=n: e.matmul(
                                pa[:, :n], wg[:, k, f * 128:(f + 1) * 128], h2[:, k, :n], start=(k == 0), stop=(k == KC - 1)),
                                reads=[wgk, h2k], writes=[pak])
                        for k in range(KC):
                            ph.op('pe', lambda e, pu=pu, wg=wg, h2=h2, f=f, k=k, n=n: e.matmul(
                                pu[:, :n], wg[:, k, D + f * 128:D + (f + 1) * 128], h2[:, k, :n], start=(k == 0), stop=(k == KC - 1)),
                                reads=[wgk, h2k], writes=[puk])
                        sa, sak = sar.next()
                        ph.op('act', lambda e, pa=pa, sa=sa, n=n: e.activation(out=sa[:, :n], in_=pa[:, :n], func=AF.Silu),
                              reads=[pak], writes=[sak])
                        tt, ttk = ttr.next()
                        ph.op('dve', lambda e, pu=pu, sa=sa, tt=tt, n=n: e.tensor_tensor(
                            out=tt[:, :n], in0=pu[:, :n], in1=sa[:, :n], op=ALU.mult), reads=[puk, sak], writes=[ttk])
                        ph.op('pool', lambda e, tt=tt, wb=wb, aT=aT, f=f, n=n: e.tensor_tensor(
                            out=aT[:, f, :n], in0=tt[:, :n], in1=wb[:, :n], op=ALU.mult), reads=[ttk, wbk], writes=[(aTk, f)])
                    for nn in range(KC):
                        py, pyk = psr.next()
                        for f in range(KC):
                            ph.op('pe', lambda e, py=py, wd=wd, aT=aT, f=f, nn=nn, n=n: e.matmul(
                                py[:, :n], wd[:, f, nn * 128:(nn + 1) * 128], aT[:, f, :n], start=(f == 0), stop=(f == KC - 1)),
                                reads=[wdk, (aTk, f)], writes=[pyk])
                        if ex == 0:
                            ph.op('dve', lambda e, py=py, nn=nn, off=off, n=n: e.tensor_copy(
                                out=acc[:, nn, off:off + n], in_=py[:, :n]), reads=[pyk], writes=[('acc', nn, off)])
                        else:
                            ph.op('dve', lambda e, py=py, nn=nn, off=off, n=n: e.tensor_tensor(
                                out=acc[:, nn, off:off + n], in0=acc[:, nn, off:off + n], in1=py[:, :n], op=ALU.add),
                                reads=[pyk, ('acc', nn, off)], writes=[('acc', nn, off)])
                    off += n
            off = 0
            for (t0, n) in chs:
                isx = t0 >= CTX
                bcol = b if isx else NB
                for i in range(n // 128):
                    tt0 = t0 + i * 128
                    xs, xk = xsr.next()
                    ph.op('sp', lambda e, xs=xs, tt0=tt0: e.dma_start(
                        out=xs[:], in_=s.XT[b, :, :, tt0:tt0 + 128].rearrange("k p t -> p k t")), writes=[xk])
                    for nn in range(KC):
                        ph.op('dve', lambda e, xs=xs, nn=nn, o=off + i * 128: e.scalar_tensor_tensor(
                            out=xs[:, nn, :], in0=acc[:, nn, o:o + 128], scalar=s.modT[:, l, 40 + nn, bcol:bcol + 1],
                            in1=xs[:, nn, :], op0=ALU.mult, op1=ALU.add),
                            reads=[xk] + [('acc', nn, oo) for oo in set([off])], writes=[xk])
                    if l < DEPTH - 1:
                        ph.op('sp', lambda e, xs=xs, tt0=tt0: e.dma_start(
                            out=s.XT[b, :, :, tt0:tt0 + 128].rearrange("k p t -> p k t"), in_=xs[:]), reads=[xk], writes=[('XTo', tt0)])
                    else:
                        ot, otk = otr.next()
                        for half in range(2):
                            pt, ptk = psr.next()
                            for j in range(4):
                                c = half * 4 + j
                                ph.op('pe', lambda e, pt=pt, xs=xs, j=j, c=c: e.transpose(
                                    pt[:, j * 128:(j + 1) * 128], xs[:, c, :], s.ident[:]), reads=[xk, 'ident'], writes=[ptk])
                            if half == 0:
                                ph.op('act', lambda e, pt=pt, ot=ot: e.activation(out=ot[:, 0:512], in_=pt[:], func=AF.Copy),
                                      reads=[ptk], writes=[(otk, 0)])
                            else:
                                ph.op('dve', lambda e, pt=pt, ot=ot: e.tensor_copy(out=ot[:, 512:1024], in_=pt[:]),
                                      reads=[ptk], writes=[(otk, 1)])
                        dst = s.out_x[b, tt0 - CTX:tt0 - CTX + 128, :] if isx else s.out_c[b, tt0:tt0 + 128, :]
                        ph.op('sp', lambda e, ot=ot, dst=dst: e.dma_start(out=dst, in_=ot[:]),
                              reads=[(otk, 0), (otk, 1)], writes=[('out', tt0)])
                off += n
            done(s, ph)


def phase_dest(s, l):
    NTA, NBLK = s.NTA, s.NBLK
    with contextlib.ExitStack() as st:
        r_ = sb(s, "dr", [128, NE], F32, st)
        g_ = sb(s, "dg", [128, NE], F32, st)
        pad = sb(s, "dpad", [128, NE], F32, st)
        pst_ = sb(s, "dpst", [128, NE], F32, st)
        pend = sb(s, "dpend", [128, NE], F32, st)
        t1r = Rot([sb(s, f"dt1{i}", [128, NE], F32, st) for i in range(2)], "dt1")
        t2r = Rot([sb(s, f"dt2{i}", [128, NE], F32, st) for i in range(2)], "dt2")
        dstr = Rot([sb(s, f"ddst{i}", [128, NE], F32, st) for i in range(2)], "ddst")
        m1r = Rot([sb(s, f"dm1{i}", [128, 2], F32, st) for i in range(2)], "dm1")
        eacc = sb(s, "deacc", [128, 64], F32, st)
        hrr = Rot([sb(s, f"dhr{i}", [128, D], BF16, st) for i in range(3)], "dhr")
        ph = new_phase(s, "d")
        ki = sb(s, "dki", [128, NE], I32, st)
        ph.op('dve', lambda e: e.tensor_scalar(out=r_[:], in0=s.cbase[:], scalar1=float(MB - 1), scalar2=1.0 / MB, op0=ALU.add, op1=ALU.mult),
              writes=['r'])
        ph.op('dve', lambda e: e.tensor_copy(out=ki[:], in_=r_[:]), reads=['r'], writes=['ki'])
        ph.op('dve', lambda e: e.tensor_copy(out=g_[:], in_=ki[:]), reads=['ki'], writes=['g'])
        ph.op('dve', lambda e: e.tensor_tensor(out=pad[:], in0=g_[:], in1=r_[:], op=ALU.is_gt), reads=['g', 'r'], writes=['pad'])
        ph.op('dve', lambda e: e.tensor_tensor(out=g_[:], in0=g_[:], in1=pad[:], op=ALU.subtract), reads=['g', 'pad'], writes=['g'])
        ph.op('dve', lambda e: e.tensor_scalar(out=pad[:], in0=g_[:], scalar1=float(MB), scalar2=None, op0=ALU.mult), reads=['g'], writes=['pad'])
        ph.op('dve', lambda e: e.memset(pst_[:], 0.0), writes=['pst'])
        for ex in range(1, NE):
            ph.op('dve', lambda e, ex=ex: e.tensor_tensor(out=pst_[:, ex:ex + 1], in0=pst_[:, ex - 1:ex], in1=pad[:, ex - 1:ex], op=ALU.add),
                  reads=['pst', 'pad'], writes=['pst'])
        ph.op('dve', lambda e: e.tensor_tensor(out=pend[:], in0=pst_[:], in1=pad[:], op=ALU.add), reads=['pst', 'pad'], writes=['pend'])
        for ti in range(NTA):
            dst, dk = dstr.next()
            ph.op('dve', lambda e, dst=dst, ti=ti: e.tensor_tensor(out=dst[:], in0=s.RKt[:, ti, :], in1=pst_[:], op=ALU.add),
                  reads=['pst'], writes=[dk])
            t1, t1k = t1r.next()
            ph.op('dve', lambda e, dst=dst, t1=t1, ti=ti: e.scalar_tensor_tensor(
                out=t1[:], in0=dst[:], scalar=1.0, in1=s.SELt[:, ti, :], op0=ALU.add, op1=ALU.mult), reads=[dk], writes=[t1k])
            m1, m1k = m1r.next()
            ph.op('dve', lambda e, t1=t1, m1=m1: e.tensor_reduce(out=m1[:, 0:1], in_=t1[:], axis=AX.X, op=ALU.max), reads=[t1k], writes=[m1k])
            t2, t2k = t2r.next()
            ph.op('dve', lambda e, dst=dst, t2=t2, ti=ti: e.scalar_tensor_tensor(
                out=t2[:], in0=s.SELt[:, ti, :], scalar=-1.0e6, in1=dst[:], op0=ALU.mult, op1=ALU.add), reads=[dk], writes=[t2k])
            ph.op('dve', lambda e, t2=t2, m1=m1: e.tensor_reduce(out=m1[:, 1:2], in_=t2[:], axis=AX.X, op=ALU.min), reads=[t2k, m1k], writes=[m1k])
            ph.op('dve', lambda e, m1=m1, ti=ti: e.tensor_scalar(out=s.DAf[:, ti:ti + 1], in0=m1[:, 0:1], scalar1=-1.0, scalar2=None, op0=ALU.add),
                  reads=[m1k], writes=['daf'])
            ph.op('dve', lambda e, m1=m1, ti=ti: e.tensor_scalar(out=s.DBf[:, ti:ti + 1], in0=m1[:, 1:2], scalar1=1.0e6, scalar2=None, op0=ALU.add),
                  reads=[m1k], writes=['dbf'])
            ph.op('dve', lambda e, t1=t1, m1=m1: e.tensor_scalar(out=t1[:], in0=t1[:], scalar1=m1[:, 0:1], scalar2=None, op0=ALU.is_equal),
                  reads=[t1k, m1k], writes=[t1k])
            ph.op('dve', lambda e, t1=t1, ti=ti: e.tensor_tensor(out=t1[:], in0=t1[:], in1=s.WGt[:, ti, :], op=ALU.mult), reads=[t1k], writes=[t1k])
            ph.op('dve', lambda e, t1=t1, ti=ti: e.tensor_reduce(out=s.WA[:, ti:ti + 1], in_=t1[:], axis=AX.X, op=ALU.add), reads=[t1k], writes=['wa'])
        ph.op('dve', lambda e: e.tensor_scalar(out=s.WB[:], in0=s.WA[:], scalar1=-1.0, scalar2=1.0, op0=ALU.mult, op1=ALU.add),
              reads=['wa'], writes=['wb'])
        ph.op('dve', lambda e: e.tensor_copy(out=s.DAi[:], in_=s.DAf[:]), reads=['daf'], writes=['dai'])
        ph.op('dve', lambda e: e.tensor_copy(out=s.DBi[:], in_=s.DBf[:]), reads=['dbf'], writes=['dbi'])
        ph.op('dve', lambda e: e.memset(eacc[:], 0.0), writes=['eacc'])
        for ex in range(NE):
            ph.op('dve', lambda e, ex=ex: e.scalar_tensor_tensor(
                out=eacc[:], in0=s.blkiota[:], scalar=pend[:, ex:ex + 1], in1=eacc[:], op0=ALU.is_ge, op1=ALU.add),
                reads=['pend', 'eacc'], writes=['eacc'])
        ph.op('dve', lambda e: e.tensor_scalar(out=eacc[:], in0=eacc[:], scalar1=float(NE - 1), scalar2=128.0, op0=ALU.min, op1=ALU.mult),
              reads=['eacc'], writes=['eacc'])
        ph.op('dve', lambda e: e.tensor_scalar(out=eacc[:], in0=eacc[:], scalar1=s.pidx[:, 0:1], scalar2=None, op0=ALU.add),
              reads=['eacc'], writes=['eacc'])
        ph.op('dve', lambda e: e.tensor_copy(out=s.IDXi[:], in_=eacc[:]), reads=['eacc'], writes=['idxi'])
        for ti in range(NTA):
            hr, hrk = hrr.next()
            ph.op('sp', lambda e, hr=hr, ti=ti: e.dma_start(out=hr[:], in_=s.H2TOK[ti * 128:(ti + 1) * 128, :]), writes=[hrk])
            for (dd, dn) in ((s.DAi, 'dai'), (s.DBi, 'dbi')):
                ph.op('pq', lambda e, hr=hr, ti=ti, dd=dd: e.indirect_dma_start(
                    out=s.XP, out_offset=bass.IndirectOffsetOnAxis(ap=dd[:, ti:ti + 1], axis=0), in_=hr[:, :], in_offset=None),
                    reads=[hrk, dn], writes=[('XPs', ti, dn)])
        done(s, ph)


def phase_moe_sparse(s, l):
    NBLK = s.NBLK
    PS = s.PS
    with contextlib.ExitStack() as st:
        wgr = Rot([sb(s, f"swg{i}", [128, KC, 2 * D], BF16, st) for i in range(2)], "swg")
        wdr = Rot([sb(s, f"swd{i}", [128, KC, D], BF16, st) for i in range(2)], "swd")
        xrr = Rot([sb(s, f"sxr{i}", [128, D], BF16, st) for i in range(4)], "sxr")
        xpr = Rot([sb(s, f"sxp{i}", [128, KC, MB], BF16, st) for i in range(2)], "sxp")
        sar = Rot([sb(s, f"ssa{i}", [128, MB], F32, st) for i in range(2)], "ssa")
        aTr = Rot([sb(s, f"saT{i}", [128, KC, MB], BF16, st) for i in range(2)], "saT")
        ypr = Rot([sb(s, f"syp{i}", [128, D], BF16, st) for i in range(2)], "syp")
        psr = Rot(PS[0:7], "ps")
        ph = new_phase(s, "s")
        for j in range(NBLK):
            wg, wgk = wgr.next()
            wd, wdk = wdr.next()
            ph.op('pq', lambda e, wg=wg, j=j: e.indirect_dma_start(
                out=wg[:].rearrange("p k n -> p (k n)"), out_offset=None, in_=s.WBgu[l % 2].rearrange("e p k n -> (e p) (k n)"),
                in_offset=bass.IndirectOffsetOnAxis(ap=s.IDXi[:, j:j + 1], axis=0)), writes=[wgk])
            ph.op('pq', lambda e, wd=wd, j=j: e.indirect_dma_start(
                out=wd[:].rearrange("p k n -> p (k n)"), out_offset=None, in_=s.WBdn[l % 2].rearrange("e p k n -> (e p) (k n)"),
                in_offset=bass.IndirectOffsetOnAxis(ap=s.IDXi[:, j:j + 1], axis=0)), writes=[wdk])
            if l + 1 < s.DEPTH:
                per = -(-2 * NE // NBLK)
                for i in range(j * per, min(2 * NE, (j + 1) * per)):
                    stage_weight(s, ph, l + 1, i)
            xp, xpk = xpr.next()
            for sub in range(MB // 128):
                xr, xrk = xrr.next()
                r0 = j * MB + sub * 128
                ph.op('sp', lambda e, xr=xr, r0=r0: e.dma_start(out=xr[:], in_=s.XP[r0:r0 + 128, :]), writes=[xrk])
                for c in range(KC):
                    ph.op('pe', lambda e, xr=xr, c=c: e.transpose(
                        s.PSB[:, c * 128:(c + 1) * 128], xr[:, c * 128:(c + 1) * 128], s.ident_bf[:]),
                        reads=[xrk, 'ident_bf'], writes=['psb'])
                if sub % 2 == 0:
                    ph.op('act', lambda e, xp=xp, sub=sub: e.activation(
                        out=xp[:, :, sub * 128:(sub + 1) * 128], in_=s.PSB[:].rearrange("p (c t) -> p c t", c=KC), func=AF.Copy),
                        reads=['psb'], writes=[(xpk, sub)])
                else:
                    ph.op('dve', lambda e, xp=xp, sub=sub: e.tensor_copy(
                        out=xp[:, :, sub * 128:(sub + 1) * 128], in_=s.PSB[:].rearrange("p (c t) -> p c t", c=KC)),
                        reads=['psb'], writes=[(xpk, sub)])
            xpkeys = [(xpk, sub) for sub in range(MB // 128)]
            aT, aTk = aTr.next()
            for f in range(KC):
                pa, pak = psr.next()
                pu, puk = psr.next()
                for k in range(KC):
                    ph.op('pe', lambda e, pa=pa, wg=wg, xp=xp, f=f, k=k: e.matmul(
                        pa[:], wg[:, k, f * 128:(f + 1) * 128], xp[:, k, :], start=(k == 0), stop=(k == KC - 1)),
                        reads=[wgk] + xpkeys, writes=[pak])
                for k in range(KC):
                    ph.op('pe', lambda e, pu=pu, wg=wg, xp=xp, f=f, k=k: e.matmul(
                        pu[:], wg[:, k, D + f * 128:D + (f + 1) * 128], xp[:, k, :], start=(k == 0), stop=(k == KC - 1)),
                        reads=[wgk] + xpkeys, writes=[puk])
                sa, sak = sar.next()
                ph.op('act', lambda e, pa=pa, sa=sa: e.activation(out=sa[:], in_=pa[:], func=AF.Silu), reads=[pak], writes=[sak])
                ph.op('dve', lambda e, pu=pu, sa=sa, aT=aT, f=f: e.tensor_tensor(out=aT[:, f, :], in0=pu[:], in1=sa[:], op=ALU.mult),
                      reads=[puk, sak], writes=[(aTk, f)])
            aTkeys = [(aTk, f) for f in range(KC)]
            for sub in range(MB // 128):
                yp, ypk = ypr.next()
                for nh in range(2):
                    py, pyk = psr.next()
                    for f in range(KC):
                        ph.op('pe', lambda e, py=py, wd=wd, aT=aT, f=f, nh=nh, sub=sub: e.matmul(
                            py[:], aT[:, f, sub * 128:(sub + 1) * 128], wd[:, f, nh * 512:(nh + 1) * 512], start=(f == 0), stop=(f == KC - 1)),
                            reads=[wdk] + aTkeys, writes=[pyk])
                    if nh == 0:
                        ph.op('act', lambda e, py=py, yp=yp: e.activation(out=yp[:, 0:512], in_=py[:], func=AF.Copy), reads=[pyk], writes=[(ypk, 0)])
                    else:
                        ph.op('dve', lambda e, py=py, yp=yp: e.tensor_copy(out=yp[:, 512:1024], in_=py[:]), reads=[pyk], writes=[(ypk, 1)])
                r0 = j * MB + sub * 128
                ph.op('sp', lambda e, yp=yp, r0=r0: e.dma_start(out=s.YP[r0:r0 + 128, :], in_=yp[:]), reads=[(ypk, 0), (ypk, 1)], writes=[('YP', r0)])
        done(s, ph)


def phase_comb(s, l):
    T, NB, NT, DEPTH = s.T, s.NB, s.NT, s.DEPTH
    PS = s.PS
    with contextlib.ExitStack() as st:
        yar = Rot([sb(s, f"cya{i}", [128, D], BF16, st) for i in range(3)], "cya")
        ybr = Rot([sb(s, f"cyb{i}", [128, D], BF16, st) for i in range(3)], "cyb")
        ysr = Rot([sb(s, f"cys{i}", [128, D], F32, st) for i in range(2)], "cys")
        xsr = Rot([sb(s, f"cxs{i}", [128, KC, 128], F32, st) for i in range(2)], "cxs")
        otr = Rot([sb(s, f"cot{i}", [128, D], F32, st) for i in range(2)], "cot")
        psr = Rot(PS[0:6], "ps")
        ph = new_phase(s, "k")
        for b in range(NB):
            for i in range(NT):
                ti = b * NT + i
                tt0 = i * 128
                isx = tt0 >= CTX
                bcol = b if isx else NB
                ya, yak = yar.next()
                yb, ybk = ybr.next()
                ph.op('pq', lambda e, ya=ya, ti=ti: e.indirect_dma_start(
                    out=ya[:, :], out_offset=None, in_=s.YP, in_offset=bass.IndirectOffsetOnAxis(ap=s.DAi[:, ti:ti + 1], axis=0)), writes=[yak])
                ph.op('pq', lambda e, yb=yb, ti=ti: e.indirect_dma_start(
                    out=yb[:, :], out_offset=None, in_=s.YP, in_offset=bass.IndirectOffsetOnAxis(ap=s.DBi[:, ti:ti + 1], axis=0)), writes=[ybk])
                ys, ysk = ysr.next()
                ph.op('pool', lambda e, ya=ya, ys=ys, ti=ti: e.tensor_scalar(out=ys[:], in0=ya[:], scalar1=s.WA[:, ti:ti + 1], scalar2=None, op0=ALU.mult),
                      reads=[yak], writes=[ysk])
                ph.op('dve', lambda e, ys=ys, yb=yb, ti=ti: e.scalar_tensor_tensor(
                    out=ys[:], in0=yb[:], scalar=s.WB[:, ti:ti + 1], in1=ys[:], op0=ALU.mult, op1=ALU.add), reads=[ysk, ybk], writes=[ysk])
                xs, xk = xsr.next()
                ph.op('sp', lambda e, xs=xs, tt0=tt0, b=b: e.dma_start(
                    out=xs[:], in_=s.XT[b, :, :, tt0:tt0 + 128].rearrange("k p t -> p k t")), writes=[xk])
                for half in range(2):
                    pt, ptk = psr.next()
                    for jj in range(4):
                        c = half * 4 + jj
                        ph.op('pe', lambda e, pt=pt, ys=ys, jj=jj, c=c: e.transpose(
                            pt[:, jj * 128:(jj + 1) * 128], ys[:, c * 128:(c + 1) * 128], s.ident[:]), reads=[ysk, 'ident'], writes=[ptk])
                    for jj in range(4):
                        c = half * 4 + jj
                        ph.op('dve', lambda e, pt=pt, xs=xs, jj=jj, c=c, bcol=bcol: e.scalar_tensor_tensor(
                            out=xs[:, c, :], in0=pt[:, jj * 128:(jj + 1) * 128], scalar=s.modT[:, l, 40 + c, bcol:bcol + 1],
                            in1=xs[:, c, :], op0=ALU.mult, op1=ALU.add), reads=[ptk, xk], writes=[xk])
                if l < DEPTH - 1:
                    ph.op('sp', lambda e, xs=xs, tt0=tt0, b=b: e.dma_start(
                        out=s.XT[b, :, :, tt0:tt0 + 128].rearrange("k p t -> p k t"), in_=xs[:]), reads=[xk], writes=[('XTo', b, tt0)])
                else:
                    ot, otk = otr.next()
                    for half in range(2):
                        pt, ptk = psr.next()
                        for jj in range(4):
                            c = half * 4 + jj
                            ph.op('pe', lambda e, pt=pt, xs=xs, jj=jj, c=c: e.transpose(
                                pt[:, jj * 128:(jj + 1) * 128], xs[:, c, :], s.ident[:]), reads=[xk, 'ident'], writes=[ptk])
                        if half == 0:
                            ph.op('act', lambda e, pt=pt, ot=ot: e.activation(out=ot[:, 0:512], in_=pt[:], func=AF.Copy), reads=[ptk], writes=[(otk, 0)])
                        else:
                            ph.op('dve', lambda e, pt=pt, ot=ot: e.tensor_copy(out=ot[:, 512:1024], in_=pt[:]), reads=[ptk], writes=[(otk, 1)])
                    dst = s.out_x[b, tt0 - CTX:tt0 - CTX + 128, :] if isx else s.out_c[b, tt0:tt0 + 128, :]
                    ph.op('sp', lambda e, ot=ot, dst=dst: e.dma_start(out=dst, in_=ot[:]), reads=[(otk, 0), (otk, 1)], writes=[('out', b, tt0)])
        done(s, ph)


def kernel_run(inputs, L, NB, DEPTH, n_cores, layers=None):
    nc, ninst = build(L, NB, DEPTH)
    consts = host_constants(L)
    in_maps = []
    for c in range(n_cores):
        m = {}
        bs = slice(c * NB, (c + 1) * NB)
        m["x"] = np.ascontiguousarray(inputs["x"][bs])
        m["c"] = np.ascontiguousarray(inputs["c"][bs])
        m["ctx"] = np.ascontiguousarray(inputs["ctx"][bs])
        for k in ("c_ctx", "ada_w", "ada_b", "norm1", "norm2", "w_in", "w_out", "win_qk_gain", "win_sink",
                  "glb_qk_gain", "w_router", "b_router", "w_gate_up", "w_down"):
            m[k] = np.ascontiguousarray(inputs[k])
        m["ret_decay"] = np.ascontiguousarray(inputs["ret_decay"]).reshape(DEPTH, 8)
        for k, v in consts.items():
            m["k_" + k] = v
        in_maps.append(m)
    res = run_bass_kernel_spmd(nc, in_maps, core_ids=list(range(n_cores)))
    if DEBUG:
        global DBG
        DBG = res.results
    xo = np.concatenate([r["out"] for r in res.results], axis=0)
    co = np.concatenate([r["ctx_out"] for r in res.results], axis=0)
    return xo, co


FUSED = True
DEBUG = False
DBG = None


def kernel(**inputs):
    inputs = {k: np.asarray(v) for k, v in inputs.items()}
    B, L, _ = inputs["x"].shape
    depth = inputs["ada_w"].shape[0]
    n_cores = 8
    NB = B // n_cores
    if FUSED:
        xo, _ = kernel_run(inputs, L, NB, depth, n_cores)
        return xo.astype(np.float32)
    x, ctx = inputs["x"], inputs["ctx"]
    for l in range(depth):
        li = dict(inputs)
        li["x"], li["ctx"] = x, ctx
        for k in ("ada_w", "ada_b", "norm1", "norm2", "w_in", "w_out", "ret_decay", "win_qk_gain", "win_sink",
                  "glb_qk_gain", "w_gate_up", "w_down"):
            li[k] = inputs[k][l:l + 1]
        x, ctx = kernel_run(li, L, NB, 1, n_cores)
    return x.astype(np.float32)
```
